# Optimizing a Trainium2 kernel written in Bass

```python
import math
import jax
import jax.numpy as jnp
from jax import lax
import numpy as np

D_MODEL = 1024
BATCH = 16
SEQ = 2048
DEPTH = 1

PLE_DIM = 256
RMS_EPS = 1e-6

ATT_PATTERNS = ((128, 1), (512, 4), (2048, 16))
ATT_GROUPS = 3
ATT_HEADS_PER_GROUP = 8
ATT_HEADS = ATT_GROUPS * ATT_HEADS_PER_GROUP
ATT_HEAD_DIM = 64
ATT_WIDTH = ATT_HEADS * ATT_HEAD_DIM
ATT_OUT_WIDTH = ATT_HEADS_PER_GROUP * ATT_HEAD_DIM
ATT_BLOCK = 128
ALIBI_MAX_BIAS = 8.0

SSD_EXPAND = 2
SSD_INNER = SSD_EXPAND * D_MODEL
SSD_HEAD_DIM = 64
SSD_HEADS = SSD_INNER // SSD_HEAD_DIM
SSD_GROUPS = 8
SSD_HEADS_PER_GROUP = SSD_HEADS // SSD_GROUPS
SSD_STATE = 128
SSD_CONV = 4
SSD_CHUNK = 128
SSD_CONV_CH = SSD_INNER + 2 * SSD_GROUPS * SSD_STATE

MOE_GROUPS = 4
MOE_EXPERTS_PER_GROUP = 4
MOE_EXPERTS = MOE_GROUPS * MOE_EXPERTS_PER_GROUP
MOE_TOP_K = 2
MOE_HIDDEN = D_MODEL // 2

IN_COLS = 3 * ATT_WIDTH + SSD_INNER + SSD_CONV_CH + SSD_HEADS + 2 * D_MODEL

kernel_name = "hybrid_dilated_attn_ssd_hmoe_block"


def rms_norm(x, g):
    xf = x.astype(jnp.float32)
    y = xf * lax.rsqrt(jnp.mean(xf * xf, axis=-1, keepdims=True) + RMS_EPS)
    return (y * g.astype(jnp.float32)).astype(x.dtype)


def alibi_slopes():
    h = jnp.arange(1, ATT_HEADS + 1, dtype=jnp.float32)
    return jnp.exp2(-ALIBI_MAX_BIAS * h / ATT_HEADS).reshape(ATT_GROUPS, ATT_HEADS_PER_GROUP)


def dilated_window_attention(q, k, v, slopes, window, dilation):
    b, s, h, dh = q.shape
    span = window // dilation
    n_sub = s // dilation
    nb = -(-n_sub // ATT_BLOCK)
    n_pad = nb * ATT_BLOCK

    def to_blocks(t):
        t = t.reshape(b, n_sub, dilation, h, dh).transpose(0, 2, 3, 1, 4)
        t = jnp.pad(t, ((0, 0), (0, 0), (0, 0), (0, n_pad - n_sub), (0, 0)))
        return t.reshape(b, dilation, h, nb, ATT_BLOCK, dh)

    def with_prev(t):
        prev = jnp.pad(t, ((0, 0), (0, 0), (0, 0), (1, 0), (0, 0), (0, 0)))[:, :, :, :nb]
        return jnp.concatenate([prev, t], axis=4)

    qb = to_blocks(q)
    kb = with_prev(to_blocks(k))
    vb = with_prev(to_blocks(v))
    scores = jnp.einsum('brhnqc,brhnkc->brhnqk', qb, kb,
                        preferred_element_type=jnp.float32) * (dh ** -0.5)
    qi = jnp.arange(ATT_BLOCK)[:, None] + ATT_BLOCK
    kj = jnp.arange(2 * ATT_BLOCK)[None, :]
    delta = qi - kj
    key_idx = (jnp.arange(nb)[:, None, None] - 1) * ATT_BLOCK + kj[None]
    valid = (delta >= 0) & (delta <= span) & (key_idx >= 0)
    bias = -slopes.astype(jnp.float32)[:, None, None] * (delta * dilation).astype(jnp.float32)
    scores = jnp.where(valid[None, None, None], scores + bias[None, None, :, None], -jnp.inf)
    m = jnp.max(scores, axis=-1, keepdims=True)
    e = jnp.exp(scores - m)
    den = jnp.sum(e, axis=-1)
    o = jnp.einsum('brhnqk,brhnkc->brhnqc', e, vb.astype(jnp.float32)) / den[..., None]
    lse = m[..., 0] + jnp.log(den)
    o = o.reshape(b, dilation, h, n_pad, dh)[:, :, :, :n_sub].transpose(0, 3, 1, 2, 4).reshape(b, s, h, dh)
    lse = lse.reshape(b, dilation, h, n_pad)[..., :n_sub].transpose(0, 3, 1, 2).reshape(b, s, h)
    return o, lse


def dilated_mixture_attention(q, k, v):
    b, s = q.shape[:2]
    slopes = alibi_slopes()
    outs, lses = [], []
    for g, (window, dilation) in enumerate(ATT_PATTERNS):
        o, l = dilated_window_attention(q[:, :, g], k[:, :, g], v[:, :, g], slopes[g], window, dilation)
        outs.append(o)
        lses.append(l)
    w = jax.nn.softmax(jnp.stack(lses), axis=0)
    o = jnp.sum(w[..., None] * jnp.stack(outs), axis=0)
    return o.reshape(b, s, ATT_OUT_WIDTH).astype(q.dtype)


def causal_depthwise_conv(x, w, bias):
    k, c = w.shape
    y = lax.conv_general_dilated(x, w[:, None, :], window_strides=(1,), padding=((k - 1, 0),),
                                 dimension_numbers=('NWC', 'WIO', 'NWC'), feature_group_count=c)
    return y + bias


def ssd_chunked(xh, dt, a, bm, cm):
    b, s, g, j, p = xh.shape
    n = bm.shape[-1]
    l = SSD_CHUNK
    c = s // l
    x_dt = (xh * dt[..., None]).reshape(b, c, l, g, j, p)
    a_dt = (dt * a).reshape(b, c, l, g, j).transpose(0, 3, 4, 1, 2)
    bc = bm.reshape(b, c, l, g, n)
    cc = cm.reshape(b, c, l, g, n)
    a_cs = jnp.cumsum(a_dt, axis=-1)
    causal = jnp.tril(jnp.ones((l, l), dtype=bool))
    seg = a_cs[..., :, None] - a_cs[..., None, :]
    cb = jnp.einsum('bclgn,bcsgn->bgcls', cc, bc)
    mix = jnp.exp(jnp.where(causal, seg, -jnp.inf)) * cb[:, :, None]
    y_diag = jnp.einsum('bgjcls,bcsgjp->bclgjp', mix, x_dt)
    decay_states = jnp.exp(a_cs[..., -1:] - a_cs).transpose(0, 3, 4, 1, 2)
    states = jnp.einsum('bcsgn,bcsgjp->bcgjpn', bc, x_dt * decay_states[..., None])
    chunk_decay = jnp.exp(a_cs[..., -1])

    def step(carry, inp):
        st, dec = inp
        return carry * dec[..., None, None] + st, carry

    init = jnp.zeros((b, g, j, p, n), jnp.float32)
    _, prev = lax.scan(step, init, (states.transpose(1, 0, 2, 3, 4, 5), chunk_decay.transpose(3, 0, 1, 2)))
    state_decay = jnp.exp(a_cs).transpose(0, 3, 4, 1, 2)
    y_off = jnp.einsum('bclgn,cbgjpn->bclgjp', cc, prev) * state_decay[..., None]
    return (y_diag + y_off).reshape(b, s, g, j, p)


def mamba2_mixer(z, xbc, dt_raw, conv_w, conv_b, dt_bias, a_log, d_skip, norm_g):
    b, s, _ = z.shape
    f32 = jnp.float32
    xbc = jax.nn.silu(causal_depthwise_conv(xbc, conv_w, conv_b))
    xs, bm, cm = jnp.split(xbc, [SSD_INNER, SSD_INNER + SSD_GROUPS * SSD_STATE], axis=-1)
    dt = jax.nn.softplus(dt_raw.astype(f32) + dt_bias.astype(f32))
    a = -jnp.exp(a_log.astype(f32))
    xh = xs.astype(f32).reshape(b, s, SSD_GROUPS, SSD_HEADS_PER_GROUP, SSD_HEAD_DIM)
    y = ssd_chunked(xh,
                    dt.reshape(b, s, SSD_GROUPS, SSD_HEADS_PER_GROUP),
                    a.reshape(SSD_GROUPS, SSD_HEADS_PER_GROUP),
                    bm.astype(f32).reshape(b, s, SSD_GROUPS, SSD_STATE),
                    cm.astype(f32).reshape(b, s, SSD_GROUPS, SSD_STATE))
    y = y + xh * d_skip.astype(f32).reshape(SSD_GROUPS, SSD_HEADS_PER_GROUP)[:, :, None]
    y = y.reshape(b, s, SSD_INNER).astype(z.dtype)
    return rms_norm(y * jax.nn.silu(z), norm_g)


def hierarchical_moe(h, w_rg, b_rg, w_re, b_re, w_gate, w_up, w_down):
    bsz, s, d = h.shape
    t = h.reshape(bsz * s, d)
    g_logits = (t @ w_rg).astype(jnp.float32) + b_rg.astype(jnp.float32)
    g_prob = jax.nn.softmax(g_logits, axis=-1)
    g_val, g_idx = lax.top_k(g_prob, 1)
    e_logits = ((t @ w_re).astype(jnp.float32) + b_re.astype(jnp.float32)).reshape(
        -1, MOE_GROUPS, MOE_EXPERTS_PER_GROUP)
    within = jnp.einsum('tg,tge->te', jax.nn.one_hot(g_idx[:, 0], MOE_GROUPS, dtype=jnp.float32), e_logits)
    e_val, e_idx = lax.top_k(within, MOE_TOP_K)
    e_w = jax.nn.softmax(e_val, axis=-1)
    expert_id = g_idx * MOE_EXPERTS_PER_GROUP + e_idx
    combine = g_val * jnp.sum(e_w[..., None] * jax.nn.one_hot(expert_id, MOE_EXPERTS, dtype=jnp.float32), axis=1)
    combine = combine.astype(t.dtype)
    out = jnp.zeros_like(t)
    for e in range(MOE_EXPERTS):
        hid = jax.nn.silu(t @ w_gate[e]) * (t @ w_up[e])
        out = out + combine[:, e:e + 1] * (hid @ w_down[e])
    return out.reshape(bsz, s, d)


def setup_inputs(seed: int = 0) -> dict:
    key = jax.random.key(seed)
    ks = jax.random.split(key, 32)
    f32 = jnp.float32

    def nrm(k, shape, fan_in):
        return jax.random.normal(k, shape, f32) * (fan_in ** -0.5)

    def gain(k, shape):
        return 1.0 + 0.05 * jax.random.normal(k, shape, f32)

    dt0 = jnp.exp(jax.random.uniform(ks[6], (DEPTH, SSD_HEADS), f32, math.log(1e-3), math.log(1e-1)))
    return {
        'x': jax.random.normal(ks[0], (BATCH, SEQ, D_MODEL), f32),
        'p': jax.random.normal(ks[1], (DEPTH, BATCH, SEQ, PLE_DIM), f32),
        'norm_mix_g': gain(ks[2], (DEPTH, D_MODEL)),
        'w_in': nrm(ks[3], (DEPTH, D_MODEL, IN_COLS), D_MODEL),
        'conv_w': nrm(ks[4], (DEPTH, SSD_CONV, SSD_CONV_CH), SSD_CONV),
        'conv_b': 0.02 * jax.random.normal(ks[5], (DEPTH, SSD_CONV_CH), f32),
        'dt_bias': dt0 + jnp.log(-jnp.expm1(-dt0)),
        'a_log': jnp.log(jax.random.uniform(ks[7], (DEPTH, SSD_HEADS), f32, 1.0, 16.0)),
        'd_skip': gain(ks[8], (DEPTH, SSD_HEADS)),
        'ssd_norm_g': gain(ks[9], (DEPTH, SSD_INNER)),
        'w_att_branch': nrm(ks[10], (DEPTH, ATT_OUT_WIDTH, D_MODEL), ATT_OUT_WIDTH),
        'w_ssd_branch': nrm(ks[11], (DEPTH, SSD_INNER, D_MODEL), SSD_INNER),
        'w_out': nrm(ks[12], (DEPTH, D_MODEL, D_MODEL), D_MODEL),
        'norm_ffn_g': gain(ks[13], (DEPTH, D_MODEL)),
        'w_router_group': nrm(ks[14], (DEPTH, D_MODEL, MOE_GROUPS), D_MODEL),
        'b_router_group': 0.01 * jax.random.normal(ks[15], (DEPTH, MOE_GROUPS), f32),
        'w_router_expert': nrm(ks[16], (DEPTH, D_MODEL, MOE_EXPERTS), D_MODEL),
        'b_router_expert': 0.01 * jax.random.normal(ks[17], (DEPTH, MOE_EXPERTS), f32),
        'w_exp_gate': nrm(ks[18], (DEPTH, MOE_EXPERTS, D_MODEL, MOE_HIDDEN), D_MODEL),
        'w_exp_up': nrm(ks[19], (DEPTH, MOE_EXPERTS, D_MODEL, MOE_HIDDEN), D_MODEL),
        'w_exp_down': nrm(ks[20], (DEPTH, MOE_EXPERTS, MOE_HIDDEN, D_MODEL), MOE_HIDDEN),
        'norm_ple_g': gain(ks[21], (DEPTH, D_MODEL)),
        'w_ple_gate': nrm(ks[22], (DEPTH, D_MODEL, D_MODEL), D_MODEL),
        'w_ple_proj': nrm(ks[23], (DEPTH, PLE_DIM, D_MODEL), PLE_DIM),
        'final_norm_g': gain(ks[24], (D_MODEL,)),
    }


def reference(x, p, norm_mix_g, w_in, conv_w, conv_b, dt_bias, a_log, d_skip, ssd_norm_g,
              w_att_branch, w_ssd_branch, w_out, norm_ffn_g, w_router_group, b_router_group,
              w_router_expert, b_router_expert, w_exp_gate, w_exp_up, w_exp_down,
              norm_ple_g, w_ple_gate, w_ple_proj, final_norm_g):
    b, s, _ = x.shape
    split_at = list(np.cumsum([ATT_WIDTH, ATT_WIDTH, ATT_WIDTH, SSD_INNER, SSD_CONV_CH, SSD_HEADS, D_MODEL]))
    att_shape = (b, s, ATT_GROUPS, ATT_HEADS_PER_GROUP, ATT_HEAD_DIM)
    for i in range(DEPTH):
        h = rms_norm(x, norm_mix_g[i])
        proj = h @ w_in[i]
        q, k, v, z, xbc, dt_raw, gate_a, gate_s = jnp.split(proj, [int(o) for o in split_at], axis=-1)
        att = dilated_mixture_attention(q.reshape(att_shape), k.reshape(att_shape), v.reshape(att_shape))
        y_att = att @ w_att_branch[i]
        ssd = mamba2_mixer(z, xbc, dt_raw, conv_w[i], conv_b[i], dt_bias[i], a_log[i], d_skip[i], ssd_norm_g[i])
        y_ssd = ssd @ w_ssd_branch[i]
        merged = jax.nn.sigmoid(gate_a) * y_att + jax.nn.sigmoid(gate_s) * y_ssd
        x = x + merged @ w_out[i]
        x = x + hierarchical_moe(rms_norm(x, norm_ffn_g[i]), w_router_group[i], b_router_group[i],
                                 w_router_expert[i], b_router_expert[i],
                                 w_exp_gate[i], w_exp_up[i], w_exp_down[i])
        ple_gate = jax.nn.sigmoid(rms_norm(x, norm_ple_g[i]) @ w_ple_gate[i])
        x = x + ple_gate * (p[i] @ w_ple_proj[i])
    return rms_norm(x, final_norm_g)
```

```python
import numpy as np
from contextlib import ExitStack
import concourse.bass as bass
import concourse.mybir as mybir
from concourse.bass_utils import run_bass_kernel_spmd

F32 = mybir.dt.float32
BF16 = mybir.dt.bfloat16
AF = mybir.ActivationFunctionType
ALU = mybir.AluOpType
AX = mybir.AxisListType

NCORES = 8
D = 1024
SEQ = 2048
NT = 16
KC = 8
PLE = 256
EPS = 1e-6
IN_COLS = 12832
OFF_Q, OFF_K, OFF_V, OFF_Z, OFF_X, OFF_B, OFF_C, OFF_DT, OFF_GA, OFF_GS = (
    0, 1536, 3072, 4608, 6656, 8704, 9728, 10752, 10784, 11808)
ATT_PAT = ((128, 1), (512, 4), (2048, 16))
NEXP = 16

ENGS = ("pe", "act", "dve", "pool", "sp")


class _Op:
    __slots__ = ("eng", "fn", "deps", "is_dma", "lane", "sig", "sigval", "fin")


class Sched:
    def __init__(self, nc, es, sem_rot=30000):
        self.nc, self.es = nc, es
        self.ops = {e: [] for e in ENGS}
        self.all_ops = []
        self.last_w = {}
        self.readers = {}
        self.lanes = {}
        self.sem_rot = sem_rot
        self.nsem = 0

    def op(self, eng, fn, reads=(), writes=(), dma=False, lane=None):
        o = _Op()
        o.eng, o.fn, o.is_dma, o.lane = eng, fn, dma, lane
        o.sig, o.sigval = False, None
        o.fin = 0.0
        def _bank(k):
            if k.startswith("pb"):
                return k[:3]
            if k.startswith("tp"):
                return "tp"
            return None
        banks = [b for b in (_bank(k) for k in list(reads) + list(writes)) if b is not None]
        reads = [k for k in reads if _bank(k) is None]
        writes = [k for k in writes if _bank(k) is None] + sorted(set(banks))
        deps = []
        for k in reads:
            w = self.last_w.get(k)
            if w is not None:
                deps.append(w)
        for k in writes:
            w = self.last_w.get(k)
            if w is not None:
                deps.append(w)
            deps.extend(self.readers.get(k, ()))
        seen = set()
        dl = []
        for d in deps:
            if id(d) in seen or d is o:
                continue
            seen.add(id(d))
            if (not d.is_dma) and (not dma) and d.eng == "pe" and eng == "pe":
                continue
            dl.append((d, self.lanes[d.lane]["count"] if d.is_dma else None))
        o.deps = dl
        if dma:
            L = self.lanes.setdefault(lane, {"count": 0, "sem": None})
            L["count"] += 16
        for k in writes:
            self.last_w[k] = o
            self.readers[k] = []
        for k in reads:
            if k not in writes:
                self.readers.setdefault(k, []).append(o)
        self.ops[eng].append(o)
        self.all_ops.append(o)
        return o

    def peek_fin(self, reads, writes):
        def _bank(k):
            if k.startswith("pb"):
                return k[:3]
            if k.startswith("tp"):
                return "tp"
            return None
        t = 0.0
        for k in list(reads) + list(writes):
            b = _bank(k)
            kk = b if b is not None else k
            w = self.last_w.get(kk)
            if w is not None and w.fin > t:
                t = w.fin
            if b is not None or k in writes:
                for r in self.readers.get(kk, ()):
                    if r.fin > t:
                        t = r.fin
        return t

    def barrier(self):
        lasts = []
        for e in ENGS:
            for o in reversed(self.ops[e]):
                if not o.is_dma and o.fn is not None:
                    lasts.append(o)
                    break
        lane_last = {}
        for o in self.all_ops:
            if o.is_dma:
                lane_last[o.lane] = o
        lasts.extend(lane_last.values())
        for e in ENGS:
            o = _Op()
            o.eng, o.fn, o.is_dma, o.lane = e, None, False, None
            o.sig, o.sigval = False, None
            o.fin = 0.0
            o.deps = [(d, self.lanes[d.lane]["count"] if d.is_dma else None) for d in lasts
                      if not (d.eng == "pe" and e == "pe" and not d.is_dma)]
            self.ops[e].append(o)
            self.all_ops.append(o)
        self.last_w = {}
        self.readers = {}

    def emit(self):
        nc, es = self.nc, self.es
        for o in self.all_ops:
            for d, _v in o.deps:
                if not d.is_dma:
                    d.sig = True
        for e in ENGS:
            cnt, sem = 0, None
            for o in self.ops[e]:
                if o.is_dma or not o.sig:
                    continue
                if sem is None or cnt >= self.sem_rot:
                    sem = es.enter_context(nc.semaphore(f"s_{e}_{self.nsem}"))
                    self.nsem += 1
                    cnt = 0
                cnt += 1
                o.sigval = (sem, cnt)
        for name, L in self.lanes.items():
            L["sem"] = es.enter_context(nc.semaphore(f"l_{self.nsem}"))
            self.nsem += 1
        block = es.enter_context(nc.Block())
        lanes = self.lanes

        def make(e):
            ops = self.ops[e]

            def body(eng):
                waited = {}
                for o in ops:
                    need = {}
                    for d, dv in o.deps:
                        if d.is_dma:
                            sem, val = lanes[d.lane]["sem"], dv
                        else:
                            sem, val = d.sigval
                        k = id(sem)
                        if k not in need or need[k][1] < val:
                            need[k] = (sem, val)
                    for k, (sem, val) in need.items():
                        if waited.get(k, 0) >= val:
                            continue
                        eng.wait_ge(sem, val)
                        waited[k] = val
                    if o.fn is None:
                        continue
                    ins = o.fn(eng)
                    if o.is_dma:
                        ins.then_inc(lanes[o.lane]["sem"], 16)
                    elif o.sig:
                        ins.then_inc(o.sigval[0], 1)
            return body

        block.tensor(make("pe"))
        block.scalar(make("act"))
        block.vector(make("dve"))
        block.gpsimd(make("pool"))
        block.sync(make("sp"))


class Mem:
    BASE = 16512
    TOP = 229344

    def __init__(self, nc):
        self.nc = nc
        self.off = self.BASE
        self.n = 0
        self.peak = 0

    def alloc(self, shape, dt, name=None):
        nbytes = int(np.prod(shape[1:])) * (4 if dt == F32 else 2)
        self.off = (self.off + 63) // 64 * 64
        off = self.off
        self.off += nbytes
        self.peak = max(self.peak, self.off)
        assert self.off <= self.TOP, f"SBUF overflow: {self.off} > {self.TOP} ({name})"
        self.n += 1
        return self.nc.alloc_sbuf_tensor_at(f"{name or 't'}{self.n}", list(shape), dt, offset=off)

    def mark(self):
        return self.off

    def release(self, m):
        self.off = m


class Prog:
    def __init__(self, nseq, stop_after=None, dbg=False, skip=()):
        self.skip = skip
        self.defer = None
        self.nseq = nseq
        self.stop_after = stop_after
        self.dbg = dbg

    def din(self, name):
        if name not in self._din:
            self._din[name] = self.nc.dram_tensor(name, list(self._dshapes[name]), F32, kind="ExternalInput").ap()
        return self._din[name]

    def rec(self, eng, fn, reads=(), writes=(), cost=0.2, dma=False, lane=None):
        if self.defer is not None and not dma:
            self.defer.append((eng, fn, list(reads), list(writes), cost))
            return
        self.S.op(eng, fn, reads=reads, writes=writes, dma=dma, lane=lane)

    @staticmethod
    def _n(ap):
        n = 1
        for d in ap.shape[1:]:
            n *= int(d)
        return n

    def _ecost(self, eng, out):
        n = self._n(out)
        if eng == "act":
            return n / 1200.0 + 0.25
        if eng == "pool":
            return n / 480.0 + 0.25
        return n / 960.0 + 0.12

    def merge(self, streams):
        S = self.S
        free = {e: 0.0 for e in ENGS}
        idx = [0] * len(streams)
        total = sum(len(st) for st in streams)
        for _ in range(total):
            best, bt = None, None
            for si, st in enumerate(streams):
                if idx[si] >= len(st):
                    continue
                eng, fn, r, w, cost = st[idx[si]]
                t = max(free[eng], S.peek_fin(r, w))
                if bt is None or t < bt - 1e-9:
                    best, bt = si, t
            eng, fn, r, w, cost = streams[best][idx[best]]
            idx[best] += 1
            o = S.op(eng, fn, reads=r, writes=w)
            o.fin = bt + cost
            free[eng] = o.fin
        for st in streams:
            pass
        for o in S.all_ops:
            o.fin = 0.0

    def mm(self, out, lhsT, rhs, start, stop, r, w):
        cost = self._n(rhs) * (4 if lhsT.dtype == F32 else 1) / 2400.0 + 0.03
        self.rec("pe", lambda e: e.matmul(out, lhsT=lhsT, rhs=rhs, start=start, stop=stop,
                                          skip_group_check=True), reads=r, writes=w, cost=cost)

    def tr(self, out, in_, ident, r, w):
        self.rec("pe", lambda e: e.transpose(out=out, in_=in_, identity=ident), reads=r, writes=w, cost=0.11)

    def act(self, out, in_, func, r, w, bias=None, scale=None, accum=None, eng="act"):
        kw = {}
        if bias is not None:
            kw["bias"] = bias
        if scale is not None:
            kw["scale"] = scale
        if accum is not None:
            kw["accum_out"] = accum
        self.rec("act", lambda e: e.activation(out=out, in_=in_, func=func, **kw), reads=r, writes=w,
                 cost=self._ecost("act", out))

    def tt(self, eng, out, in0, in1, op, r, w):
        self.rec(eng, lambda e: e.tensor_tensor(out=out, in0=in0, in1=in1, op=op), reads=r, writes=w,
                 cost=self._ecost(eng, out))

    def ts(self, eng, out, in0, s1, s2, op0, op1, r, w):
        if op1 is None:
            self.rec(eng, lambda e: e.tensor_scalar(out=out, in0=in0, scalar1=s1, scalar2=None, op0=op0),
                     reads=r, writes=w, cost=self._ecost(eng, out))
        else:
            self.rec(eng, lambda e: e.tensor_scalar(out=out, in0=in0, scalar1=s1, scalar2=s2, op0=op0, op1=op1),
                     reads=r, writes=w, cost=self._ecost(eng, out))

    def stt(self, eng, out, in0, scalar, in1, op0, op1, r, w):
        self.rec(eng, lambda e: e.scalar_tensor_tensor(out=out, in0=in0, scalar=scalar, in1=in1, op0=op0, op1=op1),
                 reads=r, writes=w, cost=self._ecost(eng, out))

    def cp(self, eng, out, in_, r, w):
        self.rec(eng, lambda e: e.tensor_copy(out=out, in_=in_), reads=r, writes=w, cost=self._ecost(eng, out))

    def memset(self, eng, ap, val, w):
        self.rec(eng, lambda e: e.memset(ap, val), writes=w, cost=self._ecost(eng, ap))

    def dma(self, eng, out, in_, r, w, lane):
        self.rec(eng, lambda e: e.dma_start(out=out, in_=in_), reads=r, writes=w, dma=True, lane=lane)

    def wload(self, dst, src2d, key, eng="pool"):
        self.dma(eng, dst, src2d.rearrange("(kc p) n -> p kc n", p=128), [], [key], key)

    def build(self):
        nc = bass.Bass("TRN2", target_bir_lowering=False)
        self.nc = nc
        ns = self.nseq
        self._din = {}
        self._dshapes = {
            "x": (ns, SEQ, D), "p": (ns, SEQ, PLE), "w_in": (D, IN_COLS), "w_att": (512, D), "w_ssd": (2048, D),
            "w_out": (D, D), "w_eg": (NEXP, D, 512), "w_eu": (NEXP, D, 512), "w_ed": (NEXP, 512, D),
            "w_pg": (D, D), "w_pp": (PLE, D), "w_r": (D, 20), "gains": (4, D), "vec32": (3, 32), "b_r": (1, 20),
            "convw": (128, 32, 4), "convb": (128, 32), "ng": (128, 16), "c_ident": (128, 128), "c_tri": (128, 128),
            "c_mask": (128, 512), "c_em": (128, 24, 256),
        }
        self.out_d = nc.dram_tensor("out", [ns, SEQ, D], F32, kind="ExternalOutput").ap()
        self.dbg_outs = {}
        with ExitStack() as es:
            self.es = es
            self.S = Sched(nc, es)
            self.M = Mem(nc)
            self.alloc_psum()
            self.setup_consts()
            for s in range(ns):
                self.seq(s)
                if self.stop_after is not None:
                    break
            self.S.barrier()
            self.S.emit()
        return nc

    def alloc_psum(self):
        nc, es = self.nc, self.es
        self.pb = [es.enter_context(nc.psum_tensor(f"pb{i}", [128, 512], F32)) for i in range(7)]
        self.tp = es.enter_context(nc.psum_tensor("tp", [128, 1024], BF16))

    def dbg_dump(self, name, ap, shape, keys, dt=F32):
        d = self.nc.dram_tensor("dbg_" + name, list(shape), dt, kind="ExternalOutput").ap()
        self.dbg_outs[name] = d
        self.dma("sp", d, ap, keys, [], "dbg_" + name)

    def setup_consts(self):
        M = self.M
        S = self.S
        self.identf = M.alloc([128, 128], F32, "identf")
        self.identb = M.alloc([128, 128], BF16, "identb")
        self.tri = M.alloc([128, 128], F32, "tri")
        self.onesf = M.alloc([128, 128], F32, "onesf")
        self.maskb = M.alloc([128, 512], BF16, "maskb")
        self.ddiag = M.alloc([128, 32, 128], BF16, "ddiag")
        self.gbc = M.alloc([128, D], F32, "gbc")
        self.cw = M.alloc([128, 32, 4], F32, "cw")
        self.cb = M.alloc([128, 32], F32, "cb")
        self.ngt = M.alloc([128, 16], F32, "ngt")
        self.v32 = M.alloc([128, 3, 32], F32, "v32")
        self.abc = M.alloc([128, 32], F32, "abc")
        self.brb = M.alloc([128, 20], F32, "brb")
        self.wrf = M.alloc([128, KC, 20], F32, "wrf")
        self.wrhi = M.alloc([128, KC, 20], BF16, "wrhi")
        self.wrlo = M.alloc([128, KC, 20], BF16, "wrlo")
        self.dma("sp", self.identf[:], self.din("c_ident"), [], ["identf"], "c0")
        self.dma("sp", self.tri[:], self.din("c_tri"), [], ["tri"], "c1")
        self.dma("pool", self.maskb[:], self.din("c_mask"), [], ["maskb"], "c2")
        self.dma("sp", self.cw[:], self.din("convw"), [], ["cw"], "c4")
        self.dma("sp", self.cb[:], self.din("convb"), [], ["cb"], "c5")
        self.dma("sp", self.ngt[:], self.din("ng"), [], ["ngt"], "c6")
        self.dma("sp", self.v32[:].rearrange("p a b -> p (a b)"),
                 self.din("vec32").rearrange("a b -> (a b)").partition_broadcast(128), [], ["v32"], "c7")
        self.dma("sp", self.brb[:], self.din("b_r")[0].partition_broadcast(128), [], ["brb"], "c8")
        self.dma("sp", self.wrf[:], self.din("w_r").rearrange("(kc p) n -> p kc n", p=128), [], ["wrf"], "c9")
        self.cp("dve", self.identb[:], self.identf[:], ["identf"], ["identb"])
        self.memset("dve", self.onesf[:], 1.0, ["onesf"])
        self.act(self.abc[:], self.v32[:, 1, :], AF.Exp, ["v32"], ["abc"])
        self.ts("dve", self.abc[:], self.abc[:], -1.0, None, ALU.mult, None, ["abc"], ["abc"])
        for h in range(32):
            self.ts("dve", self.ddiag[:, h, :], self.identf[:], self.v32[:, 2, h:h + 1], None, ALU.mult, None,
                    ["identf", "v32"], ["ddiag"])
        self.cp("dve", self.wrhi[:], self.wrf[:], ["wrf"], ["wrhi"])
        self.tt("dve", self.wrf[:], self.wrf[:], self.wrhi[:], ALU.subtract, ["wrf", "wrhi"], ["wrf"])
        self.cp("dve", self.wrlo[:], self.wrf[:], ["wrf"], ["wrlo"])
        self.const_mark = M.mark()

    def load_gain(self, idx):
        self.dma("sp", self.gbc[:], self.din("gains")[idx].partition_broadcast(128), [], ["gbc"], "gbc")

    def norm_T(self, xr, xkeys, outT, outkey, gidx, lo=None):
        M = self.M
        mk = M.mark()
        ssq = M.alloc([128, NT], F32, "ssq")
        rs = M.alloc([128, NT], F32, "rs")
        junk = M.alloc([128, D], BF16, "junk")
        hn = [M.alloc([128, D], BF16, "hn") for _ in range(2)]
        self.load_gain(gidx)
        self.memset("dve", ssq[:], 0.0, ["ssq"])
        for t in range(NT):
            self.act(junk[:], xr[:, t, :], AF.Square, [xkeys(t), "ssq"], [f"ssq:{t}", "junk"], accum=ssq[:, t:t + 1])
        self.ts("dve", rs[:], ssq[:], 1.0 / D, EPS, ALU.mult, ALU.add, [f"ssq:{t}" for t in range(NT)], ["rs"])
        self.act(rs[:], rs[:], AF.Ln, ["rs"], ["rs"])
        self.act(rs[:], rs[:], AF.Exp, ["rs"], ["rs"], scale=-0.5)
        for t in range(NT):
            h = hn[t % 2]
            hk = f"hn{t % 2}"
            self.stt("dve", h[:], xr[:, t, :], rs[:, t:t + 1], self.gbc[:], ALU.mult, ALU.mult,
                     [xkeys(t), "rs", "gbc"], [hk])
            for c in range(KC):
                self.tr(self.tp[:, c * 128:(c + 1) * 128], h[:, c * 128:(c + 1) * 128], self.identb[:],
                        [hk, "identb"], ["tp"])
            self.act(outT[:, :, t * 128:(t + 1) * 128], self.tp[:, :].rearrange("p (c n) -> p c n", c=KC), AF.Copy,
                     ["tp"], [f"{outkey}:{t}"])
        M.release(mk)
        return rs

    def seq(self, s):
        M = self.M
        S = self.S
        M.release(self.const_mark)
        self.hT = M.alloc([128, KC, SEQ], BF16, "hT")
        self.big = M.mark()
        xr = M.alloc([128, NT, D], F32, "xr")
        for t in range(NT):
            self.dma("sp", xr[:, t, :], self.din("x")[s, t * 128:(t + 1) * 128, :], [], [f"xr:{t}"], f"xr{t % 4}")
        self.norm_T(xr, lambda t: f"xr:{t}", self.hT, "hT", 0)
        S.barrier()
        if self.stop_after == "norm":
            self.dbg_hT()
            return
        M.release(self.big)
        self.hole = M.mark()
        if self.phase_ssd(s):
            return
        if self.phase_att(s):
            return
        if self.phase_out(s):
            return
        if self.phase_moe(s):
            return
        self.phase_ple(s)

    def dbg_hT(self):
        M = self.M
        tmp = M.alloc([128, KC, SEQ], F32, "dbgt")
        self.cp("dve", tmp[:], self.hT[:], [f"hT:{t}" for t in range(NT)], ["dbgt"])
        self.dbg_dump("hT", tmp[:], (128, KC, SEQ), ["dbgt"])

    def phase_ssd(self, s):
        M, S = self.M, self.S
        hT = self.hT
        hkeys = [f"hT:{t}" for t in range(NT)]
        GT = M.alloc([128, 16, SEQ], BF16, "GT")
        ssq = M.alloc([128, NT, 8], F32, "ssq_ssd")
        scr = M.mark()
        self.mb_start = scr
        if "ssd" in self.skip:
            self.Mb = M.alloc([128, NT, D], BF16, "Mb")
            self.mb_end = M.mark()
            for t in range(NT):
                self.memset("dve", self.Mb[:, t, :], 0.0, [f"Mb:{t}:0", f"Mb:{t}:1"])
            S.barrier()
            return False
        adt = M.alloc([128, NT, 32], F32, "adt")
        cdec = M.alloc([128, NT, 32], F32, "cdec")
        sd = M.alloc([128, NT, 32], F32, "sd")
        biasL = M.alloc([128, NT, 32], F32, "biasL")
        dtmark = M.mark()
        wdt = M.alloc([128, KC, 32], BF16, "wdt")
        spre = M.alloc([128, NT, 32], F32, "spre")
        dtt = M.alloc([128, NT, 32], F32, "dtt")
        lndt = M.alloc([128, NT, 32], F32, "lndt")
        self.wload(wdt[:], self.din("w_in")[:, OFF_DT:OFF_DT + 32], "wdt")
        self.memset("dve", ssq[:], 0.0, ["ssq_ssd"])
        pb = self.pb
        for t in range(NT):
            for kc in range(KC):
                self.mm(pb[0][:, t * 32:(t + 1) * 32], hT[:, kc, t * 128:(t + 1) * 128], wdt[:, kc, :],
                        kc == 0, kc == KC - 1, [hkeys[t], "wdt"], ["pb0"])
        b3 = lambda ap: ap.rearrange("p (t h) -> p t h", h=32)
        self.tt("dve", spre[:], b3(pb[0][:, :]), self.v32[:, 0, :].unsqueeze(1).to_broadcast([128, NT, 32]), ALU.add,
                ["pb0", "v32"], ["spre"])
        self.act(spre[:], spre[:], AF.Exp, ["spre"], ["spre"])
        self.ts("dve", spre[:], spre[:], 1.0, None, ALU.add, None, ["spre"], ["spre"])
        self.act(dtt[:], spre[:], AF.Ln, ["spre"], ["dtt"])
        self.act(lndt[:], dtt[:], AF.Ln, ["dtt"], ["lndt"])
        self.tt("dve", adt[:], dtt[:], self.abc[:].unsqueeze(1).to_broadcast([128, NT, 32]), ALU.mult,
                ["dtt", "abc"], ["adt"])
        for c in range(NT):
            self.mm(pb[1][:, c * 32:(c + 1) * 32], self.tri[:], adt[:, c, :], True, True, ["tri", "adt"], ["pb1"])
            self.mm(pb[2][:, c * 32:(c + 1) * 32], self.onesf[:], adt[:, c, :], True, True, ["onesf", "adt"], ["pb2"])
        self.act(cdec[:], b3(pb[2][:, :]), AF.Exp, ["pb2"], ["cdec"])
        self.act(sd[:], b3(pb[1][:, :]), AF.Exp, ["pb1"], ["sd"])
        self.stt("dve", biasL[:], b3(pb[1][:, :]), -1.0, lndt[:], ALU.mult, ALU.add, ["pb1", "lndt"], ["biasL"])
        if self.stop_after == "dt":
            self.dbg_dump("dtt", dtt[:], (128, NT, 32), ["dtt"])
            self.dbg_dump("biasL", biasL[:], (128, NT, 32), ["biasL"])
            self.dbg_dump("cdec", cdec[:], (128, NT, 32), ["cdec"])
            return True
        S.barrier()
        M.release(dtmark)
        wx = M.alloc([128, KC, 256], BF16, "wx")
        wB = M.alloc([128, KC, 128], BF16, "wB")
        wC = M.alloc([128, KC, 128], BF16, "wC")
        wz = M.alloc([128, KC, 256], BF16, "wz")
        junk = M.alloc([128, 256], BF16, "junk2")
        gb = []
        for gi in range(2):
            gb.append(dict(xT=M.alloc([128, 2, SEQ], BF16, "xT"), BT=M.alloc([128, SEQ], BF16, "BT"),
                           CT=M.alloc([128, SEQ], BF16, "CT"), sz=M.alloc([128, NT, 256], BF16, "sz"),
                           S32=M.alloc([128, 256], F32, "S32"), Sbf=M.alloc([128, 256], BF16, "Sbf")))
        amark = M.mark()
        HS = SEQ // 2
        raws = [M.alloc([128, 3 + HS], F32, "raw") for _ in range(2)]
        caccs = [M.alloc([128, HS], F32, "cacc") for _ in range(2)]
        M.release(amark)
        self._convi = 0
        sets = []
        for par in range(2):
            sets.append(dict(
                E=M.alloc([128, 4, 128], F32, "E"), mixT=M.alloc([128, 4, 128], BF16, "mixT"),
                xB=M.alloc([128, 384], BF16, "xB"), xds=M.alloc([128, 256], BF16, "xds"),
                adtb=M.alloc([128, 4, 128], F32, "adtb"), ysb=M.alloc([128, 256], F32, "ysb"),
                tmp=M.alloc([128, 256], F32, "tmp"), G=M.alloc([128, 256], BF16, "G"),
            ))
        tpB = self.pb[3].bitcast(BF16)
        bankset = [dict(cb=pb[4], R=pb[5], Y=pb[6], tp=self.tp, kcb="pb4", kR="pb5", kY="pb6", ktp="tp"),
                   dict(cb=pb[0], R=pb[1], Y=pb[2], tp=tpB, kcb="pb0", kR="pb1", kY="pb2", ktp="pb3")]

        def inproj(g, gi):
            B = gb[gi]
            sfx = f"{gi}"
            xT, BT, CT, sz = B["xT"], B["BT"], B["CT"], B["sz"]
            self.wload(wx[:], self.din("w_in")[:, OFF_X + 256 * g:OFF_X + 256 * (g + 1)], "wx")
            self.wload(wB[:], self.din("w_in")[:, OFF_B + 128 * g:OFF_B + 128 * (g + 1)], "wB")
            self.wload(wC[:], self.din("w_in")[:, OFF_C + 128 * g:OFF_C + 128 * (g + 1)], "wC")
            self.wload(wz[:], self.din("w_in")[:, OFF_Z + 256 * g:OFF_Z + 256 * (g + 1)], "wz")
            for t in range(NT):
                bank = pb[3 + (t // 2) % 2]
                bk = f"pb{3 + (t // 2) % 2}"
                half = (t % 2) * 256
                for kc in range(KC):
                    self.mm(bank[:, half:half + 256], hT[:, kc, t * 128:(t + 1) * 128], wz[:, kc, :],
                            kc == 0, kc == KC - 1, [hkeys[t], "wz"], [bk])
                if t % 2 == 1:
                    self.act(sz[:, t - 1:t + 1, :], bank[:, :].rearrange("p (a n) -> p a n", a=2), AF.Silu,
                             [bk], ["sz" + sfx])
            specs = [(wx, "wx", 0, 2 * g, xT[:, 0, :], "xT0" + sfx), (wx, "wx", 128, 2 * g + 1, xT[:, 1, :], "xT1" + sfx),
                     (wB, "wB", 0, 16 + g, BT[:, :], "BT" + sfx), (wC, "wC", 0, 24 + g, CT[:, :], "CT" + sfx)]
            units = []
            for (wt, wk, coff, ch, dst, dkey) in specs:
                for half in range(2):
                    ci = self._convi % 2
                    self._convi += 1
                    units.append((wt, wk, coff, ch, dst, dkey, half, ci))

            def stage_a(u):
                (wt, wk, coff, ch, dst, dkey, half, ci) = u
                raw, cacc = raws[ci], caccs[ci]
                rk, ck = f"raw{ci}", f"cacc{ci}"
                if half == 0:
                    self.memset("dve", raw[:, 0:3], 0.0, [rk + "h"])
                else:
                    self.cp("dve", raw[:, 0:3], raws[1 - ci][:, HS:HS + 3], [f"raw{1 - ci}:1"], [rk + "h"])
                for t2 in range(2):
                    tc = half * 2 + t2
                    bank = pb[tc % 2]
                    bk = f"pb{tc % 2}"
                    for kc in range(KC):
                        self.mm(bank[:, :], wt[:, kc, coff:coff + 128], hT[:, kc, tc * 512:(tc + 1) * 512],
                                kc == 0, kc == KC - 1, [wk] + hkeys[tc * 4:tc * 4 + 4], [bk])
                    self.act(raw[:, 3 + t2 * 512:3 + (t2 + 1) * 512], bank[:, :], AF.Copy, [bk], [rk + f":{t2}"])
                rks = [rk + "h", rk + ":0", rk + ":1"]
                self.act(cacc[:], raw[:, 3:3 + HS], AF.Identity, rks + ["cw", "cb"], [ck],
                         bias=self.cb[:, ch:ch + 1], scale=self.cw[:, ch, 3:4])

            def stage_b(u):
                (wt, wk, coff, ch, dst, dkey, half, ci) = u
                raw, cacc = raws[ci], caccs[ci]
                rk, ck = f"raw{ci}", f"cacc{ci}"
                rks = [rk + "h", rk + ":0", rk + ":1"]
                for j in (2, 1, 0):
                    self.stt("dve", cacc[:], raw[:, j:j + HS], self.cw[:, ch, j:j + 1], cacc[:], ALU.mult, ALU.add,
                             rks + ["cw", ck], [ck])
                self.act(dst[:, half * HS:(half + 1) * HS], cacc[:], AF.Silu, [ck], [dkey])

            for i in range(len(units) + 1):
                if i < len(units):
                    stage_a(units[i])
                if i >= 1:
                    stage_b(units[i - 1])

        def chunk(c, g, gi):
            B = gb[gi]
            st = sets[gi]
            bs = bankset[gi]
            sfx = f"{gi}"
            xT, BT, CT, sz, S32, Sbf = B["xT"], B["BT"], B["CT"], B["sz"], B["S32"], B["Sbf"]
            b_cb, b_R, b_Y, tpb = bs["cb"], bs["R"], bs["Y"], bs["tp"]
            k_cb, k_R, k_Y, tk = bs["kcb"], bs["kR"], bs["kY"], bs["ktp"]
            cols = slice(c * 128, (c + 1) * 128)
            self.tr(tpb[:, 0:128], xT[:, 0, cols], self.identb[:], ["xT0" + sfx, "identb"], [tk])
            self.tr(tpb[:, 128:256], xT[:, 1, cols], self.identb[:], ["xT1" + sfx, "identb"], [tk])
            self.tr(tpb[:, 256:384], BT[:, cols], self.identb[:], ["BT" + sfx, "identb"], [tk])
            self.act(st["xB"][:], tpb[:, 0:384], AF.Copy, [tk], ["xB" + sfx])
            self.mm(b_cb[:, 0:128], BT[:, cols], CT[:, cols], True, True, ["BT" + sfx, "CT" + sfx], [k_cb])
            self.cp("dve", st["adtb"][:], adt[:, c, 4 * g:4 * g + 4].unsqueeze(2).to_broadcast([128, 4, 128]),
                    ["adt"], ["adtb" + sfx])
            self.mm(b_R[:, :], self.identb[:], self.maskb[:], True, False, ["identb", "maskb"], [k_R])
            for j in range(4):
                self.mm(b_R[:, j * 128:(j + 1) * 128], st["adtb"][:, j, :], self.tri[:], False, True,
                        ["adtb" + sfx, "tri"], [k_R])
            for j in range(4):
                self.act(st["E"][:, j, :], b_R[:, j * 128:(j + 1) * 128], AF.Exp, [k_R, "biasL"], [f"E{sfx}:{j}"],
                         bias=biasL[:, c, 4 * g + j:4 * g + j + 1])
            ek = [f"E{sfx}:{j}" for j in range(4)]
            self.tt("dve", st["mixT"][:], b_cb[:, 0:128].unsqueeze(1).to_broadcast([128, 4, 128]), st["E"][:],
                    ALU.mult, [k_cb] + ek, ["mixT" + sfx])
            x4 = st["xB"][:, 0:256].rearrange("p (j d) -> p j d", d=64)
            if c < NT - 1:
                self.tt("dve", st["xds"][:].rearrange("p (j d) -> p j d", d=64), x4,
                        st["E"][:, :, 127:128].to_broadcast([128, 4, 64]), ALU.mult,
                        ["xB" + sfx] + ek, ["xds" + sfx])
            for j in range(4):
                self.mm(b_Y[:, j * 64:(j + 1) * 64], st["mixT"][:, j, :], st["xB"][:, j * 64:(j + 1) * 64],
                        True, False, ["mixT" + sfx, "xB" + sfx], [k_Y])
                self.mm(b_Y[:, j * 64:(j + 1) * 64], self.ddiag[:, 4 * g + j, :], st["xB"][:, j * 64:(j + 1) * 64],
                        False, True, ["ddiag", "xB" + sfx], [k_Y])
            if c > 0:
                self.mm(b_Y[:, 256:512], CT[:, cols], Sbf[:], True, True, ["CT" + sfx, "Sbf" + sfx], [k_Y])
            if c < NT - 1:
                self.mm(b_cb[:, 128:384], st["xB"][:, 256:384], st["xds"][:], True, True,
                        ["xB" + sfx, "xds" + sfx], [k_cb])
            if c > 0:
                self.tt("dve", st["tmp"][:].rearrange("p (j d) -> p j d", d=64),
                        b_Y[:, 256:512].rearrange("p (j d) -> p j d", d=64),
                        sd[:, c, 4 * g:4 * g + 4].unsqueeze(2).to_broadcast([128, 4, 64]), ALU.mult,
                        [k_Y, "sd"], ["tmp" + sfx])
                self.tt("dve", st["ysb"][:], b_Y[:, 0:256], st["tmp"][:], ALU.add, [k_Y, "tmp" + sfx],
                        ["ysb" + sfx])
            else:
                self.cp("dve", st["ysb"][:], b_Y[:, 0:256], [k_Y], ["ysb" + sfx])
            self.tt("pool", st["G"][:], st["ysb"][:], sz[:, c, :], ALU.mult, ["ysb" + sfx, "sz" + sfx], ["G" + sfx])
            self.act(junk[:], st["G"][:], AF.Square, ["G" + sfx, "ssq_ssd"], [f"ssqs:{c}:{g}", "junk2"],
                     accum=ssq[:, c, g:g + 1])
            self.tr(tpb[:, 384:512], st["G"][:, 0:128], self.identb[:], ["G" + sfx, "identb"], [tk])
            self.tr(tpb[:, 512:640], st["G"][:, 128:256], self.identb[:], ["G" + sfx, "identb"], [tk])
            self.act(GT[:, 2 * g, cols], tpb[:, 384:512], AF.Copy, [tk, "ngt"], [f"GT:{c}"],
                     scale=self.ngt[:, 2 * g:2 * g + 1])
            self.ts("dve", GT[:, 2 * g + 1, cols], tpb[:, 512:640], self.ngt[:, 2 * g + 1:2 * g + 2], None, ALU.mult, None,
                    [tk, "ngt"], [f"GT:{c}"])
            if c < NT - 1:
                if c == 0:
                    self.cp("dve", S32[:], b_cb[:, 128:384], [k_cb], ["S32" + sfx])
                else:
                    self.tt("dve", S32[:].rearrange("p (j d) -> p j d", d=64),
                            S32[:].rearrange("p (j d) -> p j d", d=64),
                            cdec[:, c, 4 * g:4 * g + 4].unsqueeze(2).to_broadcast([128, 4, 64]), ALU.mult,
                            ["S32" + sfx, "cdec"], ["S32" + sfx])
                    self.tt("dve", S32[:], b_cb[:, 128:384], S32[:], ALU.add, [k_cb, "S32" + sfx], ["S32" + sfx])
                self.act(Sbf[:], S32[:], AF.Copy, ["S32" + sfx], ["Sbf" + sfx])

        for gp in range(4):
            for gi in range(2):
                inproj(2 * gp + gi, gi)
            S.barrier()
            streams = []
            for gi in range(2):
                self.defer = []
                for c in range(NT):
                    chunk(c, 2 * gp + gi, gi)
                streams.append(self.defer)
                self.defer = None
            self.merge(streams)
            S.barrier()
        if self.stop_after == "ssd":
            S.barrier()
            M.release(scr)
            tmpf = M.alloc([128, 2, SEQ], F32, "dbgg")
            self.cp("dve", tmpf[:], GT[:, 0:2, :], [f"GT:{c}" for c in range(NT)], ["dbgg"])
            self.dbg_dump("GT", tmpf[:], (128, 2, SEQ), ["dbgg"])
            self.dbg_dump("ssq", ssq[:], (128, NT, 8), ["ssq_ssd"])
            return True
        S.barrier()
        M.release(scr)
        self.Mb = M.alloc([128, NT, D], BF16, "Mb")
        self.mb_end = M.mark()
        Mb = self.Mb
        red = M.alloc([128, NT], F32, "red")
        rs = M.alloc([128, NT], F32, "rs_ssd")
        wssd = M.alloc([128, 16, D], BF16, "wssd")
        wgs = M.alloc([128, KC, D], BF16, "wgs")
        sgt1 = M.alloc([128, 512], BF16, "sgt")
        sgt = [sgt1, sgt1]
        self.wload(wssd[:], self.din("w_ssd")[:, :], "wssd")
        self.wload(wgs[:], self.din("w_in")[:, OFF_GS:OFF_GS + D], "wgs")
        self.S.op("dve", lambda e: e.tensor_reduce(out=red[:], in_=ssq[:], axis=AX.X, op=ALU.add),
                  reads=["ssq_ssd"] + [f"ssqs:{c}:{g}" for c in range(NT) for g in range(8)], writes=["red"])
        self.ts("dve", rs[:], red[:], 1.0 / 2048, EPS, ALU.mult, ALU.add, ["red"], ["rs_ssd"])
        self.act(rs[:], rs[:], AF.Ln, ["rs_ssd"], ["rs_ssd"])
        self.act(rs[:], rs[:], AF.Exp, ["rs_ssd"], ["rs_ssd"], scale=-0.5)
        i = 0
        for t in range(NT):
            tcols = slice(t * 128, (t + 1) * 128)
            for half in range(2):
                hc = slice(half * 512, (half + 1) * 512)
                by, bg = pb[i % 2], pb[2 + i % 2]
                ky, kg = f"pb{i % 2}", f"pb{2 + i % 2}"
                for cc in range(16):
                    self.mm(by[:, :], GT[:, cc, tcols], wssd[:, cc, hc], cc == 0, cc == 15, [f"GT:{t}", "wssd"], [ky])
                for kc in range(KC):
                    self.mm(bg[:, :], hT[:, kc, tcols], wgs[:, kc, hc], kc == 0, kc == KC - 1, [hkeys[t], "wgs"], [kg])
                self.act(sgt[i % 2][:], bg[:, :], AF.Sigmoid, [kg], ["sgt"])
                self.stt("dve", Mb[:, t, hc], by[:, :], rs[:, t:t + 1], sgt[i % 2][:], ALU.mult, ALU.mult,
                         [ky, "rs_ssd", "sgt"], [f"Mb:{t}:{half}"])
                i += 1
        if self.stop_after == "ssdtail":
            S.barrier()
            M.release(self.hole)
            tmpf = M.alloc([128, NT, D], F32, "dbgm")
            self.cp("dve", tmpf[:], Mb[:], [f"Mb:{t}:{h}" for t in range(NT) for h in range(2)], ["dbgm"])
            self.dbg_dump("Mb", tmpf[:], (128, NT, D), ["dbgm"])
            return True
        S.barrier()
        M.release(self.mb_end)
        return False

    def phase_att(self, s):
        M, S = self.M, self.S
        hT, Mb, pb = self.hT, self.Mb, self.pb
        hkeys = [f"hT:{t}" for t in range(NT)]
        M.release(self.hole)
        attT = M.alloc([64, 8, SEQ], BF16, "attT")
        acc = M.alloc([65, 2, SEQ], F32, "acc")
        v_sb = M.alloc([128, 16, 2, 80], BF16, "v_sb")
        pexp = [M.alloc([128, 2, 256], BF16, "pexp") for _ in range(2)]
        pm = [M.alloc([128, 2, 256], BF16, "pm") for _ in range(2)]
        assert M.mark() <= self.mb_start
        M.release(self.mb_end)
        ems = [M.alloc([128, 2, 256], BF16, "em") for _ in range(2)]
        qT = [M.alloc([128, SEQ], BF16, "qT") for _ in range(2)]
        kT = [M.alloc([128, SEQ], BF16, "kT") for _ in range(2)]
        vT = [M.alloc([128, SEQ], BF16, "vT") for _ in range(2)]
        accs = [acc, M.alloc([65, 2, SEQ], F32, "acc")]
        v_sb2 = M.alloc([128, 16, 2, 80], BF16, "v_sb")
        vsb = [v_sb, v_sb2]
        wq = [M.alloc([128, KC, 128], BF16, "wq") for _ in range(2)]
        wk = [M.alloc([128, KC, 128], BF16, "wk") for _ in range(2)]
        wv = [M.alloc([128, KC, 128], BF16, "wv") for _ in range(2)]
        self.memset("dve", vsb[0][:], 1.0, ["v_sb0"])
        self.memset("dve", vsb[1][:], 1.0, ["v_sb1"])
        sbanks = (((pb[4], "pb4"), (pb[5], "pb5")), ((pb[0], "pb0"), (pb[1], "pb1")))
        obanks1 = ((pb[6], "pb6"), (pb[3], "pb3"))
        units = [(hp, g) for hp in range(4) for g in range(3)]

        def proj(ui):
            hp, g = units[ui]
            st = ui % 2
            d = ATT_PAT[g][1]
            c0 = g * 512 + hp * 128
            self.wload(wq[st][:], self.din("w_in")[:, OFF_Q + c0:OFF_Q + c0 + 128], f"wq{st}")
            self.wload(wk[st][:], self.din("w_in")[:, OFF_K + c0:OFF_K + c0 + 128], f"wk{st}")
            self.wload(wv[st][:], self.din("w_in")[:, OFF_V + c0:OFF_V + c0 + 128], f"wv{st}")
            self.dma("pool", ems[st][:], self.din("c_em")[:, g * 8 + hp * 2:g * 8 + hp * 2 + 2, :], [], [f"em{st}"], f"em{st}")
            i = 0
            for (wt, wkey, dst, dkey) in ((wq[st], f"wq{st}", qT[st], f"qT{st}"), (wk[st], f"wk{st}", kT[st], f"kT{st}"),
                                          (wv[st], f"wv{st}", vT[st], f"vT{st}")):
                for tc in range(4):
                    bank, bk = pb[2], "pb2"
                    i += 1
                    for kc in range(KC):
                        self.mm(bank[:, :], wt[:, kc, :], hT[:, kc, tc * 512:(tc + 1) * 512], kc == 0, kc == KC - 1,
                                [wkey] + hkeys[tc * 4:tc * 4 + 4], [bk])
                    dv = dst[:, :].rearrange("p (r m) -> p r m", r=d)[:, :, tc * 512 // d:(tc + 1) * 512 // d]
                    sv = bank[:, :].rearrange("p (m r) -> p r m", r=d)
                    self.act(dv, sv, AF.Copy, [bk], [dkey])
            for b4 in range(4):
                for bb in range(4):
                    blk = b4 * 4 + bb
                    self.tr(self.tp[:, bb * 128:(bb + 1) * 128], vT[st][:, blk * 128:(blk + 1) * 128], self.identb[:],
                            [f"vT{st}", "identb"], ["tp"])
                self.act(vsb[st][:, b4 * 4:(b4 + 1) * 4, :, 0:64],
                         self.tp[:, 0:512].rearrange("p (b h d) -> p b h d", b=4, h=2), AF.Copy, ["tp"], [f"v_sb{st}"])

        def blocks(ui, b4, sidx):
            hp, g = units[ui]
            st = ui % 2
            d = ATT_PAT[g][1]
            nb = (SEQ // d) // 128
            acc = accs[hp % 2]
            ap_ = f"{hp % 2}"
            for bb in range(4):
                blk = b4 * 4 + bb
                n = blk % nb
                kts = [0, 1] if n > 0 else [1]
                lo = kts[0] * 128
                for hh in range(2):
                    sbank, sk = sbanks[sidx][hh]
                    hs = slice(64 * hh, 64 * hh + 64)
                    for kt in kts:
                        kc0 = (blk - 1 + kt) * 128
                        self.mm(sbank[:, kt * 128:(kt + 1) * 128],
                                kT[st][hs, kc0:kc0 + 128], qT[st][hs, blk * 128:(blk + 1) * 128], True, True,
                                [f"kT{st}", f"qT{st}"], [sk])
                for hh in range(2):
                    sbank, sk = sbanks[sidx][hh]
                    self.act(pexp[sidx][:, hh, lo:256], sbank[:, lo:256], AF.Exp,
                             [sk], [f"pexp{sidx}:{hh}"], scale=0.125)
                self.tt("dve", pm[sidx][:, :, lo:256], pexp[sidx][:, :, lo:256],
                        ems[st][:, :, lo:256], ALU.mult,
                        [f"pexp{sidx}:0", f"pexp{sidx}:1", f"em{st}"], [f"pm{sidx}"])
                ob, ok_ = obanks1[sidx]
                bbl = blk % 2
                for hh in range(2):
                    co = hh * 256 + bbl * 128
                    for kt in kts:
                        self.mm(ob[0:65, co:co + 128], vsb[st][:, blk - 1 + kt, hh, 0:65],
                                pm[sidx][:, hh, kt * 128:(kt + 1) * 128], kt == kts[0], kt == 1,
                                [f"v_sb{st}", f"pm{sidx}"], [ok_])
                if bbl == 1:
                    b2 = blk // 2
                    for hh in range(2):
                        sv = ob[0:65, hh * 256:(hh + 1) * 256]
                        if g == 0:
                            dv = acc[0:65, hh, b2 * 256:(b2 + 1) * 256]
                        elif g == 1:
                            r_, n0 = b2 // 2, (b2 % 2) * 2
                            a0 = r_ + 512 * n0
                            dv = acc[0:65, hh, a0:a0 + 255 * 4 + 1:4]
                        else:
                            dv = acc[0:65, hh, :].rearrange("p (i r) -> p r i", r=16)[:, 2 * b2:2 * b2 + 2, :]
                            sv = sv.rearrange("p (r i) -> p r i", r=2)
                        if g == 0:
                            self.cp("dve", dv, sv, [ok_], [f"acc{ap_}{hh}:{b2}"])
                        else:
                            aks = [f"acc{ap_}{hh}:{b}" for b in range(8)]
                            self.tt("dve", dv, sv, dv, ALU.add, [ok_] + aks, aks)

        def normalise(hp):
            acc = accs[hp % 2]
            for hh in range(2):
                aks = [f"acc{hp % 2}{hh}:{b}" for b in range(8)]
                self.rec("dve", lambda e, hh=hh: e.reciprocal(out=acc[64:65, hh, :], in_=acc[64:65, hh, :]),
                         reads=aks, writes=aks, cost=2.3)
                for tc in range(4):
                    tcs = slice(tc * 512, (tc + 1) * 512)
                    self.mm(pb[6][0:64, :], self.onesf[64:65, 0:64], acc[64:65, hh, tcs], True, True,
                            ["onesf"] + aks, ["pb6"])
                    self.tt("dve", attT[0:64, 2 * hp + hh, tcs], pb[6][0:64, :], acc[0:64, hh, tcs], ALU.mult,
                            ["pb6"] + aks, ["attT"])

        proj(0)
        for ui in range(len(units)):
            hp, g = units[ui]
            streams = []
            for sidx in range(2):
                self.defer = []
                if sidx == 0 and g == 0 and hp > 0:
                    normalise(hp - 1)
                for b4 in (sidx, sidx + 2):
                    blocks(ui, b4, sidx)
                streams.append(self.defer)
            if ui + 1 < len(units):
                self.defer = []
                proj(ui + 1)
                streams.append(self.defer)
            self.defer = None
            self.merge(streams)
        normalise(3)
        if self.stop_after == "att":
            S.barrier()
            M.release(self.mb_end)
            tmpf = M.alloc([64, 4, SEQ], F32, "dbga")
            self.cp("dve", tmpf[:], attT[:, 0:4, :], ["attT"], ["dbga"])
            self.dbg_dump("attT", tmpf[:], (64, 4, SEQ), ["dbga"])
            return True
        S.barrier()
        M.release(self.mb_end)
        watt = M.alloc([64, 8, D], BF16, "watt")
        wga = M.alloc([128, KC, D], BF16, "wga")
        sgt = [M.alloc([128, 512], F32, "sgt") for _ in range(2)]
        tmp = [M.alloc([128, 512], F32, "atmp") for _ in range(2)]
        self.dma("pool", watt[:], self.din("w_att").rearrange("(h d) n -> d h n", d=64), [], ["watt"], "watt")
        self.wload(wga[:], self.din("w_in")[:, OFF_GA:OFF_GA + D], "wga")
        i = 0
        for t in range(NT):
            tcols = slice(t * 128, (t + 1) * 128)
            for half in range(2):
                hc = slice(half * 512, (half + 1) * 512)
                by, bg = pb[i % 2], pb[2 + i % 2]
                ky, kg = f"pb{i % 2}", f"pb{2 + i % 2}"
                for h in range(8):
                    self.mm(by[:, :], attT[0:64, h, tcols], watt[0:64, h, hc], h == 0, h == 7, ["attT", "watt"], [ky])
                for kc in range(KC):
                    self.mm(bg[:, :], hT[:, kc, tcols], wga[:, kc, hc], kc == 0, kc == KC - 1, [hkeys[t], "wga"], [kg])
                self.act(sgt[i % 2][:], bg[:, :], AF.Sigmoid, [kg], [f"sgt{i % 2}"])
                self.tt("dve", tmp[i % 2][:], by[:, :], sgt[i % 2][:], ALU.mult, [ky, f"sgt{i % 2}"], [f"atmp{i % 2}"])
                self.tt("pool", Mb[:, t, hc], Mb[:, t, hc], tmp[i % 2][:], ALU.add, [f"Mb:{t}:{half}", f"atmp{i % 2}"],
                        [f"Mb:{t}:{half}"])
                i += 1
        if self.stop_after == "merged":
            S.barrier()
            M.release(self.hole)
            tmpf = M.alloc([128, NT, D], F32, "dbgm")
            self.cp("dve", tmpf[:], Mb[:], [f"Mb:{t}:{h}" for t in range(NT) for h in range(2)], ["dbgm"])
            self.dbg_dump("Mb", tmpf[:], (128, NT, D), ["dbgm"])
            return True
        S.barrier()
        M.release(self.mb_end)
        return False

    def phase_out(self, s):
        M, S = self.M, self.S
        Mb, pb = self.Mb, self.pb
        M.release(self.hole)
        self.x1 = M.alloc([128, NT, D], F32, "x1")
        assert M.mark() <= self.mb_start
        M.release(self.mb_end)
        x1 = self.x1
        wout = M.alloc([128, KC, D], BF16, "wout")
        mT = [M.alloc([128, KC, 128], BF16, "mT") for _ in range(2)]
        self.wload(wout[:], self.din("w_out")[:, :], "wout")
        for t in range(NT):
            self.dma("sp", x1[:, t, :], self.din("x")[s, t * 128:(t + 1) * 128, :], [], [f"x1:{t}:0", f"x1:{t}:1"], f"xr{t % 4}")
        i = 0
        for t in range(NT):
            par = t % 2
            for kc in range(KC):
                self.tr(self.tp[:, kc * 128:(kc + 1) * 128], Mb[:, t, kc * 128:(kc + 1) * 128], self.identb[:],
                        [f"Mb:{t}:{kc // 4}", "identb"], ["tp"])
            self.act(mT[par][:], self.tp[:, :].rearrange("p (c n) -> p c n", c=KC), AF.Copy, ["tp"], [f"mT{par}"])
            for half in range(2):
                hc = slice(half * 512, (half + 1) * 512)
                bank, bk = pb[i % 4], f"pb{i % 4}"
                for kc in range(KC):
                    self.mm(bank[:, :], mT[par][:, kc, :], wout[:, kc, hc], kc == 0, kc == KC - 1, [f"mT{par}", "wout"], [bk])
                self.tt("dve", x1[:, t, hc], bank[:, :], x1[:, t, hc], ALU.add, [bk, f"x1:{t}:{half}"], [f"x1:{t}:{half}"])
                i += 1
        if self.stop_after == "x1":
            self.dbg_dump("x1", x1[:], (128, NT, D), [f"x1:{t}:{h}" for t in range(NT) for h in range(2)])
            return True
        S.barrier()
        return False

    def phase_moe(self, s):
        M, S = self.M, self.S
        pb, x1, hT = self.pb, self.x1, self.hT
        xk = lambda t: f"x1:{t}:0"
        M.release(self.mb_start)
        wset = [dict(g=M.alloc([128, KC, 512], BF16, "wg"), u=M.alloc([128, KC, 512], BF16, "wu"),
                     d=M.alloc([128, 4, D], BF16, "wd"))]
        assert M.mark() <= self.mb_end
        M.release(self.mb_end)
        comb = M.alloc([128, NT, 16], F32, "comb")
        mk = M.mark()
        ssq = M.alloc([128, NT], F32, "ssq")
        rs = M.alloc([128, NT], F32, "rs")
        junk = M.alloc([128, D], BF16, "junk")
        hnf = M.alloc([128, D], F32, "hnf")
        hi = [M.alloc([128, D], BF16, "hi") for _ in range(2)]
        lo = [M.alloc([128, D], BF16, "lo") for _ in range(2)]
        loT = [M.alloc([128, KC, 128], BF16, "loT") for _ in range(2)]
        lg = M.alloc([128, NT, 20], F32, "lg")
        sm = M.alloc([128, NT, 64], F32, "smalls")
        self.load_gain(1)
        self.memset("dve", ssq[:], 0.0, ["ssq"])
        for t in range(NT):
            self.act(junk[:], x1[:, t, :], AF.Square, [xk(t), f"x1:{t}:1", "ssq"], [f"ssq:{t}", "junk"], accum=ssq[:, t:t + 1])
        self.ts("dve", rs[:], ssq[:], 1.0 / D, EPS, ALU.mult, ALU.add, [f"ssq:{t}" for t in range(NT)], ["rs"])
        self.act(rs[:], rs[:], AF.Ln, ["rs"], ["rs"])
        self.act(rs[:], rs[:], AF.Exp, ["rs"], ["rs"], scale=-0.5)
        for t in range(NT):
            par = t % 2
            tcols = slice(t * 128, (t + 1) * 128)
            self.stt("dve", hnf[:], x1[:, t, :], rs[:, t:t + 1], self.gbc[:], ALU.mult, ALU.mult,
                     [xk(t), f"x1:{t}:1", "rs", "gbc"], ["hnf"])
            self.cp("dve", hi[par][:], hnf[:], ["hnf"], [f"hi{par}"])
            self.tt("dve", lo[par][:], hnf[:], hi[par][:], ALU.subtract, ["hnf", f"hi{par}"], [f"lo{par}"])
            for c in range(KC):
                self.tr(self.tp[:, c * 128:(c + 1) * 128], hi[par][:, c * 128:(c + 1) * 128], self.identb[:],
                        [f"hi{par}", "identb"], ["tp"])
            self.act(hT[:, :, tcols], self.tp[:, :].rearrange("p (c n) -> p c n", c=KC), AF.Copy, ["tp"], [f"hT:{t}"])
            for c in range(KC):
                self.tr(self.tp[:, c * 128:(c + 1) * 128], lo[par][:, c * 128:(c + 1) * 128], self.identb[:],
                        [f"lo{par}", "identb"], ["tp"])
            self.act(loT[par][:], self.tp[:, :].rearrange("p (c n) -> p c n", c=KC), AF.Copy, ["tp"], [f"loT{par}"])
            n = 0
            for kc in range(KC):
                for (a, ak, w, wk_) in ((hT[:, kc, tcols], f"hT:{t}", self.wrhi, "wrhi"),
                                        (hT[:, kc, tcols], f"hT:{t}", self.wrlo, "wrlo"),
                                        (loT[par][:, kc, :], f"loT{par}", self.wrhi, "wrhi")):
                    self.mm(pb[6][:, 0:20], a, w[:, kc, :], n == 0, n == 3 * KC - 1, [ak, wk_], ["pb6"])
                    n += 1
            self.tt("dve", lg[:, t, :], pb[6][:, 0:20], self.brb[:], ALU.add, ["pb6", "brb"], [f"lg:{t}"])
        self.route(lg, sm, comb)
        if self.stop_after == "route":
            self.dbg_dump("comb", comb[:], (128, NT, 16), [f"comb:{t}" for t in range(NT)])
            return True
        S.barrier()
        M.release(mk)
        wset.append(dict(g=M.alloc([128, KC, 512], BF16, "wg"), u=M.alloc([128, KC, 512], BF16, "wu"),
                         d=M.alloc([128, 4, D], BF16, "wd")))
        hid = [M.alloc([128, 4, 512], BF16, "hid") for _ in range(2)]
        sg = [M.alloc([128, 512], BF16, "sg") for _ in range(2)]
        units = [(e, tc) for e in range(NEXP) for tc in range(4)]
        cnt = {"d": 0}

        def stage_gu(u, ui):
            e, tc = u
            ws = wset[e % 2]
            sfx = f"{e % 2}"
            if tc == 0:
                self.wload(ws["g"][:], self.din("w_eg")[e], "wg" + sfx)
                self.wload(ws["u"][:], self.din("w_eu")[e], "wu" + sfx)
                self.wload(ws["d"][:], self.din("w_ed")[e], "wd" + sfx)
            hp_ = ui % 2
            tcs = slice(tc * 512, (tc + 1) * 512)
            hk = [f"hT:{t}" for t in range(tc * 4, tc * 4 + 4)]
            for fc in range(4):
                bg, bu = pb[(2 * fc) % 4], pb[(2 * fc + 1) % 4]
                kg, ku = f"pb{(2 * fc) % 4}", f"pb{(2 * fc + 1) % 4}"
                for kc in range(KC):
                    self.mm(bg[:, :], ws["g"][:, kc, fc * 128:(fc + 1) * 128], hT[:, kc, tcs], kc == 0, kc == KC - 1,
                            ["wg" + sfx] + hk, [kg])
                for kc in range(KC):
                    self.mm(bu[:, :], ws["u"][:, kc, fc * 128:(fc + 1) * 128], hT[:, kc, tcs], kc == 0, kc == KC - 1,
                            ["wu" + sfx] + hk, [ku])
                self.act(sg[fc % 2][:], bg[:, :], AF.Silu, [kg], [f"sg{fc % 2}"])
                self.tt("dve", hid[hp_][:, fc, :], bu[:, :], sg[fc % 2][:], ALU.mult, [ku, f"sg{fc % 2}"],
                        [f"hid{hp_}:{fc}"])

        def stage_d(u, ui):
            e, tc = u
            ws = wset[e % 2]
            sfx = f"{e % 2}"
            hp_ = ui % 2
            for tt_ in range(4):
                t = tc * 4 + tt_
                for half in range(2):
                    hc = slice(half * 512, (half + 1) * 512)
                    i = cnt["d"]
                    cnt["d"] += 1
                    bank, bk = pb[4 + i % 3], f"pb{4 + i % 3}"
                    for fc in range(4):
                        self.mm(bank[:, :], hid[hp_][:, fc, tt_ * 128:(tt_ + 1) * 128], ws["d"][:, fc, hc], fc == 0, fc == 3,
                                [f"hid{hp_}:{fc}", "wd" + sfx], [bk])
                    self.stt("dve", x1[:, t, hc], bank[:, :], comb[:, t, e:e + 1], x1[:, t, hc], ALU.mult, ALU.add,
                             [bk, f"comb:{t}", f"x1:{t}:{half}"], [f"x1:{t}:{half}"])

        for i in range(len(units) + 1):
            if i < len(units):
                stage_gu(units[i], i)
            if i >= 1:
                stage_d(units[i - 1], i - 1)
        if self.stop_after == "x2":
            self.dbg_dump("x2", x1[:], (128, NT, D), [f"x1:{t}:{h}" for t in range(NT) for h in range(2)])
            return True
        S.barrier()
        return False

    def route(self, lg, sm, comb):
        T = NT
        k = lambda n: f"sm:{n}"
        lgk = [f"lg:{t}" for t in range(T)]
        gl = lg[:, :, 0:4]
        el4 = lg[:, :, 4:20].rearrange("p t (g e) -> p t g e", g=4)
        col = lambda a, b: sm[:, :, a:b]
        gmax, gsum, gval, m1, m2, d12, w1, w2 = (sm[:, :, i] for i in range(8))
        ohg, ge, within, mask1 = col(8, 12), col(12, 16), col(16, 20), col(20, 24)
        w2in, mask2, ew, ewg = col(24, 28), col(28, 32), col(32, 36), col(36, 40)
        tmp16 = col(40, 56)
        b3 = lambda v: v.unsqueeze(2).to_broadcast([128, T, 4])
        op = self.S.op
        op("dve", lambda e: e.reduce_max(out=gmax, in_=gl, axis=AX.X), reads=lgk, writes=[k("gmax")])
        self.tt("dve", ohg, gl, b3(gmax), ALU.is_equal, lgk + [k("gmax")], [k("ohg")])
        self.tt("dve", ge, gl, b3(gmax), ALU.subtract, lgk + [k("gmax")], [k("ge")])
        self.act(ge, ge, AF.Exp, [k("ge")], [k("ge")])
        op("dve", lambda e: e.reduce_sum(out=gsum, in_=ge, axis=AX.X), reads=[k("ge")], writes=[k("gsum")])
        op("dve", lambda e: e.reciprocal(out=gval, in_=gsum), reads=[k("gsum")], writes=[k("gval")])
        t4 = tmp16.rearrange("p t (g e) -> p t g e", g=4)
        self.tt("dve", t4, el4, ohg.unsqueeze(3).to_broadcast([128, T, 4, 4]), ALU.mult, lgk + [k("ohg")], [k("tmp16")])
        op("dve", lambda e: e.tensor_reduce(out=within, in_=tmp16.rearrange("p t (g e) -> p t e g", g=4), axis=AX.X,
                                            op=ALU.add), reads=[k("tmp16")], writes=[k("within")])
        op("dve", lambda e: e.reduce_max(out=m1, in_=within, axis=AX.X), reads=[k("within")], writes=[k("m1")])
        self.tt("dve", mask1, within, b3(m1), ALU.is_equal, [k("within"), k("m1")], [k("mask1")])
        self.stt("dve", w2in, mask1, -1e30, within, ALU.mult, ALU.add, [k("mask1"), k("within")], [k("w2in")])
        op("dve", lambda e: e.reduce_max(out=m2, in_=w2in, axis=AX.X), reads=[k("w2in")], writes=[k("m2")])
        self.tt("dve", mask2, w2in, b3(m2), ALU.is_equal, [k("w2in"), k("m2")], [k("mask2")])
        self.tt("dve", d12, m2, m1, ALU.subtract, [k("m1"), k("m2")], [k("d12")])
        self.act(d12, d12, AF.Exp, [k("d12")], [k("d12")])
        self.ts("dve", d12, d12, 1.0, None, ALU.add, None, [k("d12")], [k("d12")])
        op("dve", lambda e: e.reciprocal(out=w1, in_=d12), reads=[k("d12")], writes=[k("w1")])
        self.ts("dve", w2, w1, -1.0, 1.0, ALU.mult, ALU.add, [k("w1")], [k("w2")])
        self.tt("dve", ew, mask1, b3(w1), ALU.mult, [k("mask1"), k("w1")], [k("ew")])
        self.tt("dve", ewg, mask2, b3(w2), ALU.mult, [k("mask2"), k("w2")], [k("ewg")])
        self.tt("dve", ew, ew, ewg, ALU.add, [k("ew"), k("ewg")], [k("ew")])
        self.tt("dve", ewg, ew, b3(gval), ALU.mult, [k("ew"), k("gval")], [k("ewg")])
        self.tt("dve", comb[:, :, :].rearrange("p t (g e) -> p t g e", g=4),
                ohg.unsqueeze(3).to_broadcast([128, T, 4, 4]), ewg.unsqueeze(2).to_broadcast([128, T, 4, 4]),
                ALU.mult, [k("ohg"), k("ewg")], [f"comb:{t}" for t in range(T)])

    def phase_ple(self, s):
        M, S = self.M, self.S
        pb, x1, hT = self.pb, self.x1, self.hT
        M.release(self.mb_start)
        xkeys = lambda t: f"x1:{t}:0"
        ssq = M.alloc([128, NT], F32, "ssq")
        rs = M.alloc([128, NT], F32, "rs")
        junk = M.alloc([128, D], BF16, "junk")
        hn = [M.alloc([128, D], BF16, "hn") for _ in range(2)]
        wpg = M.alloc([128, KC, D], BF16, "wpg")
        wpp = M.alloc([128, 2, D], BF16, "wpp")
        pT = M.alloc([128, 2, SEQ], BF16, "pT")
        ptile = [M.alloc([128, PLE], BF16, "ptile") for _ in range(2)]
        sgt = [M.alloc([128, 512], F32, "sgt") for _ in range(2)]
        tmp = [M.alloc([128, 512], F32, "ptmp") for _ in range(2)]
        ot = [M.alloc([128, D], F32, "ot") for _ in range(2)]
        self.wload(wpg[:], self.din("w_pg")[:, :], "wpg")
        self.wload(wpp[:], self.din("w_pp")[:, :], "wpp")
        self.load_gain(2)
        self.memset("dve", ssq[:], 0.0, ["ssq"])
        for t in range(NT):
            self.act(junk[:], x1[:, t, :], AF.Square, [xkeys(t), f"x1:{t}:1", "ssq"], [f"ssq:{t}", "junk"], accum=ssq[:, t:t + 1])
        self.ts("dve", rs[:], ssq[:], 1.0 / D, EPS, ALU.mult, ALU.add, [f"ssq:{t}" for t in range(NT)], ["rs"])
        self.act(rs[:], rs[:], AF.Ln, ["rs"], ["rs"])
        self.act(rs[:], rs[:], AF.Exp, ["rs"], ["rs"], scale=-0.5)
        for t in range(NT):
            par = t % 2
            tcols = slice(t * 128, (t + 1) * 128)
            self.stt("dve", hn[par][:], x1[:, t, :], rs[:, t:t + 1], self.gbc[:], ALU.mult, ALU.mult,
                     [xkeys(t), f"x1:{t}:1", "rs", "gbc"], [f"hn{par}"])
            for c in range(KC):
                self.tr(self.tp[:, c * 128:(c + 1) * 128], hn[par][:, c * 128:(c + 1) * 128], self.identb[:],
                        [f"hn{par}", "identb"], ["tp"])
            self.act(hT[:, :, tcols], self.tp[:, :].rearrange("p (c n) -> p c n", c=KC), AF.Copy, ["tp"], [f"hT:{t}"])
            self.dma("pool", ptile[par][:], self.din("p")[s, tcols, :], [], [f"ptile{par}"], f"ptile{par}")
            for c in range(2):
                self.tr(self.tp[:, c * 128:(c + 1) * 128], ptile[par][:, c * 128:(c + 1) * 128], self.identb[:],
                        [f"ptile{par}", "identb"], ["tp"])
            self.act(pT[:, :, tcols], self.tp[:, 0:256].rearrange("p (c n) -> p c n", c=2), AF.Copy, ["tp"], [f"pT:{t}"])
        S.barrier()
        self.load_gain(3)
        ssq2 = M.alloc([128, NT], F32, "ssq2")
        self.memset("dve", ssq2[:], 0.0, ["ssq2"])
        i = 0
        for t in range(NT):
            tcols = slice(t * 128, (t + 1) * 128)
            for half in range(2):
                hc = slice(half * 512, (half + 1) * 512)
                bg, bp = pb[i % 2], pb[2 + i % 2]
                kg, kp = f"pb{i % 2}", f"pb{2 + i % 2}"
                for kc in range(KC):
                    self.mm(bg[:, :], hT[:, kc, tcols], wpg[:, kc, hc], kc == 0, kc == KC - 1, [f"hT:{t}", "wpg"], [kg])
                for c in range(2):
                    self.mm(bp[:, :], pT[:, c, tcols], wpp[:, c, hc], c == 0, c == 1, [f"pT:{t}", "wpp"], [kp])
                self.act(sgt[i % 2][:], bg[:, :], AF.Sigmoid, [kg], [f"sgt{i % 2}"])
                self.tt("dve", tmp[i % 2][:], bp[:, :], sgt[i % 2][:], ALU.mult, [kp, f"sgt{i % 2}"], [f"ptmp{i % 2}"])
                self.tt("pool", x1[:, t, hc], x1[:, t, hc], tmp[i % 2][:], ALU.add, [f"x1:{t}:{half}", f"ptmp{i % 2}"],
                        [f"x1:{t}:{half}"])
                i += 1
            par = t % 2
            self.act(junk[:], x1[:, t, :], AF.Square, [f"x1:{t}:0", f"x1:{t}:1", "ssq2"], [f"ssq2:{t}", "junk"], accum=ssq2[:, t:t + 1])
            self.ts("dve", ssq2[:, t:t + 1], ssq2[:, t:t + 1], 1.0 / D, EPS, ALU.mult, ALU.add, [f"ssq2:{t}"], [f"ssq2:{t}"])
            self.act(ssq2[:, t:t + 1], ssq2[:, t:t + 1], AF.Ln, [f"ssq2:{t}"], [f"ssq2:{t}"])
            self.act(ssq2[:, t:t + 1], ssq2[:, t:t + 1], AF.Exp, [f"ssq2:{t}"], [f"ssq2:{t}"], scale=-0.5)
            self.stt("dve", ot[par][:], x1[:, t, :], ssq2[:, t:t + 1], self.gbc[:], ALU.mult, ALU.mult,
                     [f"x1:{t}:0", f"x1:{t}:1", f"ssq2:{t}", "gbc"], [f"ot{par}"])
            self.dma("sp", self.out_d[s, tcols, :], ot[par][:], [f"ot{par}"], [], f"ost{par}")
        S.barrier()
        return False


def _consts():
    ident = np.eye(128, dtype=np.float32)
    tri = np.triu(np.ones((128, 128), np.float32))
    j = np.arange(128)[:, None]
    i = np.arange(128)[None, :]
    m = np.where(j > i, -30000.0, 0.0).astype(np.float32)
    mask = np.tile(m, (1, 4))
    hh = np.arange(1, 25, dtype=np.float64)
    slopes = np.exp2(-8.0 * hh / 24.0)
    em = np.zeros((128, 24, 256), np.float64)
    for g, (win, dil) in enumerate(ATT_PAT):
        for h in range(8):
            sl = slopes[g * 8 + h] * dil
            prev = np.where(j >= i, np.exp(-sl * (128 + i - j)), 0.0)
            cur = np.where(i >= j, np.exp(-sl * (i - j)), 0.0)
            em[:, g * 8 + h, 0:128] = prev
            em[:, g * 8 + h, 128:256] = cur
    return ident, tri, mask, em.astype(np.float32)


def _prep_inputs(inputs, nseq, ncores):
    f = lambda a: np.ascontiguousarray(np.asarray(a, dtype=np.float32))
    ident, tri, mask, em = _consts()
    shared = {
        "w_in": f(inputs["w_in"][0]),
        "w_att": f(inputs["w_att_branch"][0]),
        "w_ssd": f(inputs["w_ssd_branch"][0]),
        "w_out": f(inputs["w_out"][0]),
        "w_eg": f(inputs["w_exp_gate"][0]),
        "w_eu": f(inputs["w_exp_up"][0]),
        "w_ed": f(inputs["w_exp_down"][0]),
        "w_pg": f(inputs["w_ple_gate"][0]),
        "w_pp": f(inputs["w_ple_proj"][0]),
        "w_r": f(np.concatenate([inputs["w_router_group"][0], inputs["w_router_expert"][0]], axis=1)),
        "gains": f(np.stack([inputs["norm_mix_g"][0], inputs["norm_ffn_g"][0], inputs["norm_ple_g"][0],
                             inputs["final_norm_g"]])),
        "vec32": f(np.stack([inputs["dt_bias"][0], inputs["a_log"][0], inputs["d_skip"][0]])),
        "b_r": f(np.concatenate([inputs["b_router_group"][0], inputs["b_router_expert"][0]])[None, :]),
        "convw": f(inputs["conv_w"][0].reshape(4, 32, 128).transpose(2, 1, 0)),
        "convb": f(inputs["conv_b"][0].reshape(32, 128).T),
        "ng": f(inputs["ssd_norm_g"][0].reshape(16, 128).T),
        "c_ident": ident, "c_tri": tri, "c_mask": mask, "c_em": em,
    }
    x = np.asarray(inputs["x"], np.float32)
    p = np.asarray(inputs["p"], np.float32)[0]
    maps = []
    for c in range(ncores):
        m = dict(shared)
        m["x"] = np.ascontiguousarray(x[c * nseq:(c + 1) * nseq])
        m["p"] = np.ascontiguousarray(p[c * nseq:(c + 1) * nseq])
        maps.append(m)
    return maps


def kernel(**inputs):
    nseq = 2
    prog = Prog(nseq)
    nc = prog.build()
    maps = _prep_inputs(inputs, nseq, NCORES)
    maps = [{k: v for k, v in m.items() if k in prog._din} for m in maps]
    res = run_bass_kernel_spmd(nc, maps, core_ids=list(range(NCORES)))
    out = np.concatenate([r["out"] for r in res.results], axis=0)
    return out.astype(np.float32)
```

```python
import numpy as np
from contextlib import ExitStack
import concourse.bass as bass
import concourse.mybir as mybir
from concourse.bass_utils import run_bass_kernel_spmd

F32 = mybir.dt.float32
BF16 = mybir.dt.bfloat16
AF = mybir.ActivationFunctionType
ALU = mybir.AluOpType
AX = mybir.AxisListType

NCORES = 8
D = 1024
SEQ = 2048
NT = 16
KC = 8
PLE = 256
EPS = 1e-6
IN_COLS = 12832
OFF_Q, OFF_K, OFF_V, OFF_Z, OFF_X, OFF_B, OFF_C, OFF_DT, OFF_GA, OFF_GS = (
    0, 1536, 3072, 4608, 6656, 8704, 9728, 10752, 10784, 11808)
ATT_PAT = ((128, 1), (512, 4), (2048, 16))
NEXP = 16

ENGS = ("pe", "act", "dve", "pool", "sp")


class _Op:
    __slots__ = ("eng", "fn", "deps", "is_dma", "lane", "sig", "sigval", "fin")


class Sched:
    def __init__(self, nc, es, sem_rot=30000):
        self.nc, self.es = nc, es
        self.ops = {e: [] for e in ENGS}
        self.all_ops = []
        self.last_w = {}
        self.readers = {}
        self.lanes = {}
        self.sem_rot = sem_rot
        self.nsem = 0

    def op(self, eng, fn, reads=(), writes=(), dma=False, lane=None):
        o = _Op()
        o.eng, o.fn, o.is_dma, o.lane = eng, fn, dma, lane
        o.sig, o.sigval = False, None
        o.fin = 0.0
        def _bank(k):
            if k.startswith("pb"):
                return k[:3]
            if k.startswith("tp"):
                return "tp"
            return None
        banks = [b for b in (_bank(k) for k in list(reads) + list(writes)) if b is not None]
        reads = [k for k in reads if _bank(k) is None]
        writes = [k for k in writes if _bank(k) is None] + sorted(set(banks))
        deps = []
        for k in reads:
            w = self.last_w.get(k)
            if w is not None:
                deps.append(w)
        for k in writes:
            w = self.last_w.get(k)
            if w is not None:
                deps.append(w)
            deps.extend(self.readers.get(k, ()))
        seen = set()
        dl = []
        for d in deps:
            if id(d) in seen or d is o:
                continue
            seen.add(id(d))
            if (not d.is_dma) and (not dma) and d.eng == "pe" and eng == "pe":
                continue
            dl.append((d, self.lanes[d.lane]["count"] if d.is_dma else None))
        o.deps = dl
        if dma:
            L = self.lanes.setdefault(lane, {"count": 0, "sem": None})
            L["count"] += 16
        for k in writes:
            self.last_w[k] = o
            self.readers[k] = []
        for k in reads:
            if k not in writes:
                self.readers.setdefault(k, []).append(o)
        self.ops[eng].append(o)
        self.all_ops.append(o)
        return o

    def peek_fin(self, reads, writes):
        def _bank(k):
            if k.startswith("pb"):
                return k[:3]
            if k.startswith("tp"):
                return "tp"
            return None
        t = 0.0
        for k in list(reads) + list(writes):
            b = _bank(k)
            kk = b if b is not None else k
            w = self.last_w.get(kk)
            if w is not None and w.fin > t:
                t = w.fin
            if b is not None or k in writes:
                for r in self.readers.get(kk, ()):
                    if r.fin > t:
                        t = r.fin
        return t

    def barrier(self):
        lasts = []
        for e in ENGS:
            for o in reversed(self.ops[e]):
                if not o.is_dma and o.fn is not None:
                    lasts.append(o)
                    break
        lane_last = {}
        for o in self.all_ops:
            if o.is_dma:
                lane_last[o.lane] = o
        lasts.extend(lane_last.values())
        for e in ENGS:
            o = _Op()
            o.eng, o.fn, o.is_dma, o.lane = e, None, False, None
            o.sig, o.sigval = False, None
            o.fin = 0.0
            o.deps = [(d, self.lanes[d.lane]["count"] if d.is_dma else None) for d in lasts
                      if not (d.eng == "pe" and e == "pe" and not d.is_dma)]
            self.ops[e].append(o)
            self.all_ops.append(o)
        self.last_w = {}
        self.readers = {}

    def emit(self):
        nc, es = self.nc, self.es
        for o in self.all_ops:
            for d, _v in o.deps:
                if not d.is_dma:
                    d.sig = True
        for e in ENGS:
            cnt, sem = 0, None
            for o in self.ops[e]:
                if o.is_dma or not o.sig:
                    continue
                if sem is None or cnt >= self.sem_rot:
                    sem = es.enter_context(nc.semaphore(f"s_{e}_{self.nsem}"))
                    self.nsem += 1
                    cnt = 0
                cnt += 1
                o.sigval = (sem, cnt)
        for name, L in self.lanes.items():
            L["sem"] = es.enter_context(nc.semaphore(f"l_{self.nsem}"))
            self.nsem += 1
        block = es.enter_context(nc.Block())
        lanes = self.lanes

        def make(e):
            ops = self.ops[e]

            def body(eng):
                waited = {}
                for o in ops:
                    need = {}
                    for d, dv in o.deps:
                        if d.is_dma:
                            sem, val = lanes[d.lane]["sem"], dv
                        else:
                            sem, val = d.sigval
                        k = id(sem)
                        if k not in need or need[k][1] < val:
                            need[k] = (sem, val)
                    for k, (sem, val) in need.items():
                        if waited.get(k, 0) >= val:
                            continue
                        eng.wait_ge(sem, val)
                        waited[k] = val
                    if o.fn is None:
                        continue
                    ins = o.fn(eng)
                    if o.is_dma:
                        ins.then_inc(lanes[o.lane]["sem"], 16)
                    elif o.sig:
                        ins.then_inc(o.sigval[0], 1)
            return body

        block.tensor(make("pe"))
        block.scalar(make("act"))
        block.vector(make("dve"))
        block.gpsimd(make("pool"))
        block.sync(make("sp"))


class Mem:
    BASE = 16512
    TOP = 229344

    def __init__(self, nc):
        self.nc = nc
        self.off = self.BASE
        self.n = 0
        self.peak = 0

    def alloc(self, shape, dt, name=None):
        nbytes = int(np.prod(shape[1:])) * (4 if dt == F32 else 2)
        self.off = (self.off + 63) // 64 * 64
        off = self.off
        self.off += nbytes
        self.peak = max(self.peak, self.off)
        assert self.off <= self.TOP, f"SBUF overflow: {self.off} > {self.TOP} ({name})"
        self.n += 1
        return self.nc.alloc_sbuf_tensor_at(f"{name or 't'}{self.n}", list(shape), dt, offset=off)

    def mark(self):
        return self.off

    def release(self, m):
        self.off = m


class Prog:
    def __init__(self, nseq, stop_after=None, dbg=False, skip=()):
        self.skip = skip
        self.defer = None
        self.nseq = nseq
        self.stop_after = stop_after
        self.dbg = dbg

    def din(self, name):
        if name not in self._din:
            self._din[name] = self.nc.dram_tensor(name, list(self._dshapes[name]), F32, kind="ExternalInput").ap()
        return self._din[name]

    def rec(self, eng, fn, reads=(), writes=(), cost=0.2, dma=False, lane=None):
        if self.defer is not None and not dma:
            self.defer.append((eng, fn, list(reads), list(writes), cost))
            return
        self.S.op(eng, fn, reads=reads, writes=writes, dma=dma, lane=lane)

    @staticmethod
    def _n(ap):
        n = 1
        for d in ap.shape[1:]:
            n *= int(d)
        return n

    def _ecost(self, eng, out):
        n = self._n(out)
        if eng == "act":
            return n / 1200.0 + 0.25
        if eng == "pool":
            return n / 480.0 + 0.25
        return n / 960.0 + 0.12

    def merge(self, streams):
        S = self.S
        free = {e: 0.0 for e in ENGS}
        idx = [0] * len(streams)
        total = sum(len(st) for st in streams)
        for _ in range(total):
            best, bt = None, None
            for si, st in enumerate(streams):
                if idx[si] >= len(st):
                    continue
                eng, fn, r, w, cost = st[idx[si]]
                t = max(free[eng], S.peek_fin(r, w))
                if bt is None or t < bt - 1e-9:
                    best, bt = si, t
            eng, fn, r, w, cost = streams[best][idx[best]]
            idx[best] += 1
            o = S.op(eng, fn, reads=r, writes=w)
            o.fin = bt + cost
            free[eng] = o.fin
        for st in streams:
            pass
        for o in S.all_ops:
            o.fin = 0.0

    def mm(self, out, lhsT, rhs, start, stop, r, w):
        cost = self._n(rhs) * (4 if lhsT.dtype == F32 else 1) / 2400.0 + 0.03
        self.rec("pe", lambda e: e.matmul(out, lhsT=lhsT, rhs=rhs, start=start, stop=stop,
                                          skip_group_check=True), reads=r, writes=w, cost=cost)

    def tr(self, out, in_, ident, r, w):
        self.rec("pe", lambda e: e.transpose(out=out, in_=in_, identity=ident), reads=r, writes=w, cost=0.11)

    def act(self, out, in_, func, r, w, bias=None, scale=None, accum=None, eng="act"):
        kw = {}
        if bias is not None:
            kw["bias"] = bias
        if scale is not None:
            kw["scale"] = scale
        if accum is not None:
            kw["accum_out"] = accum
        self.rec("act", lambda e: e.activation(out=out, in_=in_, func=func, **kw), reads=r, writes=w,
                 cost=self._ecost("act", out))

    def tt(self, eng, out, in0, in1, op, r, w):
        self.rec(eng, lambda e: e.tensor_tensor(out=out, in0=in0, in1=in1, op=op), reads=r, writes=w,
                 cost=self._ecost(eng, out))

    def ts(self, eng, out, in0, s1, s2, op0, op1, r, w):
        if op1 is None:
            self.rec(eng, lambda e: e.tensor_scalar(out=out, in0=in0, scalar1=s1, scalar2=None, op0=op0),
                     reads=r, writes=w, cost=self._ecost(eng, out))
        else:
            self.rec(eng, lambda e: e.tensor_scalar(out=out, in0=in0, scalar1=s1, scalar2=s2, op0=op0, op1=op1),
                     reads=r, writes=w, cost=self._ecost(eng, out))

    def stt(self, eng, out, in0, scalar, in1, op0, op1, r, w):
        self.rec(eng, lambda e: e.scalar_tensor_tensor(out=out, in0=in0, scalar=scalar, in1=in1, op0=op0, op1=op1),
                 reads=r, writes=w, cost=self._ecost(eng, out))

    def cp(self, eng, out, in_, r, w):
        self.rec(eng, lambda e: e.tensor_copy(out=out, in_=in_), reads=r, writes=w, cost=self._ecost(eng, out))

    def memset(self, eng, ap, val, w):
        self.rec(eng, lambda e: e.memset(ap, val), writes=w, cost=self._ecost(eng, ap))

    def dma(self, eng, out, in_, r, w, lane):
        self.rec(eng, lambda e: e.dma_start(out=out, in_=in_), reads=r, writes=w, dma=True, lane=lane)

    def wload(self, dst, src2d, key, eng="pool"):
        self.dma(eng, dst, src2d.rearrange("(kc p) n -> p kc n", p=128), [], [key], key)

    def build(self):
        nc = bass.Bass("TRN2", target_bir_lowering=False)
        self.nc = nc
        ns = self.nseq
        self._din = {}
        self._dshapes = {
            "x": (ns, SEQ, D), "p": (ns, SEQ, PLE), "w_in": (D, IN_COLS), "w_att": (512, D), "w_ssd": (2048, D),
            "w_out": (D, D), "w_eg": (NEXP, D, 512), "w_eu": (NEXP, D, 512), "w_ed": (NEXP, 512, D),
            "w_pg": (D, D), "w_pp": (PLE, D), "w_r": (D, 20), "gains": (4, D), "vec32": (3, 32), "b_r": (1, 20),
            "convw": (128, 32, 4), "convb": (128, 32), "ng": (128, 16), "c_ident": (128, 128), "c_tri": (128, 128),
            "c_mask": (128, 512), "c_em": (128, 24, 256),
        }
        self.out_d = nc.dram_tensor("out", [ns, SEQ, D], F32, kind="ExternalOutput").ap()
        self.dbg_outs = {}
        with ExitStack() as es:
            self.es = es
            self.S = Sched(nc, es)
            self.M = Mem(nc)
            self.alloc_psum()
            self.setup_consts()
            for s in range(ns):
                self.seq(s)
                if self.stop_after is not None:
                    break
            self.S.barrier()
            self.S.emit()
        return nc

    def alloc_psum(self):
        nc, es = self.nc, self.es
        self.pb = [es.enter_context(nc.psum_tensor(f"pb{i}", [128, 512], F32)) for i in range(7)]
        self.tp = es.enter_context(nc.psum_tensor("tp", [128, 1024], BF16))
        self.tps = [(self.tp, "tp"), (self.pb[3].bitcast(BF16), "pb3")]

    def dbg_dump(self, name, ap, shape, keys, dt=F32):
        d = self.nc.dram_tensor("dbg_" + name, list(shape), dt, kind="ExternalOutput").ap()
        self.dbg_outs[name] = d
        self.dma("sp", d, ap, keys, [], "dbg_" + name)

    def setup_consts(self):
        M = self.M
        S = self.S
        self.identf = M.alloc([128, 128], F32, "identf")
        self.identb = M.alloc([128, 128], BF16, "identb")
        self.tri = M.alloc([128, 128], F32, "tri")
        self.onesf = M.alloc([128, 128], F32, "onesf")
        self.maskb = M.alloc([128, 512], BF16, "maskb")
        self.ddiag = M.alloc([128, 32, 128], BF16, "ddiag")
        self.gbc = M.alloc([128, D], F32, "gbc")
        self.cw = M.alloc([128, 32, 4], F32, "cw")
        self.cb = M.alloc([128, 32], F32, "cb")
        self.ngt = M.alloc([128, 16], F32, "ngt")
        self.v32 = M.alloc([128, 3, 32], F32, "v32")
        self.abc = M.alloc([128, 32], F32, "abc")
        self.brb = M.alloc([128, 20], F32, "brb")
        self.wrf = M.alloc([128, KC, 20], F32, "wrf")
        self.wrhi = M.alloc([128, KC, 20], BF16, "wrhi")
        self.wrlo = M.alloc([128, KC, 20], BF16, "wrlo")
        self.dma("sp", self.identf[:], self.din("c_ident"), [], ["identf"], "c0")
        self.dma("sp", self.tri[:], self.din("c_tri"), [], ["tri"], "c1")
        self.dma("pool", self.maskb[:], self.din("c_mask"), [], ["maskb"], "c2")
        self.dma("sp", self.cw[:], self.din("convw"), [], ["cw"], "c4")
        self.dma("sp", self.cb[:], self.din("convb"), [], ["cb"], "c5")
        self.dma("sp", self.ngt[:], self.din("ng"), [], ["ngt"], "c6")
        self.dma("sp", self.v32[:].rearrange("p a b -> p (a b)"),
                 self.din("vec32").rearrange("a b -> (a b)").partition_broadcast(128), [], ["v32"], "c7")
        self.dma("sp", self.brb[:], self.din("b_r")[0].partition_broadcast(128), [], ["brb"], "c8")
        self.dma("sp", self.wrf[:], self.din("w_r").rearrange("(kc p) n -> p kc n", p=128), [], ["wrf"], "c9")
        self.cp("dve", self.identb[:], self.identf[:], ["identf"], ["identb"])
        self.memset("dve", self.onesf[:], 1.0, ["onesf"])
        self.act(self.abc[:], self.v32[:, 1, :], AF.Exp, ["v32"], ["abc"])
        self.ts("dve", self.abc[:], self.abc[:], -1.0, None, ALU.mult, None, ["abc"], ["abc"])
        for h in range(32):
            self.ts("dve", self.ddiag[:, h, :], self.identf[:], self.v32[:, 2, h:h + 1], None, ALU.mult, None,
                    ["identf", "v32"], ["ddiag"])
        self.cp("dve", self.wrhi[:], self.wrf[:], ["wrf"], ["wrhi"])
        self.tt("dve", self.wrf[:], self.wrf[:], self.wrhi[:], ALU.subtract, ["wrf", "wrhi"], ["wrf"])
        self.cp("dve", self.wrlo[:], self.wrf[:], ["wrf"], ["wrlo"])
        self.const_mark = M.mark()

    def load_gain(self, idx):
        self.dma("sp", self.gbc[:], self.din("gains")[idx].partition_broadcast(128), [], ["gbc"], "gbc")

    def norm_T(self, xr, xkeys, outT, outkey, gidx, lo=None):
        M = self.M
        mk = M.mark()
        ssq = M.alloc([128, NT], F32, "ssq")
        rs = M.alloc([128, NT], F32, "rs")
        junk = M.alloc([128, D], BF16, "junk")
        hn = [M.alloc([128, D], BF16, "hn") for _ in range(2)]
        self.load_gain(gidx)
        self.memset("dve", ssq[:], 0.0, ["ssq"])
        for t in range(NT):
            self.act(junk[:], xr[:, t, :], AF.Square, [xkeys(t), "ssq"], [f"ssq:{t}", "junk"], accum=ssq[:, t:t + 1])
        self.ts("dve", rs[:], ssq[:], 1.0 / D, EPS, ALU.mult, ALU.add, [f"ssq:{t}" for t in range(NT)], ["rs"])
        self.act(rs[:], rs[:], AF.Ln, ["rs"], ["rs"])
        self.act(rs[:], rs[:], AF.Exp, ["rs"], ["rs"], scale=-0.5)
        for t in range(NT):
            h = hn[t % 2]
            hk = f"hn{t % 2}"
            self.stt("dve", h[:], xr[:, t, :], rs[:, t:t + 1], self.gbc[:], ALU.mult, ALU.mult,
                     [xkeys(t), "rs", "gbc"], [hk])
            tpb, tk = self.tps[t % 2]
            for c in range(KC):
                self.tr(tpb[:, c * 128:(c + 1) * 128], h[:, c * 128:(c + 1) * 128], self.identb[:],
                        [hk, "identb"], [tk])
            self.act(outT[:, :, t * 128:(t + 1) * 128], tpb[:, :].rearrange("p (c n) -> p c n", c=KC), AF.Copy,
                     [tk], [f"{outkey}:{t}"])
        M.release(mk)
        return rs

    def seq(self, s):
        M = self.M
        S = self.S
        M.release(self.const_mark)
        self.hT = M.alloc([128, KC, SEQ], BF16, "hT")
        self.big = M.mark()
        xr = M.alloc([128, NT, D], F32, "xr")
        for t in range(NT):
            self.dma("sp", xr[:, t, :], self.din("x")[s, t * 128:(t + 1) * 128, :], [], [f"xr:{t}"], f"xr{t % 4}")
        self.norm_T(xr, lambda t: f"xr:{t}", self.hT, "hT", 0)
        S.barrier()
        if self.stop_after == "norm":
            self.dbg_hT()
            return
        M.release(self.big)
        self.hole = M.mark()
        if self.phase_ssd(s):
            return
        if self.phase_att(s):
            return
        if self.phase_out(s):
            return
        if self.phase_moe(s):
            return
        self.phase_ple(s)

    def dbg_hT(self):
        M = self.M
        tmp = M.alloc([128, KC, SEQ], F32, "dbgt")
        self.cp("dve", tmp[:], self.hT[:], [f"hT:{t}" for t in range(NT)], ["dbgt"])
        self.dbg_dump("hT", tmp[:], (128, KC, SEQ), ["dbgt"])

    def phase_ssd(self, s):
        M, S = self.M, self.S
        hT = self.hT
        hkeys = [f"hT:{t}" for t in range(NT)]
        GT = M.alloc([128, 16, SEQ], BF16, "GT")
        ssq = M.alloc([128, NT, 8], F32, "ssq_ssd")
        scr = M.mark()
        self.mb_start = scr
        if "ssd" in self.skip:
            self.Mb = M.alloc([128, NT, D], BF16, "Mb")
            self.mb_end = M.mark()
            for t in range(NT):
                self.memset("dve", self.Mb[:, t, :], 0.0, [f"Mb:{t}:0", f"Mb:{t}:1"])
            S.barrier()
            return False
        adt = M.alloc([128, NT, 32], F32, "adt")
        cdec = M.alloc([128, NT, 32], F32, "cdec")
        sd = M.alloc([128, NT, 32], F32, "sd")
        biasL = M.alloc([128, NT, 32], F32, "biasL")
        dtmark = M.mark()
        wdt = M.alloc([128, KC, 32], BF16, "wdt")
        spre = M.alloc([128, NT, 32], F32, "spre")
        dtt = M.alloc([128, NT, 32], F32, "dtt")
        lndt = M.alloc([128, NT, 32], F32, "lndt")
        self.wload(wdt[:], self.din("w_in")[:, OFF_DT:OFF_DT + 32], "wdt")
        self.memset("dve", ssq[:], 0.0, ["ssq_ssd"])
        pb = self.pb
        for t in range(NT):
            for kc in range(KC):
                self.mm(pb[0][:, t * 32:(t + 1) * 32], hT[:, kc, t * 128:(t + 1) * 128], wdt[:, kc, :],
                        kc == 0, kc == KC - 1, [hkeys[t], "wdt"], ["pb0"])
        b3 = lambda ap: ap.rearrange("p (t h) -> p t h", h=32)
        self.tt("dve", spre[:], b3(pb[0][:, :]), self.v32[:, 0, :].unsqueeze(1).to_broadcast([128, NT, 32]), ALU.add,
                ["pb0", "v32"], ["spre"])
        self.act(spre[:], spre[:], AF.Exp, ["spre"], ["spre"])
        self.ts("dve", spre[:], spre[:], 1.0, None, ALU.add, None, ["spre"], ["spre"])
        self.act(dtt[:], spre[:], AF.Ln, ["spre"], ["dtt"])
        self.act(lndt[:], dtt[:], AF.Ln, ["dtt"], ["lndt"])
        self.tt("dve", adt[:], dtt[:], self.abc[:].unsqueeze(1).to_broadcast([128, NT, 32]), ALU.mult,
                ["dtt", "abc"], ["adt"])
        for c in range(NT):
            self.mm(pb[1][:, c * 32:(c + 1) * 32], self.tri[:], adt[:, c, :], True, True, ["tri", "adt"], ["pb1"])
            self.mm(pb[2][:, c * 32:(c + 1) * 32], self.onesf[:], adt[:, c, :], True, True, ["onesf", "adt"], ["pb2"])
        self.act(cdec[:], b3(pb[2][:, :]), AF.Exp, ["pb2"], ["cdec"])
        self.act(sd[:], b3(pb[1][:, :]), AF.Exp, ["pb1"], ["sd"])
        self.stt("dve", biasL[:], b3(pb[1][:, :]), -1.0, lndt[:], ALU.mult, ALU.add, ["pb1", "lndt"], ["biasL"])
        if self.stop_after == "dt":
            self.dbg_dump("dtt", dtt[:], (128, NT, 32), ["dtt"])
            self.dbg_dump("biasL", biasL[:], (128, NT, 32), ["biasL"])
            self.dbg_dump("cdec", cdec[:], (128, NT, 32), ["cdec"])
            return True
        S.barrier()
        M.release(dtmark)
        wx = M.alloc([128, KC, 256], BF16, "wx")
        wB = M.alloc([128, KC, 128], BF16, "wB")
        wC = M.alloc([128, KC, 128], BF16, "wC")
        wz = M.alloc([128, KC, 256], BF16, "wz")
        junk = M.alloc([128, 256], BF16, "junk2")
        gb = []
        for gi in range(2):
            gb.append(dict(xT=M.alloc([128, 2, SEQ], BF16, "xT"), BT=M.alloc([128, SEQ], BF16, "BT"),
                           CT=M.alloc([128, SEQ], BF16, "CT"), sz=M.alloc([128, NT, 256], BF16, "sz"),
                           S32=M.alloc([128, 256], F32, "S32"), Sbf=M.alloc([128, 256], BF16, "Sbf")))
        amark = M.mark()
        HS = SEQ // 2
        raws = [M.alloc([128, 3 + HS], F32, "raw") for _ in range(2)]
        caccs = [M.alloc([128, HS], F32, "cacc") for _ in range(2)]
        M.release(amark)
        self._convi = 0
        sets = []
        for par in range(2):
            sets.append(dict(
                E=M.alloc([128, 4, 128], F32, "E"), mixT=M.alloc([128, 4, 128], BF16, "mixT"),
                xB=M.alloc([128, 384], BF16, "xB"), xds=M.alloc([128, 256], BF16, "xds"),
                adtb=M.alloc([128, 4, 128], F32, "adtb"), ysb=M.alloc([128, 256], F32, "ysb"),
                tmp=M.alloc([128, 256], F32, "tmp"), G=M.alloc([128, 256], BF16, "G"),
            ))
        tpB = self.pb[3].bitcast(BF16)
        bankset = [dict(cb=pb[4], R=pb[5], Y=pb[6], tp=self.tp, kcb="pb4", kR="pb5", kY="pb6", ktp="tp"),
                   dict(cb=pb[0], R=pb[1], Y=pb[2], tp=tpB, kcb="pb0", kR="pb1", kY="pb2", ktp="pb3")]

        def inproj(g, gi):
            B = gb[gi]
            sfx = f"{gi}"
            xT, BT, CT, sz = B["xT"], B["BT"], B["CT"], B["sz"]
            self.wload(wx[:], self.din("w_in")[:, OFF_X + 256 * g:OFF_X + 256 * (g + 1)], "wx")
            self.wload(wB[:], self.din("w_in")[:, OFF_B + 128 * g:OFF_B + 128 * (g + 1)], "wB")
            self.wload(wC[:], self.din("w_in")[:, OFF_C + 128 * g:OFF_C + 128 * (g + 1)], "wC")
            self.wload(wz[:], self.din("w_in")[:, OFF_Z + 256 * g:OFF_Z + 256 * (g + 1)], "wz")
            for t in range(NT):
                bank = pb[3 + (t // 2) % 2]
                bk = f"pb{3 + (t // 2) % 2}"
                half = (t % 2) * 256
                for kc in range(KC):
                    self.mm(bank[:, half:half + 256], hT[:, kc, t * 128:(t + 1) * 128], wz[:, kc, :],
                            kc == 0, kc == KC - 1, [hkeys[t], "wz"], [bk])
                if t % 2 == 1:
                    self.act(sz[:, t - 1:t + 1, :], bank[:, :].rearrange("p (a n) -> p a n", a=2), AF.Silu,
                             [bk], ["sz" + sfx])
            specs = [(wx, "wx", 0, 2 * g, xT[:, 0, :], "xT0" + sfx), (wx, "wx", 128, 2 * g + 1, xT[:, 1, :], "xT1" + sfx),
                     (wB, "wB", 0, 16 + g, BT[:, :], "BT" + sfx), (wC, "wC", 0, 24 + g, CT[:, :], "CT" + sfx)]
            units = []
            for (wt, wk, coff, ch, dst, dkey) in specs:
                for half in range(2):
                    ci = self._convi % 2
                    self._convi += 1
                    units.append((wt, wk, coff, ch, dst, dkey, half, ci))

            def stage_a(u):
                (wt, wk, coff, ch, dst, dkey, half, ci) = u
                raw, cacc = raws[ci], caccs[ci]
                rk, ck = f"raw{ci}", f"cacc{ci}"
                if half == 0:
                    self.memset("dve", raw[:, 0:3], 0.0, [rk + "h"])
                else:
                    self.cp("dve", raw[:, 0:3], raws[1 - ci][:, HS:HS + 3], [f"raw{1 - ci}:1"], [rk + "h"])
                for t2 in range(2):
                    tc = half * 2 + t2
                    bank = pb[tc % 2]
                    bk = f"pb{tc % 2}"
                    for kc in range(KC):
                        self.mm(bank[:, :], wt[:, kc, coff:coff + 128], hT[:, kc, tc * 512:(tc + 1) * 512],
                                kc == 0, kc == KC - 1, [wk] + hkeys[tc * 4:tc * 4 + 4], [bk])
                    self.act(raw[:, 3 + t2 * 512:3 + (t2 + 1) * 512], bank[:, :], AF.Copy, [bk], [rk + f":{t2}"])
                rks = [rk + "h", rk + ":0", rk + ":1"]
                self.act(cacc[:], raw[:, 3:3 + HS], AF.Identity, rks + ["cw", "cb"], [ck],
                         bias=self.cb[:, ch:ch + 1], scale=self.cw[:, ch, 3:4])

            def stage_b(u):
                (wt, wk, coff, ch, dst, dkey, half, ci) = u
                raw, cacc = raws[ci], caccs[ci]
                rk, ck = f"raw{ci}", f"cacc{ci}"
                rks = [rk + "h", rk + ":0", rk + ":1"]
                for j in (2, 1, 0):
                    self.stt("dve", cacc[:], raw[:, j:j + HS], self.cw[:, ch, j:j + 1], cacc[:], ALU.mult, ALU.add,
                             rks + ["cw", ck], [ck])
                self.act(dst[:, half * HS:(half + 1) * HS], cacc[:], AF.Silu, [ck], [dkey])

            for i in range(len(units) + 1):
                if i < len(units):
                    stage_a(units[i])
                if i >= 1:
                    stage_b(units[i - 1])

        def chunk(c, g, gi):
            B = gb[gi]
            st = sets[gi]
            bs = bankset[gi]
            sfx = f"{gi}"
            xT, BT, CT, sz, S32, Sbf = B["xT"], B["BT"], B["CT"], B["sz"], B["S32"], B["Sbf"]
            b_cb, b_R, b_Y, tpb = bs["cb"], bs["R"], bs["Y"], bs["tp"]
            k_cb, k_R, k_Y, tk = bs["kcb"], bs["kR"], bs["kY"], bs["ktp"]
            cols = slice(c * 128, (c + 1) * 128)
            self.tr(tpb[:, 0:128], xT[:, 0, cols], self.identb[:], ["xT0" + sfx, "identb"], [tk])
            self.tr(tpb[:, 128:256], xT[:, 1, cols], self.identb[:], ["xT1" + sfx, "identb"], [tk])
            self.tr(tpb[:, 256:384], BT[:, cols], self.identb[:], ["BT" + sfx, "identb"], [tk])
            self.act(st["xB"][:], tpb[:, 0:384], AF.Copy, [tk], ["xB" + sfx])
            self.mm(b_cb[:, 0:128], BT[:, cols], CT[:, cols], True, True, ["BT" + sfx, "CT" + sfx], [k_cb])
            self.cp("dve", st["adtb"][:], adt[:, c, 4 * g:4 * g + 4].unsqueeze(2).to_broadcast([128, 4, 128]),
                    ["adt"], ["adtb" + sfx])
            self.mm(b_R[:, :], self.identb[:], self.maskb[:], True, False, ["identb", "maskb"], [k_R])
            for j in range(4):
                self.mm(b_R[:, j * 128:(j + 1) * 128], st["adtb"][:, j, :], self.tri[:], False, True,
                        ["adtb" + sfx, "tri"], [k_R])
            for j in range(4):
                self.act(st["E"][:, j, :], b_R[:, j * 128:(j + 1) * 128], AF.Exp, [k_R, "biasL"], [f"E{sfx}:{j}"],
                         bias=biasL[:, c, 4 * g + j:4 * g + j + 1])
            ek = [f"E{sfx}:{j}" for j in range(4)]
            self.tt("dve", st["mixT"][:], b_cb[:, 0:128].unsqueeze(1).to_broadcast([128, 4, 128]), st["E"][:],
                    ALU.mult, [k_cb] + ek, ["mixT" + sfx])
            x4 = st["xB"][:, 0:256].rearrange("p (j d) -> p j d", d=64)
            if c < NT - 1:
                self.tt("dve", st["xds"][:].rearrange("p (j d) -> p j d", d=64), x4,
                        st["E"][:, :, 127:128].to_broadcast([128, 4, 64]), ALU.mult,
                        ["xB" + sfx] + ek, ["xds" + sfx])
            for j in range(4):
                self.mm(b_Y[:, j * 64:(j + 1) * 64], st["mixT"][:, j, :], st["xB"][:, j * 64:(j + 1) * 64],
                        True, False, ["mixT" + sfx, "xB" + sfx], [k_Y])
                self.mm(b_Y[:, j * 64:(j + 1) * 64], self.ddiag[:, 4 * g + j, :], st["xB"][:, j * 64:(j + 1) * 64],
                        False, True, ["ddiag", "xB" + sfx], [k_Y])
            if c > 0:
                self.mm(b_Y[:, 256:512], CT[:, cols], Sbf[:], True, True, ["CT" + sfx, "Sbf" + sfx], [k_Y])
            if c < NT - 1:
                self.mm(b_cb[:, 128:384], st["xB"][:, 256:384], st["xds"][:], True, True,
                        ["xB" + sfx, "xds" + sfx], [k_cb])
            if c > 0:
                self.tt("dve", st["tmp"][:].rearrange("p (j d) -> p j d", d=64),
                        b_Y[:, 256:512].rearrange("p (j d) -> p j d", d=64),
                        sd[:, c, 4 * g:4 * g + 4].unsqueeze(2).to_broadcast([128, 4, 64]), ALU.mult,
                        [k_Y, "sd"], ["tmp" + sfx])
                self.tt("dve", st["ysb"][:], b_Y[:, 0:256], st["tmp"][:], ALU.add, [k_Y, "tmp" + sfx],
                        ["ysb" + sfx])
            else:
                self.cp("dve", st["ysb"][:], b_Y[:, 0:256], [k_Y], ["ysb" + sfx])
            self.tt("pool", st["G"][:], st["ysb"][:], sz[:, c, :], ALU.mult, ["ysb" + sfx, "sz" + sfx], ["G" + sfx])
            self.act(junk[:], st["G"][:], AF.Square, ["G" + sfx, "ssq_ssd"], [f"ssqs:{c}:{g}", "junk2"],
                     accum=ssq[:, c, g:g + 1])
            self.tr(tpb[:, 384:512], st["G"][:, 0:128], self.identb[:], ["G" + sfx, "identb"], [tk])
            self.tr(tpb[:, 512:640], st["G"][:, 128:256], self.identb[:], ["G" + sfx, "identb"], [tk])
            self.act(GT[:, 2 * g, cols], tpb[:, 384:512], AF.Copy, [tk, "ngt"], [f"GT:{c}"],
                     scale=self.ngt[:, 2 * g:2 * g + 1])
            self.ts("dve", GT[:, 2 * g + 1, cols], tpb[:, 512:640], self.ngt[:, 2 * g + 1:2 * g + 2], None, ALU.mult, None,
                    [tk, "ngt"], [f"GT:{c}"])
            if c < NT - 1:
                if c == 0:
                    self.cp("dve", S32[:], b_cb[:, 128:384], [k_cb], ["S32" + sfx])
                else:
                    self.tt("dve", S32[:].rearrange("p (j d) -> p j d", d=64),
                            S32[:].rearrange("p (j d) -> p j d", d=64),
                            cdec[:, c, 4 * g:4 * g + 4].unsqueeze(2).to_broadcast([128, 4, 64]), ALU.mult,
                            ["S32" + sfx, "cdec"], ["S32" + sfx])
                    self.tt("dve", S32[:], b_cb[:, 128:384], S32[:], ALU.add, [k_cb, "S32" + sfx], ["S32" + sfx])
                self.act(Sbf[:], S32[:], AF.Copy, ["S32" + sfx], ["Sbf" + sfx])

        for gp in range(4):
            for gi in range(2):
                inproj(2 * gp + gi, gi)
            S.barrier()
            streams = []
            for gi in range(2):
                self.defer = []
                for c in range(NT):
                    chunk(c, 2 * gp + gi, gi)
                streams.append(self.defer)
                self.defer = None
            self.merge(streams)
            S.barrier()
        if self.stop_after == "ssd":
            S.barrier()
            M.release(scr)
            tmpf = M.alloc([128, 2, SEQ], F32, "dbgg")
            self.cp("dve", tmpf[:], GT[:, 0:2, :], [f"GT:{c}" for c in range(NT)], ["dbgg"])
            self.dbg_dump("GT", tmpf[:], (128, 2, SEQ), ["dbgg"])
            self.dbg_dump("ssq", ssq[:], (128, NT, 8), ["ssq_ssd"])
            return True
        S.barrier()
        M.release(scr)
        self.Mb = M.alloc([128, NT, D], BF16, "Mb")
        self.mb_end = M.mark()
        Mb = self.Mb
        red = M.alloc([128, NT], F32, "red")
        rs = M.alloc([128, NT], F32, "rs_ssd")
        wssd = M.alloc([128, 16, D], BF16, "wssd")
        wgs = M.alloc([128, KC, D], BF16, "wgs")
        sgt1 = M.alloc([128, 512], BF16, "sgt")
        sgt = [sgt1, sgt1]
        self.wload(wssd[:], self.din("w_ssd")[:, :], "wssd")
        self.wload(wgs[:], self.din("w_in")[:, OFF_GS:OFF_GS + D], "wgs")
        self.S.op("dve", lambda e: e.tensor_reduce(out=red[:], in_=ssq[:], axis=AX.X, op=ALU.add),
                  reads=["ssq_ssd"] + [f"ssqs:{c}:{g}" for c in range(NT) for g in range(8)], writes=["red"])
        self.ts("dve", rs[:], red[:], 1.0 / 2048, EPS, ALU.mult, ALU.add, ["red"], ["rs_ssd"])
        self.act(rs[:], rs[:], AF.Ln, ["rs_ssd"], ["rs_ssd"])
        self.act(rs[:], rs[:], AF.Exp, ["rs_ssd"], ["rs_ssd"], scale=-0.5)
        i = 0
        for t in range(NT):
            tcols = slice(t * 128, (t + 1) * 128)
            for half in range(2):
                hc = slice(half * 512, (half + 1) * 512)
                by, bg = pb[i % 2], pb[2 + i % 2]
                ky, kg = f"pb{i % 2}", f"pb{2 + i % 2}"
                for cc in range(16):
                    self.mm(by[:, :], GT[:, cc, tcols], wssd[:, cc, hc], cc == 0, cc == 15, [f"GT:{t}", "wssd"], [ky])
                for kc in range(KC):
                    self.mm(bg[:, :], hT[:, kc, tcols], wgs[:, kc, hc], kc == 0, kc == KC - 1, [hkeys[t], "wgs"], [kg])
                self.act(sgt[i % 2][:], bg[:, :], AF.Sigmoid, [kg], ["sgt"])
                self.stt("dve", Mb[:, t, hc], by[:, :], rs[:, t:t + 1], sgt[i % 2][:], ALU.mult, ALU.mult,
                         [ky, "rs_ssd", "sgt"], [f"Mb:{t}:{half}"])
                i += 1
        if self.stop_after == "ssdtail":
            S.barrier()
            M.release(self.hole)
            tmpf = M.alloc([128, NT, D], F32, "dbgm")
            self.cp("dve", tmpf[:], Mb[:], [f"Mb:{t}:{h}" for t in range(NT) for h in range(2)], ["dbgm"])
            self.dbg_dump("Mb", tmpf[:], (128, NT, D), ["dbgm"])
            return True
        S.barrier()
        M.release(self.mb_end)
        return False

    def phase_att(self, s):
        M, S = self.M, self.S
        hT, Mb, pb = self.hT, self.Mb, self.pb
        hkeys = [f"hT:{t}" for t in range(NT)]
        M.release(self.hole)
        attT = M.alloc([64, 8, SEQ], BF16, "attT")
        acc = M.alloc([65, 2, SEQ], F32, "acc")
        v_sb = M.alloc([128, 16, 2, 80], BF16, "v_sb")
        pexp = [M.alloc([128, 2, 256], BF16, "pexp") for _ in range(2)]
        pm = [M.alloc([128, 2, 256], BF16, "pm") for _ in range(2)]
        assert M.mark() <= self.mb_start
        M.release(self.mb_end)
        ems = [M.alloc([128, 2, 256], BF16, "em") for _ in range(2)]
        qT = [M.alloc([128, SEQ], BF16, "qT") for _ in range(2)]
        kT = [M.alloc([128, SEQ], BF16, "kT") for _ in range(2)]
        vT = [M.alloc([128, SEQ], BF16, "vT") for _ in range(2)]
        accs = [acc, M.alloc([65, 2, SEQ], F32, "acc")]
        v_sb2 = M.alloc([128, 16, 2, 80], BF16, "v_sb")
        vsb = [v_sb, v_sb2]
        wq = [M.alloc([128, KC, 128], BF16, "wq") for _ in range(2)]
        wk = [M.alloc([128, KC, 128], BF16, "wk") for _ in range(2)]
        wv = [M.alloc([128, KC, 128], BF16, "wv") for _ in range(2)]
        self.memset("dve", vsb[0][:], 1.0, ["v_sb0"])
        self.memset("dve", vsb[1][:], 1.0, ["v_sb1"])
        sbanks = (((pb[4], "pb4"), (pb[5], "pb5")), ((pb[0], "pb0"), (pb[1], "pb1")))
        obanks1 = ((pb[6], "pb6"), (pb[3], "pb3"))
        units = [(hp, g) for hp in range(4) for g in range(3)]

        def proj(ui):
            hp, g = units[ui]
            st = ui % 2
            d = ATT_PAT[g][1]
            c0 = g * 512 + hp * 128
            self.wload(wq[st][:], self.din("w_in")[:, OFF_Q + c0:OFF_Q + c0 + 128], f"wq{st}")
            self.wload(wk[st][:], self.din("w_in")[:, OFF_K + c0:OFF_K + c0 + 128], f"wk{st}")
            self.wload(wv[st][:], self.din("w_in")[:, OFF_V + c0:OFF_V + c0 + 128], f"wv{st}")
            self.dma("pool", ems[st][:], self.din("c_em")[:, g * 8 + hp * 2:g * 8 + hp * 2 + 2, :], [], [f"em{st}"], f"em{st}")
            i = 0
            for (wt, wkey, dst, dkey) in ((wq[st], f"wq{st}", qT[st], f"qT{st}"), (wk[st], f"wk{st}", kT[st], f"kT{st}"),
                                          (wv[st], f"wv{st}", vT[st], f"vT{st}")):
                for tc in range(4):
                    bank, bk = pb[2], "pb2"
                    i += 1
                    for kc in range(KC):
                        self.mm(bank[:, :], wt[:, kc, :], hT[:, kc, tc * 512:(tc + 1) * 512], kc == 0, kc == KC - 1,
                                [wkey] + hkeys[tc * 4:tc * 4 + 4], [bk])
                    dv = dst[:, :].rearrange("p (r m) -> p r m", r=d)[:, :, tc * 512 // d:(tc + 1) * 512 // d]
                    sv = bank[:, :].rearrange("p (m r) -> p r m", r=d)
                    self.act(dv, sv, AF.Copy, [bk], [dkey])
            for b4 in range(4):
                for bb in range(4):
                    blk = b4 * 4 + bb
                    self.tr(self.tp[:, bb * 128:(bb + 1) * 128], vT[st][:, blk * 128:(blk + 1) * 128], self.identb[:],
                            [f"vT{st}", "identb"], ["tp"])
                self.act(vsb[st][:, b4 * 4:(b4 + 1) * 4, :, 0:64],
                         self.tp[:, 0:512].rearrange("p (b h d) -> p b h d", b=4, h=2), AF.Copy, ["tp"], [f"v_sb{st}"])

        def blocks(ui, b4, sidx):
            hp, g = units[ui]
            st = ui % 2
            d = ATT_PAT[g][1]
            nb = (SEQ // d) // 128
            acc = accs[hp % 2]
            ap_ = f"{hp % 2}"
            for bb in range(4):
                blk = b4 * 4 + bb
                n = blk % nb
                kts = [0, 1] if n > 0 else [1]
                lo = kts[0] * 128
                for hh in range(2):
                    sbank, sk = sbanks[sidx][hh]
                    hs = slice(64 * hh, 64 * hh + 64)
                    for kt in kts:
                        kc0 = (blk - 1 + kt) * 128
                        self.mm(sbank[:, kt * 128:(kt + 1) * 128],
                                kT[st][hs, kc0:kc0 + 128], qT[st][hs, blk * 128:(blk + 1) * 128], True, True,
                                [f"kT{st}", f"qT{st}"], [sk])
                for hh in range(2):
                    sbank, sk = sbanks[sidx][hh]
                    self.act(pexp[sidx][:, hh, lo:256], sbank[:, lo:256], AF.Exp,
                             [sk], [f"pexp{sidx}:{hh}"], scale=0.125)
                self.tt("dve", pm[sidx][:, :, lo:256], pexp[sidx][:, :, lo:256],
                        ems[st][:, :, lo:256], ALU.mult,
                        [f"pexp{sidx}:0", f"pexp{sidx}:1", f"em{st}"], [f"pm{sidx}"])
                ob, ok_ = obanks1[sidx]
                bbl = blk % 2
                for hh in range(2):
                    co = hh * 256 + bbl * 128
                    for kt in kts:
                        self.mm(ob[0:65, co:co + 128], vsb[st][:, blk - 1 + kt, hh, 0:65],
                                pm[sidx][:, hh, kt * 128:(kt + 1) * 128], kt == kts[0], kt == 1,
                                [f"v_sb{st}", f"pm{sidx}"], [ok_])
                if bbl == 1:
                    b2 = blk // 2
                    for hh in range(2):
                        sv = ob[0:65, hh * 256:(hh + 1) * 256]
                        if g == 0:
                            dv = acc[0:65, hh, b2 * 256:(b2 + 1) * 256]
                        elif g == 1:
                            r_, n0 = b2 // 2, (b2 % 2) * 2
                            a0 = r_ + 512 * n0
                            dv = acc[0:65, hh, a0:a0 + 255 * 4 + 1:4]
                        else:
                            dv = acc[0:65, hh, :].rearrange("p (i r) -> p r i", r=16)[:, 2 * b2:2 * b2 + 2, :]
                            sv = sv.rearrange("p (r i) -> p r i", r=2)
                        if g == 0:
                            self.cp("dve", dv, sv, [ok_], [f"acc{ap_}{hh}:{b2}"])
                        else:
                            aks = [f"acc{ap_}{hh}:{b}" for b in range(8)]
                            self.tt("dve", dv, sv, dv, ALU.add, [ok_] + aks, aks)

        def normalise(hp):
            acc = accs[hp % 2]
            for hh in range(2):
                aks = [f"acc{hp % 2}{hh}:{b}" for b in range(8)]
                self.rec("dve", lambda e, hh=hh: e.reciprocal(out=acc[64:65, hh, :], in_=acc[64:65, hh, :]),
                         reads=aks, writes=aks, cost=2.3)
                for tc in range(4):
                    tcs = slice(tc * 512, (tc + 1) * 512)
                    self.mm(pb[6][0:64, :], self.onesf[64:65, 0:64], acc[64:65, hh, tcs], True, True,
                            ["onesf"] + aks, ["pb6"])
                    self.tt("dve", attT[0:64, 2 * hp + hh, tcs], pb[6][0:64, :], acc[0:64, hh, tcs], ALU.mult,
                            ["pb6"] + aks, ["attT"])

        proj(0)
        for ui in range(len(units)):
            hp, g = units[ui]
            streams = []
            for sidx in range(2):
                self.defer = []
                if sidx == 0 and g == 0 and hp > 0:
                    normalise(hp - 1)
                for b4 in (sidx, sidx + 2):
                    blocks(ui, b4, sidx)
                streams.append(self.defer)
            if ui + 1 < len(units):
                self.defer = []
                proj(ui + 1)
                streams.append(self.defer)
            self.defer = None
            self.merge(streams)
        normalise(3)
        if self.stop_after == "att":
            S.barrier()
            M.release(self.mb_end)
            tmpf = M.alloc([64, 4, SEQ], F32, "dbga")
            self.cp("dve", tmpf[:], attT[:, 0:4, :], ["attT"], ["dbga"])
            self.dbg_dump("attT", tmpf[:], (64, 4, SEQ), ["dbga"])
            return True
        S.barrier()
        M.release(self.mb_end)
        watt = M.alloc([64, 8, D], BF16, "watt")
        wga = M.alloc([128, KC, D], BF16, "wga")
        sgt = [M.alloc([128, 512], F32, "sgt") for _ in range(2)]
        tmp = [M.alloc([128, 512], F32, "atmp") for _ in range(2)]
        self.dma("pool", watt[:], self.din("w_att").rearrange("(h d) n -> d h n", d=64), [], ["watt"], "watt")
        self.wload(wga[:], self.din("w_in")[:, OFF_GA:OFF_GA + D], "wga")
        i = 0
        for t in range(NT):
            tcols = slice(t * 128, (t + 1) * 128)
            for half in range(2):
                hc = slice(half * 512, (half + 1) * 512)
                by, bg = pb[i % 2], pb[2 + i % 2]
                ky, kg = f"pb{i % 2}", f"pb{2 + i % 2}"
                for h in range(8):
                    self.mm(by[:, :], attT[0:64, h, tcols], watt[0:64, h, hc], h == 0, h == 7, ["attT", "watt"], [ky])
                for kc in range(KC):
                    self.mm(bg[:, :], hT[:, kc, tcols], wga[:, kc, hc], kc == 0, kc == KC - 1, [hkeys[t], "wga"], [kg])
                self.act(sgt[i % 2][:], bg[:, :], AF.Sigmoid, [kg], [f"sgt{i % 2}"])
                self.tt("dve", tmp[i % 2][:], by[:, :], sgt[i % 2][:], ALU.mult, [ky, f"sgt{i % 2}"], [f"atmp{i % 2}"])
                self.tt("pool", Mb[:, t, hc], Mb[:, t, hc], tmp[i % 2][:], ALU.add, [f"Mb:{t}:{half}", f"atmp{i % 2}"],
                        [f"Mb:{t}:{half}"])
                i += 1
        if self.stop_after == "merged":
            S.barrier()
            M.release(self.hole)
            tmpf = M.alloc([128, NT, D], F32, "dbgm")
            self.cp("dve", tmpf[:], Mb[:], [f"Mb:{t}:{h}" for t in range(NT) for h in range(2)], ["dbgm"])
            self.dbg_dump("Mb", tmpf[:], (128, NT, D), ["dbgm"])
            return True
        S.barrier()
        M.release(self.mb_end)
        return False

    def phase_out(self, s):
        M, S = self.M, self.S
        Mb, pb = self.Mb, self.pb
        M.release(self.hole)
        self.x1 = M.alloc([128, NT, D], F32, "x1")
        assert M.mark() <= self.mb_start
        M.release(self.mb_end)
        x1 = self.x1
        wout = M.alloc([128, KC, D], BF16, "wout")
        mT = [M.alloc([128, KC, 128], BF16, "mT") for _ in range(2)]
        self.wload(wout[:], self.din("w_out")[:, :], "wout")
        for t in range(NT):
            self.dma("sp", x1[:, t, :], self.din("x")[s, t * 128:(t + 1) * 128, :], [], [f"x1:{t}:0", f"x1:{t}:1"], f"xr{t % 4}")
        i = 0
        for t in range(NT):
            par = t % 2
            tpb, tk = self.tps[t % 2]
            for kc in range(KC):
                self.tr(tpb[:, kc * 128:(kc + 1) * 128], Mb[:, t, kc * 128:(kc + 1) * 128], self.identb[:],
                        [f"Mb:{t}:{kc // 4}", "identb"], [tk])
            self.act(mT[par][:], tpb[:, :].rearrange("p (c n) -> p c n", c=KC), AF.Copy, [tk], [f"mT{par}"])
            for half in range(2):
                hc = slice(half * 512, (half + 1) * 512)
                bank, bk = pb[i % 3], f"pb{i % 3}"
                for kc in range(KC):
                    self.mm(bank[:, :], mT[par][:, kc, :], wout[:, kc, hc], kc == 0, kc == KC - 1, [f"mT{par}", "wout"], [bk])
                self.tt("dve", x1[:, t, hc], bank[:, :], x1[:, t, hc], ALU.add, [bk, f"x1:{t}:{half}"], [f"x1:{t}:{half}"])
                i += 1
        if self.stop_after == "x1":
            self.dbg_dump("x1", x1[:], (128, NT, D), [f"x1:{t}:{h}" for t in range(NT) for h in range(2)])
            return True
        S.barrier()
        return False

    def phase_moe(self, s):
        M, S = self.M, self.S
        pb, x1, hT = self.pb, self.x1, self.hT
        xk = lambda t: f"x1:{t}:0"
        M.release(self.mb_start)
        wset = [dict(g=M.alloc([128, KC, 512], BF16, "wg"), u=M.alloc([128, KC, 512], BF16, "wu"),
                     d=M.alloc([128, 4, D], BF16, "wd"))]
        assert M.mark() <= self.mb_end
        M.release(self.mb_end)
        comb = M.alloc([128, NT, 16], F32, "comb")
        mk = M.mark()
        ssq = M.alloc([128, NT], F32, "ssq")
        rs = M.alloc([128, NT], F32, "rs")
        junk = M.alloc([128, D], BF16, "junk")
        hnf = M.alloc([128, D], F32, "hnf")
        hi = [M.alloc([128, D], BF16, "hi") for _ in range(2)]
        lo = [M.alloc([128, D], BF16, "lo") for _ in range(2)]
        loT = [M.alloc([128, KC, 128], BF16, "loT") for _ in range(2)]
        lg = M.alloc([128, NT, 20], F32, "lg")
        sm = M.alloc([128, NT, 64], F32, "smalls")
        self.load_gain(1)
        self.memset("dve", ssq[:], 0.0, ["ssq"])
        for t in range(NT):
            self.act(junk[:], x1[:, t, :], AF.Square, [xk(t), f"x1:{t}:1", "ssq"], [f"ssq:{t}", "junk"], accum=ssq[:, t:t + 1])
        self.ts("dve", rs[:], ssq[:], 1.0 / D, EPS, ALU.mult, ALU.add, [f"ssq:{t}" for t in range(NT)], ["rs"])
        self.act(rs[:], rs[:], AF.Ln, ["rs"], ["rs"])
        self.act(rs[:], rs[:], AF.Exp, ["rs"], ["rs"], scale=-0.5)
        for t in range(NT):
            par = t % 2
            tcols = slice(t * 128, (t + 1) * 128)
            self.stt("dve", hnf[:], x1[:, t, :], rs[:, t:t + 1], self.gbc[:], ALU.mult, ALU.mult,
                     [xk(t), f"x1:{t}:1", "rs", "gbc"], ["hnf"])
            self.cp("dve", hi[par][:], hnf[:], ["hnf"], [f"hi{par}"])
            self.tt("dve", lo[par][:], hnf[:], hi[par][:], ALU.subtract, ["hnf", f"hi{par}"], [f"lo{par}"])
            for c in range(KC):
                self.tr(self.tp[:, c * 128:(c + 1) * 128], hi[par][:, c * 128:(c + 1) * 128], self.identb[:],
                        [f"hi{par}", "identb"], ["tp"])
            self.act(hT[:, :, tcols], self.tp[:, :].rearrange("p (c n) -> p c n", c=KC), AF.Copy, ["tp"], [f"hT:{t}"])
            tpb, tk = self.tps[1]
            for c in range(KC):
                self.tr(tpb[:, c * 128:(c + 1) * 128], lo[par][:, c * 128:(c + 1) * 128], self.identb[:],
                        [f"lo{par}", "identb"], [tk])
            self.act(loT[par][:], tpb[:, :].rearrange("p (c n) -> p c n", c=KC), AF.Copy, [tk], [f"loT{par}"])
            n = 0
            for kc in range(KC):
                for (a, ak, w, wk_) in ((hT[:, kc, tcols], f"hT:{t}", self.wrhi, "wrhi"),
                                        (hT[:, kc, tcols], f"hT:{t}", self.wrlo, "wrlo"),
                                        (loT[par][:, kc, :], f"loT{par}", self.wrhi, "wrhi")):
                    self.mm(pb[6][:, 0:20], a, w[:, kc, :], n == 0, n == 3 * KC - 1, [ak, wk_], ["pb6"])
                    n += 1
            self.tt("dve", lg[:, t, :], pb[6][:, 0:20], self.brb[:], ALU.add, ["pb6", "brb"], [f"lg:{t}"])
        self.route(lg, sm, comb)
        if self.stop_after == "route":
            self.dbg_dump("comb", comb[:], (128, NT, 16), [f"comb:{t}" for t in range(NT)])
            return True
        S.barrier()
        M.release(mk)
        wset.append(dict(g=M.alloc([128, KC, 512], BF16, "wg"), u=M.alloc([128, KC, 512], BF16, "wu"),
                         d=M.alloc([128, 4, D], BF16, "wd")))
        hid = [M.alloc([128, 4, 512], BF16, "hid") for _ in range(2)]
        sg = [M.alloc([128, 512], BF16, "sg") for _ in range(2)]
        units = [(e, tc) for e in range(NEXP) for tc in range(4)]
        cnt = {"d": 0}

        def stage_gu(u, ui):
            e, tc = u
            ws = wset[e % 2]
            sfx = f"{e % 2}"
            if tc == 0:
                self.wload(ws["g"][:], self.din("w_eg")[e], "wg" + sfx)
                self.wload(ws["u"][:], self.din("w_eu")[e], "wu" + sfx)
                self.wload(ws["d"][:], self.din("w_ed")[e], "wd" + sfx)
            hp_ = ui % 2
            tcs = slice(tc * 512, (tc + 1) * 512)
            hk = [f"hT:{t}" for t in range(tc * 4, tc * 4 + 4)]
            for fc in range(4):
                bg, bu = pb[(2 * fc) % 4], pb[(2 * fc + 1) % 4]
                kg, ku = f"pb{(2 * fc) % 4}", f"pb{(2 * fc + 1) % 4}"
                for kc in range(KC):
                    self.mm(bg[:, :], ws["g"][:, kc, fc * 128:(fc + 1) * 128], hT[:, kc, tcs], kc == 0, kc == KC - 1,
                            ["wg" + sfx] + hk, [kg])
                for kc in range(KC):
                    self.mm(bu[:, :], ws["u"][:, kc, fc * 128:(fc + 1) * 128], hT[:, kc, tcs], kc == 0, kc == KC - 1,
                            ["wu" + sfx] + hk, [ku])
                self.act(sg[fc % 2][:], bg[:, :], AF.Silu, [kg], [f"sg{fc % 2}"])
                self.tt("dve", hid[hp_][:, fc, :], bu[:, :], sg[fc % 2][:], ALU.mult, [ku, f"sg{fc % 2}"],
                        [f"hid{hp_}:{fc}"])

        def stage_d(u, ui):
            e, tc = u
            ws = wset[e % 2]
            sfx = f"{e % 2}"
            hp_ = ui % 2
            for tt_ in range(4):
                t = tc * 4 + tt_
                for half in range(2):
                    hc = slice(half * 512, (half + 1) * 512)
                    i = cnt["d"]
                    cnt["d"] += 1
                    bank, bk = pb[4 + i % 3], f"pb{4 + i % 3}"
                    for fc in range(4):
                        self.mm(bank[:, :], hid[hp_][:, fc, tt_ * 128:(tt_ + 1) * 128], ws["d"][:, fc, hc], fc == 0, fc == 3,
                                [f"hid{hp_}:{fc}", "wd" + sfx], [bk])
                    self.stt("dve", x1[:, t, hc], bank[:, :], comb[:, t, e:e + 1], x1[:, t, hc], ALU.mult, ALU.add,
                             [bk, f"comb:{t}", f"x1:{t}:{half}"], [f"x1:{t}:{half}"])

        for i in range(len(units) + 1):
            if i < len(units):
                stage_gu(units[i], i)
            if i >= 1:
                stage_d(units[i - 1], i - 1)
        if self.stop_after == "x2":
            self.dbg_dump("x2", x1[:], (128, NT, D), [f"x1:{t}:{h}" for t in range(NT) for h in range(2)])
            return True
        S.barrier()
        return False

    def route(self, lg, sm, comb):
        T = NT
        k = lambda n: f"sm:{n}"
        lgk = [f"lg:{t}" for t in range(T)]
        gl = lg[:, :, 0:4]
        el4 = lg[:, :, 4:20].rearrange("p t (g e) -> p t g e", g=4)
        col = lambda a, b: sm[:, :, a:b]
        gmax, gsum, gval, m1, m2, d12, w1, w2 = (sm[:, :, i] for i in range(8))
        ohg, ge, within, mask1 = col(8, 12), col(12, 16), col(16, 20), col(20, 24)
        w2in, mask2, ew, ewg = col(24, 28), col(28, 32), col(32, 36), col(36, 40)
        tmp16 = col(40, 56)
        b3 = lambda v: v.unsqueeze(2).to_broadcast([128, T, 4])
        op = self.S.op
        op("dve", lambda e: e.reduce_max(out=gmax, in_=gl, axis=AX.X), reads=lgk, writes=[k("gmax")])
        self.tt("dve", ohg, gl, b3(gmax), ALU.is_equal, lgk + [k("gmax")], [k("ohg")])
        self.tt("dve", ge, gl, b3(gmax), ALU.subtract, lgk + [k("gmax")], [k("ge")])
        self.act(ge, ge, AF.Exp, [k("ge")], [k("ge")])
        op("dve", lambda e: e.reduce_sum(out=gsum, in_=ge, axis=AX.X), reads=[k("ge")], writes=[k("gsum")])
        op("dve", lambda e: e.reciprocal(out=gval, in_=gsum), reads=[k("gsum")], writes=[k("gval")])
        t4 = tmp16.rearrange("p t (g e) -> p t g e", g=4)
        self.tt("dve", t4, el4, ohg.unsqueeze(3).to_broadcast([128, T, 4, 4]), ALU.mult, lgk + [k("ohg")], [k("tmp16")])
        op("dve", lambda e: e.tensor_reduce(out=within, in_=tmp16.rearrange("p t (g e) -> p t e g", g=4), axis=AX.X,
                                            op=ALU.add), reads=[k("tmp16")], writes=[k("within")])
        op("dve", lambda e: e.reduce_max(out=m1, in_=within, axis=AX.X), reads=[k("within")], writes=[k("m1")])
        self.tt("dve", mask1, within, b3(m1), ALU.is_equal, [k("within"), k("m1")], [k("mask1")])
        self.stt("dve", w2in, mask1, -1e30, within, ALU.mult, ALU.add, [k("mask1"), k("within")], [k("w2in")])
        op("dve", lambda e: e.reduce_max(out=m2, in_=w2in, axis=AX.X), reads=[k("w2in")], writes=[k("m2")])
        self.tt("dve", mask2, w2in, b3(m2), ALU.is_equal, [k("w2in"), k("m2")], [k("mask2")])
        self.tt("dve", d12, m2, m1, ALU.subtract, [k("m1"), k("m2")], [k("d12")])
        self.act(d12, d12, AF.Exp, [k("d12")], [k("d12")])
        self.ts("dve", d12, d12, 1.0, None, ALU.add, None, [k("d12")], [k("d12")])
        op("dve", lambda e: e.reciprocal(out=w1, in_=d12), reads=[k("d12")], writes=[k("w1")])
        self.ts("dve", w2, w1, -1.0, 1.0, ALU.mult, ALU.add, [k("w1")], [k("w2")])
        self.tt("dve", ew, mask1, b3(w1), ALU.mult, [k("mask1"), k("w1")], [k("ew")])
        self.tt("dve", ewg, mask2, b3(w2), ALU.mult, [k("mask2"), k("w2")], [k("ewg")])
        self.tt("dve", ew, ew, ewg, ALU.add, [k("ew"), k("ewg")], [k("ew")])
        self.tt("dve", ewg, ew, b3(gval), ALU.mult, [k("ew"), k("gval")], [k("ewg")])
        self.tt("dve", comb[:, :, :].rearrange("p t (g e) -> p t g e", g=4),
                ohg.unsqueeze(3).to_broadcast([128, T, 4, 4]), ewg.unsqueeze(2).to_broadcast([128, T, 4, 4]),
                ALU.mult, [k("ohg"), k("ewg")], [f"comb:{t}" for t in range(T)])

    def phase_ple(self, s):
        M, S = self.M, self.S
        pb, x1, hT = self.pb, self.x1, self.hT
        M.release(self.mb_start)
        xkeys = lambda t: f"x1:{t}:0"
        ssq = M.alloc([128, NT], F32, "ssq")
        rs = M.alloc([128, NT], F32, "rs")
        junk = M.alloc([128, D], BF16, "junk")
        hn = [M.alloc([128, D], BF16, "hn") for _ in range(2)]
        wpg = M.alloc([128, KC, D], BF16, "wpg")
        wpp = M.alloc([128, 2, D], BF16, "wpp")
        pT = M.alloc([128, 2, SEQ], BF16, "pT")
        ptile = [M.alloc([128, PLE], BF16, "ptile") for _ in range(2)]
        sgt = [M.alloc([128, 512], F32, "sgt") for _ in range(2)]
        tmp = [M.alloc([128, 512], F32, "ptmp") for _ in range(2)]
        ot = [M.alloc([128, D], F32, "ot") for _ in range(2)]
        self.wload(wpg[:], self.din("w_pg")[:, :], "wpg")
        self.wload(wpp[:], self.din("w_pp")[:, :], "wpp")
        self.load_gain(2)
        self.memset("dve", ssq[:], 0.0, ["ssq"])
        for t in range(NT):
            self.act(junk[:], x1[:, t, :], AF.Square, [xkeys(t), f"x1:{t}:1", "ssq"], [f"ssq:{t}", "junk"], accum=ssq[:, t:t + 1])
        self.ts("dve", rs[:], ssq[:], 1.0 / D, EPS, ALU.mult, ALU.add, [f"ssq:{t}" for t in range(NT)], ["rs"])
        self.act(rs[:], rs[:], AF.Ln, ["rs"], ["rs"])
        self.act(rs[:], rs[:], AF.Exp, ["rs"], ["rs"], scale=-0.5)
        for t in range(NT):
            par = t % 2
            tcols = slice(t * 128, (t + 1) * 128)
            self.stt("dve", hn[par][:], x1[:, t, :], rs[:, t:t + 1], self.gbc[:], ALU.mult, ALU.mult,
                     [xkeys(t), f"x1:{t}:1", "rs", "gbc"], [f"hn{par}"])
            for c in range(KC):
                self.tr(self.tp[:, c * 128:(c + 1) * 128], hn[par][:, c * 128:(c + 1) * 128], self.identb[:],
                        [f"hn{par}", "identb"], ["tp"])
            self.act(hT[:, :, tcols], self.tp[:, :].rearrange("p (c n) -> p c n", c=KC), AF.Copy, ["tp"], [f"hT:{t}"])
            self.dma("pool", ptile[par][:], self.din("p")[s, tcols, :], [], [f"ptile{par}"], f"ptile{par}")
            tpb, tk = self.tps[1]
            for c in range(2):
                self.tr(tpb[:, c * 128:(c + 1) * 128], ptile[par][:, c * 128:(c + 1) * 128], self.identb[:],
                        [f"ptile{par}", "identb"], [tk])
            self.act(pT[:, :, tcols], tpb[:, 0:256].rearrange("p (c n) -> p c n", c=2), AF.Copy, [tk], [f"pT:{t}"])
        S.barrier()
        self.load_gain(3)
        ssq2 = M.alloc([128, NT], F32, "ssq2")
        self.memset("dve", ssq2[:], 0.0, ["ssq2"])
        i = 0
        GRP = 8
        for t0 in range(0, NT, GRP):
            tiles = range(t0, t0 + GRP)
            for t in tiles:
                tcols = slice(t * 128, (t + 1) * 128)
                for half in range(2):
                    hc = slice(half * 512, (half + 1) * 512)
                    bg, bp = pb[i % 2], pb[2 + i % 2]
                    kg, kp = f"pb{i % 2}", f"pb{2 + i % 2}"
                    for kc in range(KC):
                        self.mm(bg[:, :], hT[:, kc, tcols], wpg[:, kc, hc], kc == 0, kc == KC - 1, [f"hT:{t}", "wpg"], [kg])
                    for c in range(2):
                        self.mm(bp[:, :], pT[:, c, tcols], wpp[:, c, hc], c == 0, c == 1, [f"pT:{t}", "wpp"], [kp])
                    self.act(sgt[i % 2][:], bg[:, :], AF.Sigmoid, [kg], [f"sgt{i % 2}"])
                    self.tt("dve", tmp[i % 2][:], bp[:, :], sgt[i % 2][:], ALU.mult, [kp, f"sgt{i % 2}"], [f"ptmp{i % 2}"])
                    self.tt("pool", x1[:, t, hc], x1[:, t, hc], tmp[i % 2][:], ALU.add, [f"x1:{t}:{half}", f"ptmp{i % 2}"],
                            [f"x1:{t}:{half}"])
                    i += 1
            for t in tiles:
                self.act(junk[:], x1[:, t, :], AF.Square, [f"x1:{t}:0", f"x1:{t}:1", "ssq2"], [f"ssq2:{t}", "junk"],
                         accum=ssq2[:, t:t + 1])
            gk = [f"ssq2:{t}" for t in tiles]
            gsl = slice(t0, t0 + GRP)
            self.ts("dve", ssq2[:, gsl], ssq2[:, gsl], 1.0 / D, EPS, ALU.mult, ALU.add, gk, gk)
            self.act(ssq2[:, gsl], ssq2[:, gsl], AF.Ln, gk, gk)
            self.act(ssq2[:, gsl], ssq2[:, gsl], AF.Exp, gk, gk, scale=-0.5)
            for t in tiles:
                par = t % 2
                tcols = slice(t * 128, (t + 1) * 128)
                self.stt("dve", ot[par][:], x1[:, t, :], ssq2[:, t:t + 1], self.gbc[:], ALU.mult, ALU.mult,
                         [f"x1:{t}:0", f"x1:{t}:1", f"ssq2:{t}", "gbc"], [f"ot{par}"])
                self.dma("sp", self.out_d[s, tcols, :], ot[par][:], [f"ot{par}"], [], f"ost{par}")
        S.barrier()
        return False


def _consts():
    ident = np.eye(128, dtype=np.float32)
    tri = np.triu(np.ones((128, 128), np.float32))
    j = np.arange(128)[:, None]
    i = np.arange(128)[None, :]
    m = np.where(j > i, -30000.0, 0.0).astype(np.float32)
    mask = np.tile(m, (1, 4))
    hh = np.arange(1, 25, dtype=np.float64)
    slopes = np.exp2(-8.0 * hh / 24.0)
    em = np.zeros((128, 24, 256), np.float64)
    for g, (win, dil) in enumerate(ATT_PAT):
        for h in range(8):
            sl = slopes[g * 8 + h] * dil
            prev = np.where(j >= i, np.exp(-sl * (128 + i - j)), 0.0)
            cur = np.where(i >= j, np.exp(-sl * (i - j)), 0.0)
            em[:, g * 8 + h, 0:128] = prev
            em[:, g * 8 + h, 128:256] = cur
    return ident, tri, mask, em.astype(np.float32)


def _prep_inputs(inputs, nseq, ncores):
    f = lambda a: np.ascontiguousarray(np.asarray(a, dtype=np.float32))
    ident, tri, mask, em = _consts()
    shared = {
        "w_in": f(inputs["w_in"][0]),
        "w_att": f(inputs["w_att_branch"][0]),
        "w_ssd": f(inputs["w_ssd_branch"][0]),
        "w_out": f(inputs["w_out"][0]),
        "w_eg": f(inputs["w_exp_gate"][0]),
        "w_eu": f(inputs["w_exp_up"][0]),
        "w_ed": f(inputs["w_exp_down"][0]),
        "w_pg": f(inputs["w_ple_gate"][0]),
        "w_pp": f(inputs["w_ple_proj"][0]),
        "w_r": f(np.concatenate([inputs["w_router_group"][0], inputs["w_router_expert"][0]], axis=1)),
        "gains": f(np.stack([inputs["norm_mix_g"][0], inputs["norm_ffn_g"][0], inputs["norm_ple_g"][0],
                             inputs["final_norm_g"]])),
        "vec32": f(np.stack([inputs["dt_bias"][0], inputs["a_log"][0], inputs["d_skip"][0]])),
        "b_r": f(np.concatenate([inputs["b_router_group"][0], inputs["b_router_expert"][0]])[None, :]),
        "convw": f(inputs["conv_w"][0].reshape(4, 32, 128).transpose(2, 1, 0)),
        "convb": f(inputs["conv_b"][0].reshape(32, 128).T),
        "ng": f(inputs["ssd_norm_g"][0].reshape(16, 128).T),
        "c_ident": ident, "c_tri": tri, "c_mask": mask, "c_em": em,
    }
    x = np.asarray(inputs["x"], np.float32)
    p = np.asarray(inputs["p"], np.float32)[0]
    maps = []
    for c in range(ncores):
        m = dict(shared)
        m["x"] = np.ascontiguousarray(x[c * nseq:(c + 1) * nseq])
        m["p"] = np.ascontiguousarray(p[c * nseq:(c + 1) * nseq])
        maps.append(m)
    return maps


def kernel(**inputs):
    nseq = 2
    prog = Prog(nseq)
    nc = prog.build()
    maps = _prep_inputs(inputs, nseq, NCORES)
    maps = [{k: v for k, v in m.items() if k in prog._din} for m in maps]
    res = run_bass_kernel_spmd(nc, maps, core_ids=list(range(NCORES)))
    out = np.concatenate([r["out"] for r in res.results], axis=0)
    return out.astype(np.float32)
```

```python
import numpy as np
from contextlib import ExitStack
import concourse.bass as bass
import concourse.mybir as mybir
from concourse.bass_utils import run_bass_kernel_spmd

F32 = mybir.dt.float32
BF16 = mybir.dt.bfloat16
AF = mybir.ActivationFunctionType
ALU = mybir.AluOpType
AX = mybir.AxisListType

NCORES = 8
D = 1024
SEQ = 2048
NT = 16
KC = 8
PLE = 256
EPS = 1e-6
IN_COLS = 12832
OFF_Q, OFF_K, OFF_V, OFF_Z, OFF_X, OFF_B, OFF_C, OFF_DT, OFF_GA, OFF_GS = (
    0, 1536, 3072, 4608, 6656, 8704, 9728, 10752, 10784, 11808)
ATT_PAT = ((128, 1), (512, 4), (2048, 16))
NEXP = 16

ENGS = ("pe", "act", "dve", "pool", "sp")


class _Op:
    __slots__ = ("eng", "fn", "deps", "is_dma", "lane", "sig", "sigval", "fin")


class Sched:
    def __init__(self, nc, es, sem_rot=30000):
        self.nc, self.es = nc, es
        self.ops = {e: [] for e in ENGS}
        self.all_ops = []
        self.last_w = {}
        self.readers = {}
        self.lanes = {}
        self.sem_rot = sem_rot
        self.nsem = 0

    def op(self, eng, fn, reads=(), writes=(), dma=False, lane=None):
        o = _Op()
        o.eng, o.fn, o.is_dma, o.lane = eng, fn, dma, lane
        o.sig, o.sigval = False, None
        o.fin = 0.0
        def _bank(k):
            if k.startswith("pb"):
                return k[:3]
            if k.startswith("tp"):
                return "tp"
            return None
        banks = [b for b in (_bank(k) for k in list(reads) + list(writes)) if b is not None]
        reads = [k for k in reads if _bank(k) is None]
        writes = [k for k in writes if _bank(k) is None] + sorted(set(banks))
        deps = []
        for k in reads:
            w = self.last_w.get(k)
            if w is not None:
                deps.append(w)
        for k in writes:
            w = self.last_w.get(k)
            if w is not None:
                deps.append(w)
            deps.extend(self.readers.get(k, ()))
        seen = set()
        dl = []
        for d in deps:
            if id(d) in seen or d is o:
                continue
            seen.add(id(d))
            if (not d.is_dma) and (not dma) and d.eng == "pe" and eng == "pe":
                continue
            dl.append((d, self.lanes[d.lane]["count"] if d.is_dma else None))
        o.deps = dl
        if dma:
            L = self.lanes.setdefault(lane, {"count": 0, "sem": None})
            L["count"] += 16
        for k in writes:
            self.last_w[k] = o
            self.readers[k] = []
        for k in reads:
            if k not in writes:
                self.readers.setdefault(k, []).append(o)
        self.ops[eng].append(o)
        self.all_ops.append(o)
        return o

    def peek_fin(self, reads, writes):
        def _bank(k):
            if k.startswith("pb"):
                return k[:3]
            if k.startswith("tp"):
                return "tp"
            return None
        t = 0.0
        for k in list(reads) + list(writes):
            b = _bank(k)
            kk = b if b is not None else k
            w = self.last_w.get(kk)
            if w is not None and w.fin > t:
                t = w.fin
            if b is not None or k in writes:
                for r in self.readers.get(kk, ()):
                    if r.fin > t:
                        t = r.fin
        return t

    def barrier(self):
        lasts = []
        for e in ENGS:
            for o in reversed(self.ops[e]):
                if not o.is_dma and o.fn is not None:
                    lasts.append(o)
                    break
        lane_last = {}
        for o in self.all_ops:
            if o.is_dma:
                lane_last[o.lane] = o
        lasts.extend(lane_last.values())
        for e in ENGS:
            o = _Op()
            o.eng, o.fn, o.is_dma, o.lane = e, None, False, None
            o.sig, o.sigval = False, None
            o.fin = 0.0
            o.deps = [(d, self.lanes[d.lane]["count"] if d.is_dma else None) for d in lasts
                      if not (d.eng == "pe" and e == "pe" and not d.is_dma)]
            self.ops[e].append(o)
            self.all_ops.append(o)
        self.last_w = {}
        self.readers = {}

    def emit(self):
        nc, es = self.nc, self.es
        for o in self.all_ops:
            for d, _v in o.deps:
                if not d.is_dma:
                    d.sig = True
        for e in ENGS:
            cnt, sem = 0, None
            for o in self.ops[e]:
                if o.is_dma or not o.sig:
                    continue
                if sem is None or cnt >= self.sem_rot:
                    sem = es.enter_context(nc.semaphore(f"s_{e}_{self.nsem}"))
                    self.nsem += 1
                    cnt = 0
                cnt += 1
                o.sigval = (sem, cnt)
        for name, L in self.lanes.items():
            L["sem"] = es.enter_context(nc.semaphore(f"l_{self.nsem}"))
            self.nsem += 1
        block = es.enter_context(nc.Block())
        lanes = self.lanes

        def make(e):
            ops = self.ops[e]

            def body(eng):
                waited = {}
                for o in ops:
                    need = {}
                    for d, dv in o.deps:
                        if d.is_dma:
                            sem, val = lanes[d.lane]["sem"], dv
                        else:
                            sem, val = d.sigval
                        k = id(sem)
                        if k not in need or need[k][1] < val:
                            need[k] = (sem, val)
                    for k, (sem, val) in need.items():
                        if waited.get(k, 0) >= val:
                            continue
                        eng.wait_ge(sem, val)
                        waited[k] = val
                    if o.fn is None:
                        continue
                    ins = o.fn(eng)
                    if o.is_dma:
                        ins.then_inc(lanes[o.lane]["sem"], 16)
                    elif o.sig:
                        ins.then_inc(o.sigval[0], 1)
            return body

        block.tensor(make("pe"))
        block.scalar(make("act"))
        block.vector(make("dve"))
        block.gpsimd(make("pool"))
        block.sync(make("sp"))


class Mem:
    BASE = 16512
    TOP = 229344

    def __init__(self, nc):
        self.nc = nc
        self.off = self.BASE
        self.n = 0
        self.peak = 0

    def alloc(self, shape, dt, name=None):
        nbytes = int(np.prod(shape[1:])) * (4 if dt == F32 else 2)
        self.off = (self.off + 63) // 64 * 64
        off = self.off
        self.off += nbytes
        self.peak = max(self.peak, self.off)
        assert self.off <= self.TOP, f"SBUF overflow: {self.off} > {self.TOP} ({name})"
        self.n += 1
        return self.nc.alloc_sbuf_tensor_at(f"{name or 't'}{self.n}", list(shape), dt, offset=off)

    def mark(self):
        return self.off

    def release(self, m):
        self.off = m


class Prog:
    def __init__(self, nseq, stop_after=None, dbg=False, skip=()):
        self.skip = skip
        self.defer = None
        self.nseq = nseq
        self.stop_after = stop_after
        self.dbg = dbg

    def din(self, name):
        if name not in self._din:
            self._din[name] = self.nc.dram_tensor(name, list(self._dshapes[name]), F32, kind="ExternalInput").ap()
        return self._din[name]

    def rec(self, eng, fn, reads=(), writes=(), cost=0.2, dma=False, lane=None):
        if self.defer is not None and not dma:
            self.defer.append((eng, fn, list(reads), list(writes), cost))
            return
        self.S.op(eng, fn, reads=reads, writes=writes, dma=dma, lane=lane)

    @staticmethod
    def _n(ap):
        n = 1
        for d in ap.shape[1:]:
            n *= int(d)
        return n

    def _ecost(self, eng, out):
        n = self._n(out)
        if eng == "act":
            return n / 1200.0 + 0.25
        if eng == "pool":
            return n / 480.0 + 0.25
        return n / 960.0 + 0.12

    def merge(self, streams):
        S = self.S
        free = {e: 0.0 for e in ENGS}
        idx = [0] * len(streams)
        total = sum(len(st) for st in streams)
        for _ in range(total):
            best, bt = None, None
            for si, st in enumerate(streams):
                if idx[si] >= len(st):
                    continue
                eng, fn, r, w, cost = st[idx[si]]
                t = max(free[eng], S.peek_fin(r, w))
                if bt is None or t < bt - 1e-9:
                    best, bt = si, t
            eng, fn, r, w, cost = streams[best][idx[best]]
            idx[best] += 1
            o = S.op(eng, fn, reads=r, writes=w)
            o.fin = bt + cost
            free[eng] = o.fin
        for st in streams:
            pass
        for o in S.all_ops:
            o.fin = 0.0

    def mm(self, out, lhsT, rhs, start, stop, r, w):
        cost = self._n(rhs) * (4 if lhsT.dtype == F32 else 1) / 2400.0 + 0.03
        self.rec("pe", lambda e: e.matmul(out, lhsT=lhsT, rhs=rhs, start=start, stop=stop,
                                          skip_group_check=True), reads=r, writes=w, cost=cost)

    def tr(self, out, in_, ident, r, w):
        self.rec("pe", lambda e: e.transpose(out=out, in_=in_, identity=ident), reads=r, writes=w, cost=0.11)

    def act(self, out, in_, func, r, w, bias=None, scale=None, accum=None, eng="act"):
        kw = {}
        if bias is not None:
            kw["bias"] = bias
        if scale is not None:
            kw["scale"] = scale
        if accum is not None:
            kw["accum_out"] = accum
        self.rec("act", lambda e: e.activation(out=out, in_=in_, func=func, **kw), reads=r, writes=w,
                 cost=self._ecost("act", out))

    def tt(self, eng, out, in0, in1, op, r, w):
        self.rec(eng, lambda e: e.tensor_tensor(out=out, in0=in0, in1=in1, op=op), reads=r, writes=w,
                 cost=self._ecost(eng, out))

    def ts(self, eng, out, in0, s1, s2, op0, op1, r, w):
        if op1 is None:
            self.rec(eng, lambda e: e.tensor_scalar(out=out, in0=in0, scalar1=s1, scalar2=None, op0=op0),
                     reads=r, writes=w, cost=self._ecost(eng, out))
        else:
            self.rec(eng, lambda e: e.tensor_scalar(out=out, in0=in0, scalar1=s1, scalar2=s2, op0=op0, op1=op1),
                     reads=r, writes=w, cost=self._ecost(eng, out))

    def stt(self, eng, out, in0, scalar, in1, op0, op1, r, w):
        self.rec(eng, lambda e: e.scalar_tensor_tensor(out=out, in0=in0, scalar=scalar, in1=in1, op0=op0, op1=op1),
                 reads=r, writes=w, cost=self._ecost(eng, out))

    def cp(self, eng, out, in_, r, w):
        self.rec(eng, lambda e: e.tensor_copy(out=out, in_=in_), reads=r, writes=w, cost=self._ecost(eng, out))

    def memset(self, eng, ap, val, w):
        self.rec(eng, lambda e: e.memset(ap, val), writes=w, cost=self._ecost(eng, ap))

    def dma(self, eng, out, in_, r, w, lane):
        self.rec(eng, lambda e: e.dma_start(out=out, in_=in_), reads=r, writes=w, dma=True, lane=lane)

    def wload(self, dst, src2d, key, eng="pool"):
        self.dma(eng, dst, src2d.rearrange("(kc p) n -> p kc n", p=128), [], [key], key)

    def build(self):
        nc = bass.Bass("TRN2", target_bir_lowering=False)
        self.nc = nc
        ns = self.nseq
        self._din = {}
        self._dshapes = {
            "x": (ns, SEQ, D), "p": (ns, SEQ, PLE), "w_in": (D, IN_COLS), "w_att": (512, D), "w_ssd": (2048, D),
            "w_out": (D, D), "w_eg": (NEXP, D, 512), "w_eu": (NEXP, D, 512), "w_ed": (NEXP, 512, D),
            "w_pg": (D, D), "w_pp": (PLE, D), "w_r": (D, 20), "gains": (4, D), "vec32": (3, 32), "b_r": (1, 20),
            "convw": (128, 32, 4), "convb": (128, 32), "ng": (128, 16), "c_ident": (128, 128), "c_tri": (128, 128),
            "c_mask": (128, 512), "c_em": (128, 24, 256),
        }
        self.out_d = nc.dram_tensor("out", [ns, SEQ, D], F32, kind="ExternalOutput").ap()
        self.dbg_outs = {}
        with ExitStack() as es:
            self.es = es
            self.S = Sched(nc, es)
            self.M = Mem(nc)
            self.alloc_psum()
            self.setup_consts()
            for s in range(ns):
                self.seq(s)
                if self.stop_after is not None:
                    break
            self.S.barrier()
            self.S.emit()
        return nc

    def alloc_psum(self):
        nc, es = self.nc, self.es
        self.pb = [es.enter_context(nc.psum_tensor(f"pb{i}", [128, 512], F32)) for i in range(7)]
        self.tp = es.enter_context(nc.psum_tensor("tp", [128, 1024], BF16))
        self.tps = [(self.tp, "tp"), (self.pb[3].bitcast(BF16), "pb3")]

    def dbg_dump(self, name, ap, shape, keys, dt=F32):
        d = self.nc.dram_tensor("dbg_" + name, list(shape), dt, kind="ExternalOutput").ap()
        self.dbg_outs[name] = d
        self.dma("sp", d, ap, keys, [], "dbg_" + name)

    def setup_consts(self):
        M = self.M
        S = self.S
        self.identf = M.alloc([128, 128], F32, "identf")
        self.identb = M.alloc([128, 128], BF16, "identb")
        self.tri = M.alloc([128, 128], F32, "tri")
        self.onesf = M.alloc([128, 128], F32, "onesf")
        self.maskb = M.alloc([128, 512], BF16, "maskb")
        self.ddiag = M.alloc([128, 32, 128], BF16, "ddiag")
        self.gbc = M.alloc([128, D], F32, "gbc")
        self.cw = M.alloc([128, 32, 4], F32, "cw")
        self.cb = M.alloc([128, 32], F32, "cb")
        self.ngt = M.alloc([128, 16], F32, "ngt")
        self.v32 = M.alloc([128, 3, 32], F32, "v32")
        self.abc = M.alloc([128, 32], F32, "abc")
        self.brb = M.alloc([128, 20], F32, "brb")
        self.wrf = M.alloc([128, KC, 20], F32, "wrf")
        self.wrhi = M.alloc([128, KC, 20], BF16, "wrhi")
        self.wrlo = M.alloc([128, KC, 20], BF16, "wrlo")
        self.dma("sp", self.identf[:], self.din("c_ident"), [], ["identf"], "c0")
        self.dma("sp", self.tri[:], self.din("c_tri"), [], ["tri"], "c1")
        self.dma("pool", self.maskb[:], self.din("c_mask"), [], ["maskb"], "c2")
        self.dma("sp", self.cw[:], self.din("convw"), [], ["cw"], "c4")
        self.dma("sp", self.cb[:], self.din("convb"), [], ["cb"], "c5")
        self.dma("sp", self.ngt[:], self.din("ng"), [], ["ngt"], "c6")
        self.dma("sp", self.v32[:].rearrange("p a b -> p (a b)"),
                 self.din("vec32").rearrange("a b -> (a b)").partition_broadcast(128), [], ["v32"], "c7")
        self.dma("sp", self.brb[:], self.din("b_r")[0].partition_broadcast(128), [], ["brb"], "c8")
        self.dma("sp", self.wrf[:], self.din("w_r").rearrange("(kc p) n -> p kc n", p=128), [], ["wrf"], "c9")
        self.cp("dve", self.identb[:], self.identf[:], ["identf"], ["identb"])
        self.memset("dve", self.onesf[:], 1.0, ["onesf"])
        self.act(self.abc[:], self.v32[:, 1, :], AF.Exp, ["v32"], ["abc"])
        self.ts("dve", self.abc[:], self.abc[:], -1.0, None, ALU.mult, None, ["abc"], ["abc"])
        for h in range(32):
            self.ts("dve", self.ddiag[:, h, :], self.identf[:], self.v32[:, 2, h:h + 1], None, ALU.mult, None,
                    ["identf", "v32"], ["ddiag"])
        self.cp("dve", self.wrhi[:], self.wrf[:], ["wrf"], ["wrhi"])
        self.tt("dve", self.wrf[:], self.wrf[:], self.wrhi[:], ALU.subtract, ["wrf", "wrhi"], ["wrf"])
        self.cp("dve", self.wrlo[:], self.wrf[:], ["wrf"], ["wrlo"])
        self.const_mark = M.mark()

    def load_gain(self, idx):
        self.dma("sp", self.gbc[:], self.din("gains")[idx].partition_broadcast(128), [], ["gbc"], "gbc")

    def norm_T(self, xr, xkeys, outT, outkey, gidx, lo=None):
        M = self.M
        mk = M.mark()
        ssq = M.alloc([128, NT], F32, "ssq")
        rs = M.alloc([128, NT], F32, "rs")
        junk = M.alloc([128, D], BF16, "junk")
        hn = [M.alloc([128, D], BF16, "hn") for _ in range(2)]
        self.load_gain(gidx)
        self.memset("dve", ssq[:], 0.0, ["ssq"])
        for t in range(NT):
            self.act(junk[:], xr[:, t, :], AF.Square, [xkeys(t), "ssq"], [f"ssq:{t}", "junk"], accum=ssq[:, t:t + 1])
        self.ts("dve", rs[:], ssq[:], 1.0 / D, EPS, ALU.mult, ALU.add, [f"ssq:{t}" for t in range(NT)], ["rs"])
        self.act(rs[:], rs[:], AF.Ln, ["rs"], ["rs"])
        self.act(rs[:], rs[:], AF.Exp, ["rs"], ["rs"], scale=-0.5)
        for t in range(NT):
            h = hn[t % 2]
            hk = f"hn{t % 2}"
            self.stt("dve", h[:], xr[:, t, :], rs[:, t:t + 1], self.gbc[:], ALU.mult, ALU.mult,
                     [xkeys(t), "rs", "gbc"], [hk])
            tpb, tk = self.tps[t % 2]
            for c in range(KC):
                self.tr(tpb[:, c * 128:(c + 1) * 128], h[:, c * 128:(c + 1) * 128], self.identb[:],
                        [hk, "identb"], [tk])
            self.act(outT[:, :, t * 128:(t + 1) * 128], tpb[:, :].rearrange("p (c n) -> p c n", c=KC), AF.Copy,
                     [tk], [f"{outkey}:{t}"])
        M.release(mk)
        return rs

    def seq(self, s):
        M = self.M
        S = self.S
        M.release(self.const_mark)
        self.hT = M.alloc([128, KC, SEQ], BF16, "hT")
        self.big = M.mark()
        xr = M.alloc([128, NT, D], F32, "xr")
        for t in range(NT):
            self.dma("sp", xr[:, t, :], self.din("x")[s, t * 128:(t + 1) * 128, :], [], [f"xr:{t}"], f"xr{t % 4}")
        self.norm_T(xr, lambda t: f"xr:{t}", self.hT, "hT", 0)
        S.barrier()
        if self.stop_after == "norm":
            self.dbg_hT()
            return
        M.release(self.big)
        self.hole = M.mark()
        if self.phase_ssd(s):
            return
        if self.phase_att(s):
            return
        if self.phase_out(s):
            return
        if self.phase_moe(s):
            return
        self.phase_ple(s)

    def dbg_hT(self):
        M = self.M
        tmp = M.alloc([128, KC, SEQ], F32, "dbgt")
        self.cp("dve", tmp[:], self.hT[:], [f"hT:{t}" for t in range(NT)], ["dbgt"])
        self.dbg_dump("hT", tmp[:], (128, KC, SEQ), ["dbgt"])

    def phase_ssd(self, s):
        M, S = self.M, self.S
        hT = self.hT
        hkeys = [f"hT:{t}" for t in range(NT)]
        GT = M.alloc([128, 16, SEQ], BF16, "GT")
        ssq = M.alloc([128, NT, 8], F32, "ssq_ssd")
        scr = M.mark()
        self.mb_start = scr
        if "ssd" in self.skip:
            self.Mb = M.alloc([128, NT, D], BF16, "Mb")
            self.mb_end = M.mark()
            for t in range(NT):
                self.memset("dve", self.Mb[:, t, :], 0.0, [f"Mb:{t}:0", f"Mb:{t}:1"])
            S.barrier()
            return False
        adt = M.alloc([128, NT, 32], F32, "adt")
        cdec = M.alloc([128, NT, 32], F32, "cdec")
        sd = M.alloc([128, NT, 32], F32, "sd")
        biasL = M.alloc([128, NT, 32], F32, "biasL")
        dtmark = M.mark()
        wdt = M.alloc([128, KC, 32], BF16, "wdt")
        spre = M.alloc([128, NT, 32], F32, "spre")
        dtt = M.alloc([128, NT, 32], F32, "dtt")
        lndt = M.alloc([128, NT, 32], F32, "lndt")
        self.wload(wdt[:], self.din("w_in")[:, OFF_DT:OFF_DT + 32], "wdt")
        self.memset("dve", ssq[:], 0.0, ["ssq_ssd"])
        pb = self.pb
        for t in range(NT):
            for kc in range(KC):
                self.mm(pb[0][:, t * 32:(t + 1) * 32], hT[:, kc, t * 128:(t + 1) * 128], wdt[:, kc, :],
                        kc == 0, kc == KC - 1, [hkeys[t], "wdt"], ["pb0"])
        b3 = lambda ap: ap.rearrange("p (t h) -> p t h", h=32)
        self.tt("dve", spre[:], b3(pb[0][:, :]), self.v32[:, 0, :].unsqueeze(1).to_broadcast([128, NT, 32]), ALU.add,
                ["pb0", "v32"], ["spre"])
        self.act(spre[:], spre[:], AF.Exp, ["spre"], ["spre"])
        self.ts("dve", spre[:], spre[:], 1.0, None, ALU.add, None, ["spre"], ["spre"])
        self.act(dtt[:], spre[:], AF.Ln, ["spre"], ["dtt"])
        self.act(lndt[:], dtt[:], AF.Ln, ["dtt"], ["lndt"])
        self.tt("dve", adt[:], dtt[:], self.abc[:].unsqueeze(1).to_broadcast([128, NT, 32]), ALU.mult,
                ["dtt", "abc"], ["adt"])
        for c in range(NT):
            self.mm(pb[1][:, c * 32:(c + 1) * 32], self.tri[:], adt[:, c, :], True, True, ["tri", "adt"], ["pb1"])
            self.mm(pb[2][:, c * 32:(c + 1) * 32], self.onesf[:], adt[:, c, :], True, True, ["onesf", "adt"], ["pb2"])
        self.act(cdec[:], b3(pb[2][:, :]), AF.Exp, ["pb2"], ["cdec"])
        self.act(sd[:], b3(pb[1][:, :]), AF.Exp, ["pb1"], ["sd"])
        self.stt("dve", biasL[:], b3(pb[1][:, :]), -1.0, lndt[:], ALU.mult, ALU.add, ["pb1", "lndt"], ["biasL"])
        if self.stop_after == "dt":
            self.dbg_dump("dtt", dtt[:], (128, NT, 32), ["dtt"])
            self.dbg_dump("biasL", biasL[:], (128, NT, 32), ["biasL"])
            self.dbg_dump("cdec", cdec[:], (128, NT, 32), ["cdec"])
            return True
        S.barrier()
        M.release(dtmark)
        wx = M.alloc([128, KC, 256], BF16, "wx")
        wB = M.alloc([128, KC, 128], BF16, "wB")
        wC = M.alloc([128, KC, 128], BF16, "wC")
        wz = M.alloc([128, KC, 256], BF16, "wz")
        junk = M.alloc([128, 256], BF16, "junk2")
        gb = []
        for gi in range(2):
            gb.append(dict(xT=M.alloc([128, 2, SEQ], BF16, "xT"), BT=M.alloc([128, SEQ], BF16, "BT"),
                           CT=M.alloc([128, SEQ], BF16, "CT"), sz=M.alloc([128, NT, 256], BF16, "sz"),
                           S32=M.alloc([128, 256], F32, "S32"), Sbf=M.alloc([128, 256], BF16, "Sbf")))
        amark = M.mark()
        HS = SEQ // 2
        raws = [M.alloc([128, 3 + HS], F32, "raw") for _ in range(2)]
        caccs = [M.alloc([128, HS], F32, "cacc") for _ in range(2)]
        M.release(amark)
        self._convi = 0
        sets = []
        for par in range(2):
            sets.append(dict(
                E=M.alloc([128, 4, 128], F32, "E"), mixT=M.alloc([128, 4, 128], BF16, "mixT"),
                xB=M.alloc([128, 384], BF16, "xB"), xds=M.alloc([128, 256], BF16, "xds"),
                adtb=M.alloc([128, 4, 128], F32, "adtb"), ysb=M.alloc([128, 256], F32, "ysb"),
                tmp=M.alloc([128, 256], F32, "tmp"), G=M.alloc([128, 256], BF16, "G"),
            ))
        tpB = self.pb[3].bitcast(BF16)
        bankset = [dict(cb=pb[4], R=pb[5], Y=pb[6], tp=self.tp, kcb="pb4", kR="pb5", kY="pb6", ktp="tp"),
                   dict(cb=pb[0], R=pb[1], Y=pb[2], tp=tpB, kcb="pb0", kR="pb1", kY="pb2", ktp="pb3")]

        def inproj(g, gi):
            B = gb[gi]
            sfx = f"{gi}"
            xT, BT, CT, sz = B["xT"], B["BT"], B["CT"], B["sz"]
            self.wload(wx[:], self.din("w_in")[:, OFF_X + 256 * g:OFF_X + 256 * (g + 1)], "wx")
            self.wload(wB[:], self.din("w_in")[:, OFF_B + 128 * g:OFF_B + 128 * (g + 1)], "wB")
            self.wload(wC[:], self.din("w_in")[:, OFF_C + 128 * g:OFF_C + 128 * (g + 1)], "wC")
            self.wload(wz[:], self.din("w_in")[:, OFF_Z + 256 * g:OFF_Z + 256 * (g + 1)], "wz")
            for t in range(NT):
                bank = pb[3 + (t // 2) % 2]
                bk = f"pb{3 + (t // 2) % 2}"
                half = (t % 2) * 256
                for kc in range(KC):
                    self.mm(bank[:, half:half + 256], hT[:, kc, t * 128:(t + 1) * 128], wz[:, kc, :],
                            kc == 0, kc == KC - 1, [hkeys[t], "wz"], [bk])
                if t % 2 == 1:
                    self.act(sz[:, t - 1:t + 1, :], bank[:, :].rearrange("p (a n) -> p a n", a=2), AF.Silu,
                             [bk], ["sz" + sfx])
            specs = [(wx, "wx", 0, 2 * g, xT[:, 0, :], "xT0" + sfx), (wx, "wx", 128, 2 * g + 1, xT[:, 1, :], "xT1" + sfx),
                     (wB, "wB", 0, 16 + g, BT[:, :], "BT" + sfx), (wC, "wC", 0, 24 + g, CT[:, :], "CT" + sfx)]
            units = []
            for (wt, wk, coff, ch, dst, dkey) in specs:
                for half in range(2):
                    ci = self._convi % 2
                    self._convi += 1
                    units.append((wt, wk, coff, ch, dst, dkey, half, ci))

            def stage_a(u):
                (wt, wk, coff, ch, dst, dkey, half, ci) = u
                raw, cacc = raws[ci], caccs[ci]
                rk, ck = f"raw{ci}", f"cacc{ci}"
                if half == 0:
                    self.memset("dve", raw[:, 0:3], 0.0, [rk + "h"])
                else:
                    self.cp("dve", raw[:, 0:3], raws[1 - ci][:, HS:HS + 3], [f"raw{1 - ci}:1"], [rk + "h"])
                for t2 in range(2):
                    tc = half * 2 + t2
                    bank = pb[tc % 2]
                    bk = f"pb{tc % 2}"
                    for kc in range(KC):
                        self.mm(bank[:, :], wt[:, kc, coff:coff + 128], hT[:, kc, tc * 512:(tc + 1) * 512],
                                kc == 0, kc == KC - 1, [wk] + hkeys[tc * 4:tc * 4 + 4], [bk])
                    self.act(raw[:, 3 + t2 * 512:3 + (t2 + 1) * 512], bank[:, :], AF.Copy, [bk], [rk + f":{t2}"])
                rks = [rk + "h", rk + ":0", rk + ":1"]
                self.act(cacc[:], raw[:, 3:3 + HS], AF.Identity, rks + ["cw", "cb"], [ck],
                         bias=self.cb[:, ch:ch + 1], scale=self.cw[:, ch, 3:4])

            def stage_b(u):
                (wt, wk, coff, ch, dst, dkey, half, ci) = u
                raw, cacc = raws[ci], caccs[ci]
                rk, ck = f"raw{ci}", f"cacc{ci}"
                rks = [rk + "h", rk + ":0", rk + ":1"]
                for j in (2, 1, 0):
                    self.stt("dve", cacc[:], raw[:, j:j + HS], self.cw[:, ch, j:j + 1], cacc[:], ALU.mult, ALU.add,
                             rks + ["cw", ck], [ck])
                self.act(dst[:, half * HS:(half + 1) * HS], cacc[:], AF.Silu, [ck], [dkey])

            for i in range(len(units) + 1):
                if i < len(units):
                    stage_a(units[i])
                if i >= 1:
                    stage_b(units[i - 1])

        def chunk(c, g, gi):
            B = gb[gi]
            st = sets[gi]
            bs = bankset[gi]
            sfx = f"{gi}"
            xT, BT, CT, sz, S32, Sbf = B["xT"], B["BT"], B["CT"], B["sz"], B["S32"], B["Sbf"]
            b_cb, b_R, b_Y, tpb = bs["cb"], bs["R"], bs["Y"], bs["tp"]
            k_cb, k_R, k_Y, tk = bs["kcb"], bs["kR"], bs["kY"], bs["ktp"]
            cols = slice(c * 128, (c + 1) * 128)
            self.tr(tpb[:, 0:128], xT[:, 0, cols], self.identb[:], ["xT0" + sfx, "identb"], [tk])
            self.tr(tpb[:, 128:256], xT[:, 1, cols], self.identb[:], ["xT1" + sfx, "identb"], [tk])
            self.tr(tpb[:, 256:384], BT[:, cols], self.identb[:], ["BT" + sfx, "identb"], [tk])
            self.act(st["xB"][:], tpb[:, 0:384], AF.Copy, [tk], ["xB" + sfx])
            self.mm(b_cb[:, 0:128], BT[:, cols], CT[:, cols], True, True, ["BT" + sfx, "CT" + sfx], [k_cb])
            self.cp("pool", st["adtb"][:], adt[:, c, 4 * g:4 * g + 4].unsqueeze(2).to_broadcast([128, 4, 128]),
                    ["adt"], ["adtb" + sfx])
            self.mm(b_R[:, :], self.identb[:], self.maskb[:], True, False, ["identb", "maskb"], [k_R])
            for j in range(4):
                self.mm(b_R[:, j * 128:(j + 1) * 128], st["adtb"][:, j, :], self.tri[:], False, True,
                        ["adtb" + sfx, "tri"], [k_R])
            for j in range(4):
                self.act(st["E"][:, j, :], b_R[:, j * 128:(j + 1) * 128], AF.Exp, [k_R, "biasL"], [f"E{sfx}:{j}"],
                         bias=biasL[:, c, 4 * g + j:4 * g + j + 1])
            ek = [f"E{sfx}:{j}" for j in range(4)]
            self.tt("dve", st["mixT"][:], b_cb[:, 0:128].unsqueeze(1).to_broadcast([128, 4, 128]), st["E"][:],
                    ALU.mult, [k_cb] + ek, ["mixT" + sfx])
            x4 = st["xB"][:, 0:256].rearrange("p (j d) -> p j d", d=64)
            if c < NT - 1:
                self.tt("pool", st["xds"][:].rearrange("p (j d) -> p j d", d=64), x4,
                        st["E"][:, :, 127:128].to_broadcast([128, 4, 64]), ALU.mult,
                        ["xB" + sfx] + ek, ["xds" + sfx])
            for j in range(4):
                self.mm(b_Y[:, j * 64:(j + 1) * 64], st["mixT"][:, j, :], st["xB"][:, j * 64:(j + 1) * 64],
                        True, False, ["mixT" + sfx, "xB" + sfx], [k_Y])
                self.mm(b_Y[:, j * 64:(j + 1) * 64], self.ddiag[:, 4 * g + j, :], st["xB"][:, j * 64:(j + 1) * 64],
                        False, True, ["ddiag", "xB" + sfx], [k_Y])
            if c > 0:
                self.mm(b_Y[:, 256:512], CT[:, cols], Sbf[:], True, True, ["CT" + sfx, "Sbf" + sfx], [k_Y])
            if c < NT - 1:
                self.mm(b_cb[:, 128:384], st["xB"][:, 256:384], st["xds"][:], True, True,
                        ["xB" + sfx, "xds" + sfx], [k_cb])
            if c > 0:
                self.tt("dve", st["tmp"][:].rearrange("p (j d) -> p j d", d=64),
                        b_Y[:, 256:512].rearrange("p (j d) -> p j d", d=64),
                        sd[:, c, 4 * g:4 * g + 4].unsqueeze(2).to_broadcast([128, 4, 64]), ALU.mult,
                        [k_Y, "sd"], ["tmp" + sfx])
                self.tt("dve", st["ysb"][:], b_Y[:, 0:256], st["tmp"][:], ALU.add, [k_Y, "tmp" + sfx],
                        ["ysb" + sfx])
            else:
                self.cp("dve", st["ysb"][:], b_Y[:, 0:256], [k_Y], ["ysb" + sfx])
            self.tt("pool", st["G"][:], st["ysb"][:], sz[:, c, :], ALU.mult, ["ysb" + sfx, "sz" + sfx], ["G" + sfx])
            self.act(junk[:], st["G"][:], AF.Square, ["G" + sfx, "ssq_ssd"], [f"ssqs:{c}:{g}", "junk2"],
                     accum=ssq[:, c, g:g + 1])
            self.tr(tpb[:, 384:512], st["G"][:, 0:128], self.identb[:], ["G" + sfx, "identb"], [tk])
            self.tr(tpb[:, 512:640], st["G"][:, 128:256], self.identb[:], ["G" + sfx, "identb"], [tk])
            self.act(GT[:, 2 * g, cols], tpb[:, 384:512], AF.Copy, [tk, "ngt"], [f"GT:{c}"],
                     scale=self.ngt[:, 2 * g:2 * g + 1])
            self.ts("dve", GT[:, 2 * g + 1, cols], tpb[:, 512:640], self.ngt[:, 2 * g + 1:2 * g + 2], None, ALU.mult, None,
                    [tk, "ngt"], [f"GT:{c}"])
            if c < NT - 1:
                if c == 0:
                    self.cp("dve", S32[:], b_cb[:, 128:384], [k_cb], ["S32" + sfx])
                else:
                    self.tt("dve", S32[:].rearrange("p (j d) -> p j d", d=64),
                            S32[:].rearrange("p (j d) -> p j d", d=64),
                            cdec[:, c, 4 * g:4 * g + 4].unsqueeze(2).to_broadcast([128, 4, 64]), ALU.mult,
                            ["S32" + sfx, "cdec"], ["S32" + sfx])
                    self.tt("dve", S32[:], b_cb[:, 128:384], S32[:], ALU.add, [k_cb, "S32" + sfx], ["S32" + sfx])
                self.act(Sbf[:], S32[:], AF.Copy, ["S32" + sfx], ["Sbf" + sfx])

        for gp in range(4):
            for gi in range(2):
                inproj(2 * gp + gi, gi)
            S.barrier()
            streams = []
            for gi in range(2):
                self.defer = []
                for c in range(NT):
                    chunk(c, 2 * gp + gi, gi)
                streams.append(self.defer)
                self.defer = None
            self.merge(streams)
            S.barrier()
        if self.stop_after == "ssd":
            S.barrier()
            M.release(scr)
            tmpf = M.alloc([128, 2, SEQ], F32, "dbgg")
            self.cp("dve", tmpf[:], GT[:, 0:2, :], [f"GT:{c}" for c in range(NT)], ["dbgg"])
            self.dbg_dump("GT", tmpf[:], (128, 2, SEQ), ["dbgg"])
            self.dbg_dump("ssq", ssq[:], (128, NT, 8), ["ssq_ssd"])
            return True
        S.barrier()
        M.release(scr)
        self.Mb = M.alloc([128, NT, D], BF16, "Mb")
        self.mb_end = M.mark()
        Mb = self.Mb
        red = M.alloc([128, NT], F32, "red")
        rs = M.alloc([128, NT], F32, "rs_ssd")
        wssd = M.alloc([128, 16, D], BF16, "wssd")
        wgs = M.alloc([128, KC, D], BF16, "wgs")
        sgt1 = M.alloc([128, 512], BF16, "sgt")
        sgt = [sgt1, sgt1]
        self.wload(wssd[:], self.din("w_ssd")[:, :], "wssd")
        self.wload(wgs[:], self.din("w_in")[:, OFF_GS:OFF_GS + D], "wgs")
        self.S.op("dve", lambda e: e.tensor_reduce(out=red[:], in_=ssq[:], axis=AX.X, op=ALU.add),
                  reads=["ssq_ssd"] + [f"ssqs:{c}:{g}" for c in range(NT) for g in range(8)], writes=["red"])
        self.ts("dve", rs[:], red[:], 1.0 / 2048, EPS, ALU.mult, ALU.add, ["red"], ["rs_ssd"])
        self.act(rs[:], rs[:], AF.Ln, ["rs_ssd"], ["rs_ssd"])
        self.act(rs[:], rs[:], AF.Exp, ["rs_ssd"], ["rs_ssd"], scale=-0.5)
        i = 0
        for t in range(NT):
            tcols = slice(t * 128, (t + 1) * 128)
            for half in range(2):
                hc = slice(half * 512, (half + 1) * 512)
                by, bg = pb[i % 2], pb[2 + i % 2]
                ky, kg = f"pb{i % 2}", f"pb{2 + i % 2}"
                for cc in range(16):
                    self.mm(by[:, :], GT[:, cc, tcols], wssd[:, cc, hc], cc == 0, cc == 15, [f"GT:{t}", "wssd"], [ky])
                for kc in range(KC):
                    self.mm(bg[:, :], hT[:, kc, tcols], wgs[:, kc, hc], kc == 0, kc == KC - 1, [hkeys[t], "wgs"], [kg])
                self.act(sgt[i % 2][:], bg[:, :], AF.Sigmoid, [kg], ["sgt"])
                self.stt("dve", Mb[:, t, hc], by[:, :], rs[:, t:t + 1], sgt[i % 2][:], ALU.mult, ALU.mult,
                         [ky, "rs_ssd", "sgt"], [f"Mb:{t}:{half}"])
                i += 1
        if self.stop_after == "ssdtail":
            S.barrier()
            M.release(self.hole)
            tmpf = M.alloc([128, NT, D], F32, "dbgm")
            self.cp("dve", tmpf[:], Mb[:], [f"Mb:{t}:{h}" for t in range(NT) for h in range(2)], ["dbgm"])
            self.dbg_dump("Mb", tmpf[:], (128, NT, D), ["dbgm"])
            return True
        S.barrier()
        M.release(self.mb_end)
        return False

    def phase_att(self, s):
        M, S = self.M, self.S
        hT, Mb, pb = self.hT, self.Mb, self.pb
        hkeys = [f"hT:{t}" for t in range(NT)]
        M.release(self.hole)
        attT = M.alloc([64, 8, SEQ], BF16, "attT")
        acc = M.alloc([65, 2, SEQ], F32, "acc")
        v_sb = M.alloc([128, 16, 2, 80], BF16, "v_sb")
        pexp = [M.alloc([128, 2, 256], BF16, "pexp") for _ in range(2)]
        pm = [M.alloc([128, 2, 256], BF16, "pm") for _ in range(2)]
        assert M.mark() <= self.mb_start
        M.release(self.mb_end)
        ems = [M.alloc([128, 2, 256], BF16, "em") for _ in range(2)]
        qT = [M.alloc([128, SEQ], BF16, "qT") for _ in range(2)]
        kT = [M.alloc([128, SEQ], BF16, "kT") for _ in range(2)]
        vT = [M.alloc([128, SEQ], BF16, "vT") for _ in range(2)]
        accs = [acc, M.alloc([65, 2, SEQ], F32, "acc")]
        v_sb2 = M.alloc([128, 16, 2, 80], BF16, "v_sb")
        vsb = [v_sb, v_sb2]
        wq = [M.alloc([128, KC, 128], BF16, "wq") for _ in range(2)]
        wk = [M.alloc([128, KC, 128], BF16, "wk") for _ in range(2)]
        wv = [M.alloc([128, KC, 128], BF16, "wv") for _ in range(2)]
        self.memset("dve", vsb[0][:], 1.0, ["v_sb0"])
        self.memset("dve", vsb[1][:], 1.0, ["v_sb1"])
        sbanks = (((pb[4], "pb4"), (pb[5], "pb5")), ((pb[0], "pb0"), (pb[1], "pb1")))
        obanks1 = ((pb[6], "pb6"), (pb[3], "pb3"))
        units = [(hp, g) for hp in range(4) for g in range(3)]

        def proj(ui):
            hp, g = units[ui]
            st = ui % 2
            d = ATT_PAT[g][1]
            c0 = g * 512 + hp * 128
            self.wload(wq[st][:], self.din("w_in")[:, OFF_Q + c0:OFF_Q + c0 + 128], f"wq{st}")
            self.wload(wk[st][:], self.din("w_in")[:, OFF_K + c0:OFF_K + c0 + 128], f"wk{st}")
            self.wload(wv[st][:], self.din("w_in")[:, OFF_V + c0:OFF_V + c0 + 128], f"wv{st}")
            self.dma("pool", ems[st][:], self.din("c_em")[:, g * 8 + hp * 2:g * 8 + hp * 2 + 2, :], [], [f"em{st}"], f"em{st}")
            i = 0
            for (wt, wkey, dst, dkey) in ((wq[st], f"wq{st}", qT[st], f"qT{st}"), (wk[st], f"wk{st}", kT[st], f"kT{st}"),
                                          (wv[st], f"wv{st}", vT[st], f"vT{st}")):
                for tc in range(4):
                    bank, bk = pb[2], "pb2"
                    i += 1
                    for kc in range(KC):
                        self.mm(bank[:, :], wt[:, kc, :], hT[:, kc, tc * 512:(tc + 1) * 512], kc == 0, kc == KC - 1,
                                [wkey] + hkeys[tc * 4:tc * 4 + 4], [bk])
                    dv = dst[:, :].rearrange("p (r m) -> p r m", r=d)[:, :, tc * 512 // d:(tc + 1) * 512 // d]
                    sv = bank[:, :].rearrange("p (m r) -> p r m", r=d)
                    self.act(dv, sv, AF.Copy, [bk], [dkey])
            for b4 in range(4):
                for bb in range(4):
                    blk = b4 * 4 + bb
                    self.tr(self.tp[:, bb * 128:(bb + 1) * 128], vT[st][:, blk * 128:(blk + 1) * 128], self.identb[:],
                            [f"vT{st}", "identb"], ["tp"])
                self.act(vsb[st][:, b4 * 4:(b4 + 1) * 4, :, 0:64],
                         self.tp[:, 0:512].rearrange("p (b h d) -> p b h d", b=4, h=2), AF.Copy, ["tp"], [f"v_sb{st}"])

        def blocks(ui, b4, sidx):
            hp, g = units[ui]
            st = ui % 2
            d = ATT_PAT[g][1]
            nb = (SEQ // d) // 128
            acc = accs[hp % 2]
            ap_ = f"{hp % 2}"
            for bb in range(4):
                blk = b4 * 4 + bb
                n = blk % nb
                kts = [0, 1] if n > 0 else [1]
                lo = kts[0] * 128
                for hh in range(2):
                    sbank, sk = sbanks[sidx][hh]
                    hs = slice(64 * hh, 64 * hh + 64)
                    for kt in kts:
                        kc0 = (blk - 1 + kt) * 128
                        self.mm(sbank[:, kt * 128:(kt + 1) * 128],
                                kT[st][hs, kc0:kc0 + 128], qT[st][hs, blk * 128:(blk + 1) * 128], True, True,
                                [f"kT{st}", f"qT{st}"], [sk])
                for hh in range(2):
                    sbank, sk = sbanks[sidx][hh]
                    self.act(pexp[sidx][:, hh, lo:256], sbank[:, lo:256], AF.Exp,
                             [sk], [f"pexp{sidx}:{hh}"], scale=0.125)
                self.tt("dve", pm[sidx][:, :, lo:256], pexp[sidx][:, :, lo:256],
                        ems[st][:, :, lo:256], ALU.mult,
                        [f"pexp{sidx}:0", f"pexp{sidx}:1", f"em{st}"], [f"pm{sidx}"])
                ob, ok_ = obanks1[sidx]
                bbl = blk % 2
                for hh in range(2):
                    co = hh * 256 + bbl * 128
                    for kt in kts:
                        self.mm(ob[0:65, co:co + 128], vsb[st][:, blk - 1 + kt, hh, 0:65],
                                pm[sidx][:, hh, kt * 128:(kt + 1) * 128], kt == kts[0], kt == 1,
                                [f"v_sb{st}", f"pm{sidx}"], [ok_])
                if bbl == 1:
                    b2 = blk // 2
                    for hh in range(2):
                        sv = ob[0:65, hh * 256:(hh + 1) * 256]
                        if g == 0:
                            dv = acc[0:65, hh, b2 * 256:(b2 + 1) * 256]
                        elif g == 1:
                            r_, n0 = b2 // 2, (b2 % 2) * 2
                            a0 = r_ + 512 * n0
                            dv = acc[0:65, hh, a0:a0 + 255 * 4 + 1:4]
                        else:
                            dv = acc[0:65, hh, :].rearrange("p (i r) -> p r i", r=16)[:, 2 * b2:2 * b2 + 2, :]
                            sv = sv.rearrange("p (r i) -> p r i", r=2)
                        if g == 0:
                            self.cp("dve", dv, sv, [ok_], [f"acc{ap_}{hh}:{b2}"])
                        else:
                            aks = [f"acc{ap_}{hh}:{b}" for b in range(8)]
                            self.tt("dve", dv, sv, dv, ALU.add, [ok_] + aks, aks)

        def normalise(hp):
            acc = accs[hp % 2]
            for hh in range(2):
                aks = [f"acc{hp % 2}{hh}:{b}" for b in range(8)]
                self.rec("dve", lambda e, hh=hh: e.reciprocal(out=acc[64:65, hh, :], in_=acc[64:65, hh, :]),
                         reads=aks, writes=aks, cost=2.3)
                for tc in range(4):
                    tcs = slice(tc * 512, (tc + 1) * 512)
                    self.mm(pb[6][0:64, :], self.onesf[64:65, 0:64], acc[64:65, hh, tcs], True, True,
                            ["onesf"] + aks, ["pb6"])
                    self.tt("dve", attT[0:64, 2 * hp + hh, tcs], pb[6][0:64, :], acc[0:64, hh, tcs], ALU.mult,
                            ["pb6"] + aks, ["attT"])

        proj(0)
        for ui in range(len(units)):
            hp, g = units[ui]
            streams = []
            for sidx in range(2):
                self.defer = []
                if sidx == 0 and g == 0 and hp > 0:
                    normalise(hp - 1)
                for b4 in (sidx, sidx + 2):
                    blocks(ui, b4, sidx)
                streams.append(self.defer)
            if ui + 1 < len(units):
                self.defer = []
                proj(ui + 1)
                streams.append(self.defer)
            self.defer = None
            self.merge(streams)
        normalise(3)
        if self.stop_after == "att":
            S.barrier()
            M.release(self.mb_end)
            tmpf = M.alloc([64, 4, SEQ], F32, "dbga")
            self.cp("dve", tmpf[:], attT[:, 0:4, :], ["attT"], ["dbga"])
            self.dbg_dump("attT", tmpf[:], (64, 4, SEQ), ["dbga"])
            return True
        S.barrier()
        M.release(self.mb_end)
        watt = M.alloc([64, 8, D], BF16, "watt")
        wga = M.alloc([128, KC, D], BF16, "wga")
        sgt = [M.alloc([128, 512], F32, "sgt") for _ in range(2)]
        tmp = [M.alloc([128, 512], F32, "atmp") for _ in range(2)]
        self.dma("pool", watt[:], self.din("w_att").rearrange("(h d) n -> d h n", d=64), [], ["watt"], "watt")
        self.wload(wga[:], self.din("w_in")[:, OFF_GA:OFF_GA + D], "wga")
        i = 0
        for t in range(NT):
            tcols = slice(t * 128, (t + 1) * 128)
            for half in range(2):
                hc = slice(half * 512, (half + 1) * 512)
                by, bg = pb[i % 2], pb[2 + i % 2]
                ky, kg = f"pb{i % 2}", f"pb{2 + i % 2}"
                for h in range(8):
                    self.mm(by[:, :], attT[0:64, h, tcols], watt[0:64, h, hc], h == 0, h == 7, ["attT", "watt"], [ky])
                for kc in range(KC):
                    self.mm(bg[:, :], hT[:, kc, tcols], wga[:, kc, hc], kc == 0, kc == KC - 1, [hkeys[t], "wga"], [kg])
                self.act(sgt[i % 2][:], bg[:, :], AF.Sigmoid, [kg], [f"sgt{i % 2}"])
                self.tt("dve", tmp[i % 2][:], by[:, :], sgt[i % 2][:], ALU.mult, [ky, f"sgt{i % 2}"], [f"atmp{i % 2}"])
                self.tt("pool", Mb[:, t, hc], Mb[:, t, hc], tmp[i % 2][:], ALU.add, [f"Mb:{t}:{half}", f"atmp{i % 2}"],
                        [f"Mb:{t}:{half}"])
                i += 1
        if self.stop_after == "merged":
            S.barrier()
            M.release(self.hole)
            tmpf = M.alloc([128, NT, D], F32, "dbgm")
            self.cp("dve", tmpf[:], Mb[:], [f"Mb:{t}:{h}" for t in range(NT) for h in range(2)], ["dbgm"])
            self.dbg_dump("Mb", tmpf[:], (128, NT, D), ["dbgm"])
            return True
        S.barrier()
        M.release(self.mb_end)
        return False

    def phase_out(self, s):
        M, S = self.M, self.S
        Mb, pb = self.Mb, self.pb
        M.release(self.hole)
        self.x1 = M.alloc([128, NT, D], F32, "x1")
        assert M.mark() <= self.mb_start
        M.release(self.mb_end)
        x1 = self.x1
        wout = M.alloc([128, KC, D], BF16, "wout")
        mT = [M.alloc([128, KC, 128], BF16, "mT") for _ in range(2)]
        self.wload(wout[:], self.din("w_out")[:, :], "wout")
        for t in range(NT):
            self.dma("sp", x1[:, t, :], self.din("x")[s, t * 128:(t + 1) * 128, :], [], [f"x1:{t}:0", f"x1:{t}:1"], f"xr{t % 4}")
        i = 0
        for t in range(NT):
            par = t % 2
            tpb, tk = self.tps[t % 2]
            for kc in range(KC):
                self.tr(tpb[:, kc * 128:(kc + 1) * 128], Mb[:, t, kc * 128:(kc + 1) * 128], self.identb[:],
                        [f"Mb:{t}:{kc // 4}", "identb"], [tk])
            self.act(mT[par][:], tpb[:, :].rearrange("p (c n) -> p c n", c=KC), AF.Copy, [tk], [f"mT{par}"])
            for half in range(2):
                hc = slice(half * 512, (half + 1) * 512)
                bank, bk = pb[i % 3], f"pb{i % 3}"
                for kc in range(KC):
                    self.mm(bank[:, :], mT[par][:, kc, :], wout[:, kc, hc], kc == 0, kc == KC - 1, [f"mT{par}", "wout"], [bk])
                self.tt("dve", x1[:, t, hc], bank[:, :], x1[:, t, hc], ALU.add, [bk, f"x1:{t}:{half}"], [f"x1:{t}:{half}"])
                i += 1
        if self.stop_after == "x1":
            self.dbg_dump("x1", x1[:], (128, NT, D), [f"x1:{t}:{h}" for t in range(NT) for h in range(2)])
            return True
        S.barrier()
        return False

    def phase_moe(self, s):
        M, S = self.M, self.S
        pb, x1, hT = self.pb, self.x1, self.hT
        xk = lambda t: f"x1:{t}:0"
        M.release(self.mb_start)
        wset = [dict(g=M.alloc([128, KC, 512], BF16, "wg"), u=M.alloc([128, KC, 512], BF16, "wu"),
                     d=M.alloc([128, 4, D], BF16, "wd"))]
        assert M.mark() <= self.mb_end
        M.release(self.mb_end)
        comb = M.alloc([128, NT, 16], F32, "comb")
        mk = M.mark()
        ssq = M.alloc([128, NT], F32, "ssq")
        rs = M.alloc([128, NT], F32, "rs")
        junk = M.alloc([128, D], BF16, "junk")
        hnf = M.alloc([128, D], F32, "hnf")
        hi = [M.alloc([128, D], BF16, "hi") for _ in range(2)]
        lo = [M.alloc([128, D], BF16, "lo") for _ in range(2)]
        loT = [M.alloc([128, KC, 128], BF16, "loT") for _ in range(2)]
        lg = M.alloc([128, NT, 20], F32, "lg")
        sm = M.alloc([128, NT, 64], F32, "smalls")
        self.load_gain(1)
        self.memset("dve", ssq[:], 0.0, ["ssq"])
        for t in range(NT):
            self.act(junk[:], x1[:, t, :], AF.Square, [xk(t), f"x1:{t}:1", "ssq"], [f"ssq:{t}", "junk"], accum=ssq[:, t:t + 1])
        self.ts("dve", rs[:], ssq[:], 1.0 / D, EPS, ALU.mult, ALU.add, [f"ssq:{t}" for t in range(NT)], ["rs"])
        self.act(rs[:], rs[:], AF.Ln, ["rs"], ["rs"])
        self.act(rs[:], rs[:], AF.Exp, ["rs"], ["rs"], scale=-0.5)
        for t in range(NT):
            par = t % 2
            tcols = slice(t * 128, (t + 1) * 128)
            self.stt("dve", hnf[:], x1[:, t, :], rs[:, t:t + 1], self.gbc[:], ALU.mult, ALU.mult,
                     [xk(t), f"x1:{t}:1", "rs", "gbc"], ["hnf"])
            self.cp("dve", hi[par][:], hnf[:], ["hnf"], [f"hi{par}"])
            self.tt("dve", lo[par][:], hnf[:], hi[par][:], ALU.subtract, ["hnf", f"hi{par}"], [f"lo{par}"])
            for c in range(KC):
                self.tr(self.tp[:, c * 128:(c + 1) * 128], hi[par][:, c * 128:(c + 1) * 128], self.identb[:],
                        [f"hi{par}", "identb"], ["tp"])
            self.act(hT[:, :, tcols], self.tp[:, :].rearrange("p (c n) -> p c n", c=KC), AF.Copy, ["tp"], [f"hT:{t}"])
            tpb, tk = self.tps[1]
            for c in range(KC):
                self.tr(tpb[:, c * 128:(c + 1) * 128], lo[par][:, c * 128:(c + 1) * 128], self.identb[:],
                        [f"lo{par}", "identb"], [tk])
            self.act(loT[par][:], tpb[:, :].rearrange("p (c n) -> p c n", c=KC), AF.Copy, [tk], [f"loT{par}"])
            n = 0
            for kc in range(KC):
                for (a, ak, w, wk_) in ((hT[:, kc, tcols], f"hT:{t}", self.wrhi, "wrhi"),
                                        (hT[:, kc, tcols], f"hT:{t}", self.wrlo, "wrlo"),
                                        (loT[par][:, kc, :], f"loT{par}", self.wrhi, "wrhi")):
                    self.mm(pb[6][:, 0:20], a, w[:, kc, :], n == 0, n == 3 * KC - 1, [ak, wk_], ["pb6"])
                    n += 1
            self.tt("dve", lg[:, t, :], pb[6][:, 0:20], self.brb[:], ALU.add, ["pb6", "brb"], [f"lg:{t}"])
        self.route(lg, sm, comb)
        if self.stop_after == "route":
            self.dbg_dump("comb", comb[:], (128, NT, 16), [f"comb:{t}" for t in range(NT)])
            return True
        S.barrier()
        M.release(mk)
        wset.append(dict(g=M.alloc([128, KC, 512], BF16, "wg"), u=M.alloc([128, KC, 512], BF16, "wu"),
                         d=M.alloc([128, 4, D], BF16, "wd")))
        hid = [M.alloc([128, 4, 512], BF16, "hid") for _ in range(2)]
        sg = [M.alloc([128, 512], BF16, "sg") for _ in range(2)]
        units = [(e, tc) for e in range(NEXP) for tc in range(4)]
        cnt = {"d": 0}

        def stage_gu(u, ui):
            e, tc = u
            ws = wset[e % 2]
            sfx = f"{e % 2}"
            if tc == 0:
                self.wload(ws["g"][:], self.din("w_eg")[e], "wg" + sfx)
                self.wload(ws["u"][:], self.din("w_eu")[e], "wu" + sfx)
                self.wload(ws["d"][:], self.din("w_ed")[e], "wd" + sfx)
            hp_ = ui % 2
            tcs = slice(tc * 512, (tc + 1) * 512)
            hk = [f"hT:{t}" for t in range(tc * 4, tc * 4 + 4)]
            for fc in range(4):
                bg, bu = pb[(2 * fc) % 4], pb[(2 * fc + 1) % 4]
                kg, ku = f"pb{(2 * fc) % 4}", f"pb{(2 * fc + 1) % 4}"
                for kc in range(KC):
                    self.mm(bg[:, :], ws["g"][:, kc, fc * 128:(fc + 1) * 128], hT[:, kc, tcs], kc == 0, kc == KC - 1,
                            ["wg" + sfx] + hk, [kg])
                for kc in range(KC):
                    self.mm(bu[:, :], ws["u"][:, kc, fc * 128:(fc + 1) * 128], hT[:, kc, tcs], kc == 0, kc == KC - 1,
                            ["wu" + sfx] + hk, [ku])
                self.act(sg[fc % 2][:], bg[:, :], AF.Silu, [kg], [f"sg{fc % 2}"])
                self.tt("dve", hid[hp_][:, fc, :], bu[:, :], sg[fc % 2][:], ALU.mult, [ku, f"sg{fc % 2}"],
                        [f"hid{hp_}:{fc}"])

        def stage_d(u, ui):
            e, tc = u
            ws = wset[e % 2]
            sfx = f"{e % 2}"
            hp_ = ui % 2
            for tt_ in range(4):
                t = tc * 4 + tt_
                for half in range(2):
                    hc = slice(half * 512, (half + 1) * 512)
                    i = cnt["d"]
                    cnt["d"] += 1
                    bank, bk = pb[4 + i % 3], f"pb{4 + i % 3}"
                    for fc in range(4):
                        self.mm(bank[:, :], hid[hp_][:, fc, tt_ * 128:(tt_ + 1) * 128], ws["d"][:, fc, hc], fc == 0, fc == 3,
                                [f"hid{hp_}:{fc}", "wd" + sfx], [bk])
                    self.stt("dve", x1[:, t, hc], bank[:, :], comb[:, t, e:e + 1], x1[:, t, hc], ALU.mult, ALU.add,
                             [bk, f"comb:{t}", f"x1:{t}:{half}"], [f"x1:{t}:{half}"])

        for i in range(len(units) + 1):
            if i < len(units):
                stage_gu(units[i], i)
            if i >= 1:
                stage_d(units[i - 1], i - 1)
        if self.stop_after == "x2":
            self.dbg_dump("x2", x1[:], (128, NT, D), [f"x1:{t}:{h}" for t in range(NT) for h in range(2)])
            return True
        S.barrier()
        return False

    def route(self, lg, sm, comb):
        T = NT
        k = lambda n: f"sm:{n}"
        lgk = [f"lg:{t}" for t in range(T)]
        gl = lg[:, :, 0:4]
        el4 = lg[:, :, 4:20].rearrange("p t (g e) -> p t g e", g=4)
        col = lambda a, b: sm[:, :, a:b]
        gmax, gsum, gval, m1, m2, d12, w1, w2 = (sm[:, :, i] for i in range(8))
        ohg, ge, within, mask1 = col(8, 12), col(12, 16), col(16, 20), col(20, 24)
        w2in, mask2, ew, ewg = col(24, 28), col(28, 32), col(32, 36), col(36, 40)
        tmp16 = col(40, 56)
        b3 = lambda v: v.unsqueeze(2).to_broadcast([128, T, 4])
        op = self.S.op
        op("dve", lambda e: e.reduce_max(out=gmax, in_=gl, axis=AX.X), reads=lgk, writes=[k("gmax")])
        self.tt("dve", ohg, gl, b3(gmax), ALU.is_equal, lgk + [k("gmax")], [k("ohg")])
        self.tt("dve", ge, gl, b3(gmax), ALU.subtract, lgk + [k("gmax")], [k("ge")])
        self.act(ge, ge, AF.Exp, [k("ge")], [k("ge")])
        op("dve", lambda e: e.reduce_sum(out=gsum, in_=ge, axis=AX.X), reads=[k("ge")], writes=[k("gsum")])
        op("dve", lambda e: e.reciprocal(out=gval, in_=gsum), reads=[k("gsum")], writes=[k("gval")])
        t4 = tmp16.rearrange("p t (g e) -> p t g e", g=4)
        self.tt("dve", t4, el4, ohg.unsqueeze(3).to_broadcast([128, T, 4, 4]), ALU.mult, lgk + [k("ohg")], [k("tmp16")])
        op("dve", lambda e: e.tensor_reduce(out=within, in_=tmp16.rearrange("p t (g e) -> p t e g", g=4), axis=AX.X,
                                            op=ALU.add), reads=[k("tmp16")], writes=[k("within")])
        op("dve", lambda e: e.reduce_max(out=m1, in_=within, axis=AX.X), reads=[k("within")], writes=[k("m1")])
        self.tt("dve", mask1, within, b3(m1), ALU.is_equal, [k("within"), k("m1")], [k("mask1")])
        self.stt("dve", w2in, mask1, -1e30, within, ALU.mult, ALU.add, [k("mask1"), k("within")], [k("w2in")])
        op("dve", lambda e: e.reduce_max(out=m2, in_=w2in, axis=AX.X), reads=[k("w2in")], writes=[k("m2")])
        self.tt("dve", mask2, w2in, b3(m2), ALU.is_equal, [k("w2in"), k("m2")], [k("mask2")])
        self.tt("dve", d12, m2, m1, ALU.subtract, [k("m1"), k("m2")], [k("d12")])
        self.act(d12, d12, AF.Exp, [k("d12")], [k("d12")])
        self.ts("dve", d12, d12, 1.0, None, ALU.add, None, [k("d12")], [k("d12")])
        op("dve", lambda e: e.reciprocal(out=w1, in_=d12), reads=[k("d12")], writes=[k("w1")])
        self.ts("dve", w2, w1, -1.0, 1.0, ALU.mult, ALU.add, [k("w1")], [k("w2")])
        self.tt("dve", ew, mask1, b3(w1), ALU.mult, [k("mask1"), k("w1")], [k("ew")])
        self.tt("dve", ewg, mask2, b3(w2), ALU.mult, [k("mask2"), k("w2")], [k("ewg")])
        self.tt("dve", ew, ew, ewg, ALU.add, [k("ew"), k("ewg")], [k("ew")])
        self.tt("dve", ewg, ew, b3(gval), ALU.mult, [k("ew"), k("gval")], [k("ewg")])
        self.tt("dve", comb[:, :, :].rearrange("p t (g e) -> p t g e", g=4),
                ohg.unsqueeze(3).to_broadcast([128, T, 4, 4]), ewg.unsqueeze(2).to_broadcast([128, T, 4, 4]),
                ALU.mult, [k("ohg"), k("ewg")], [f"comb:{t}" for t in range(T)])

    def phase_ple(self, s):
        M, S = self.M, self.S
        pb, x1, hT = self.pb, self.x1, self.hT
        M.release(self.mb_start)
        xkeys = lambda t: f"x1:{t}:0"
        ssq = M.alloc([128, NT], F32, "ssq")
        rs = M.alloc([128, NT], F32, "rs")
        junk = M.alloc([128, D], BF16, "junk")
        hn = [M.alloc([128, D], BF16, "hn") for _ in range(2)]
        wpg = M.alloc([128, KC, D], BF16, "wpg")
        wpp = M.alloc([128, 2, D], BF16, "wpp")
        pT = M.alloc([128, 2, SEQ], BF16, "pT")
        ptile = [M.alloc([128, PLE], BF16, "ptile") for _ in range(2)]
        sgt = [M.alloc([128, 512], F32, "sgt") for _ in range(2)]
        tmp = [M.alloc([128, 512], F32, "ptmp") for _ in range(2)]
        ot = [M.alloc([128, D], F32, "ot") for _ in range(2)]
        self.wload(wpg[:], self.din("w_pg")[:, :], "wpg")
        self.wload(wpp[:], self.din("w_pp")[:, :], "wpp")
        self.load_gain(2)
        self.memset("dve", ssq[:], 0.0, ["ssq"])
        for t in range(NT):
            self.act(junk[:], x1[:, t, :], AF.Square, [xkeys(t), f"x1:{t}:1", "ssq"], [f"ssq:{t}", "junk"], accum=ssq[:, t:t + 1])
        self.ts("dve", rs[:], ssq[:], 1.0 / D, EPS, ALU.mult, ALU.add, [f"ssq:{t}" for t in range(NT)], ["rs"])
        self.act(rs[:], rs[:], AF.Ln, ["rs"], ["rs"])
        self.act(rs[:], rs[:], AF.Exp, ["rs"], ["rs"], scale=-0.5)
        for t in range(NT):
            par = t % 2
            tcols = slice(t * 128, (t + 1) * 128)
            self.stt("dve", hn[par][:], x1[:, t, :], rs[:, t:t + 1], self.gbc[:], ALU.mult, ALU.mult,
                     [xkeys(t), f"x1:{t}:1", "rs", "gbc"], [f"hn{par}"])
            for c in range(KC):
                self.tr(self.tp[:, c * 128:(c + 1) * 128], hn[par][:, c * 128:(c + 1) * 128], self.identb[:],
                        [f"hn{par}", "identb"], ["tp"])
            self.act(hT[:, :, tcols], self.tp[:, :].rearrange("p (c n) -> p c n", c=KC), AF.Copy, ["tp"], [f"hT:{t}"])
            self.dma("pool", ptile[par][:], self.din("p")[s, tcols, :], [], [f"ptile{par}"], f"ptile{par}")
            tpb, tk = self.tps[1]
            for c in range(2):
                self.tr(tpb[:, c * 128:(c + 1) * 128], ptile[par][:, c * 128:(c + 1) * 128], self.identb[:],
                        [f"ptile{par}", "identb"], [tk])
            self.act(pT[:, :, tcols], tpb[:, 0:256].rearrange("p (c n) -> p c n", c=2), AF.Copy, [tk], [f"pT:{t}"])
        S.barrier()
        self.load_gain(3)
        ssq2 = M.alloc([128, NT], F32, "ssq2")
        self.memset("dve", ssq2[:], 0.0, ["ssq2"])
        i = 0
        GRP = 8
        for t0 in range(0, NT, GRP):
            tiles = range(t0, t0 + GRP)
            for t in tiles:
                tcols = slice(t * 128, (t + 1) * 128)
                for half in range(2):
                    hc = slice(half * 512, (half + 1) * 512)
                    bg, bp = pb[i % 2], pb[2 + i % 2]
                    kg, kp = f"pb{i % 2}", f"pb{2 + i % 2}"
                    for kc in range(KC):
                        self.mm(bg[:, :], hT[:, kc, tcols], wpg[:, kc, hc], kc == 0, kc == KC - 1, [f"hT:{t}", "wpg"], [kg])
                    for c in range(2):
                        self.mm(bp[:, :], pT[:, c, tcols], wpp[:, c, hc], c == 0, c == 1, [f"pT:{t}", "wpp"], [kp])
                    self.act(sgt[i % 2][:], bg[:, :], AF.Sigmoid, [kg], [f"sgt{i % 2}"])
                    self.tt("dve", tmp[i % 2][:], bp[:, :], sgt[i % 2][:], ALU.mult, [kp, f"sgt{i % 2}"], [f"ptmp{i % 2}"])
                    self.tt("pool", x1[:, t, hc], x1[:, t, hc], tmp[i % 2][:], ALU.add, [f"x1:{t}:{half}", f"ptmp{i % 2}"],
                            [f"x1:{t}:{half}"])
                    i += 1
            for t in tiles:
                self.act(junk[:], x1[:, t, :], AF.Square, [f"x1:{t}:0", f"x1:{t}:1", "ssq2"], [f"ssq2:{t}", "junk"],
                         accum=ssq2[:, t:t + 1])
            gk = [f"ssq2:{t}" for t in tiles]
            gsl = slice(t0, t0 + GRP)
            self.ts("dve", ssq2[:, gsl], ssq2[:, gsl], 1.0 / D, EPS, ALU.mult, ALU.add, gk, gk)
            self.act(ssq2[:, gsl], ssq2[:, gsl], AF.Ln, gk, gk)
            self.act(ssq2[:, gsl], ssq2[:, gsl], AF.Exp, gk, gk, scale=-0.5)
            for t in tiles:
                par = t % 2
                tcols = slice(t * 128, (t + 1) * 128)
                self.stt("dve", ot[par][:], x1[:, t, :], ssq2[:, t:t + 1], self.gbc[:], ALU.mult, ALU.mult,
                         [f"x1:{t}:0", f"x1:{t}:1", f"ssq2:{t}", "gbc"], [f"ot{par}"])
                self.dma("sp", self.out_d[s, tcols, :], ot[par][:], [f"ot{par}"], [], f"ost{par}")
        S.barrier()
        return False


def _consts():
    ident = np.eye(128, dtype=np.float32)
    tri = np.triu(np.ones((128, 128), np.float32))
    j = np.arange(128)[:, None]
    i = np.arange(128)[None, :]
    m = np.where(j > i, -30000.0, 0.0).astype(np.float32)
    mask = np.tile(m, (1, 4))
    hh = np.arange(1, 25, dtype=np.float64)
    slopes = np.exp2(-8.0 * hh / 24.0)
    em = np.zeros((128, 24, 256), np.float64)
    for g, (win, dil) in enumerate(ATT_PAT):
        for h in range(8):
            sl = slopes[g * 8 + h] * dil
            prev = np.where(j >= i, np.exp(-sl * (128 + i - j)), 0.0)
            cur = np.where(i >= j, np.exp(-sl * (i - j)), 0.0)
            em[:, g * 8 + h, 0:128] = prev
            em[:, g * 8 + h, 128:256] = cur
    return ident, tri, mask, em.astype(np.float32)


def _prep_inputs(inputs, nseq, ncores):
    f = lambda a: np.ascontiguousarray(np.asarray(a, dtype=np.float32))
    ident, tri, mask, em = _consts()
    shared = {
        "w_in": f(inputs["w_in"][0]),
        "w_att": f(inputs["w_att_branch"][0]),
        "w_ssd": f(inputs["w_ssd_branch"][0]),
        "w_out": f(inputs["w_out"][0]),
        "w_eg": f(inputs["w_exp_gate"][0]),
        "w_eu": f(inputs["w_exp_up"][0]),
        "w_ed": f(inputs["w_exp_down"][0]),
        "w_pg": f(inputs["w_ple_gate"][0]),
        "w_pp": f(inputs["w_ple_proj"][0]),
        "w_r": f(np.concatenate([inputs["w_router_group"][0], inputs["w_router_expert"][0]], axis=1)),
        "gains": f(np.stack([inputs["norm_mix_g"][0], inputs["norm_ffn_g"][0], inputs["norm_ple_g"][0],
                             inputs["final_norm_g"]])),
        "vec32": f(np.stack([inputs["dt_bias"][0], inputs["a_log"][0], inputs["d_skip"][0]])),
        "b_r": f(np.concatenate([inputs["b_router_group"][0], inputs["b_router_expert"][0]])[None, :]),
        "convw": f(inputs["conv_w"][0].reshape(4, 32, 128).transpose(2, 1, 0)),
        "convb": f(inputs["conv_b"][0].reshape(32, 128).T),
        "ng": f(inputs["ssd_norm_g"][0].reshape(16, 128).T),
        "c_ident": ident, "c_tri": tri, "c_mask": mask, "c_em": em,
    }
    x = np.asarray(inputs["x"], np.float32)
    p = np.asarray(inputs["p"], np.float32)[0]
    maps = []
    for c in range(ncores):
        m = dict(shared)
        m["x"] = np.ascontiguousarray(x[c * nseq:(c + 1) * nseq])
        m["p"] = np.ascontiguousarray(p[c * nseq:(c + 1) * nseq])
        maps.append(m)
    return maps


def kernel(**inputs):
    nseq = 2
    prog = Prog(nseq)
    nc = prog.build()
    maps = _prep_inputs(inputs, nseq, NCORES)
    maps = [{k: v for k, v in m.items() if k in prog._din} for m in maps]
    res = run_bass_kernel_spmd(nc, maps, core_ids=list(range(NCORES)))
    out = np.concatenate([r["out"] for r in res.results], axis=0)
    return out.astype(np.float32)
```
